# Optimizing a Trainium2 kernel written in Bass

```python
import math
import jax, jax.numpy as jnp
from jax import lax
import numpy as np

D_MODEL = 1024
BATCH = 16
SEQ = 4096
DEPTH = 4

CTX_LEN = 256
GRID_W = 64
HEAD_DIM = 64
DIFF_HEADS = 8
DIFF_V_DIM = 2 * HEAD_DIM
SWA_Q_HEADS = 16
SWA_KV_HEADS = 4
SWA_GROUP = SWA_Q_HEADS // SWA_KV_HEADS
WINDOW = 128
Q_BLOCK = 128
N_EXPERTS = 16
EXPERT_FF = 2816
CAPACITY_FACTOR = 2
ROPE_THETA = 10000.0
EPS = 1e-6
NEG_INF = -1e30
ADA_INIT_SCALE = 0.5
F32 = jnp.float32

DIFF_Q_W = DIFF_HEADS * 2 * HEAD_DIM
DIFF_K_W = DIFF_HEADS * 2 * HEAD_DIM
DIFF_V_W = DIFF_HEADS * DIFF_V_DIM
SWA_Q_W = SWA_Q_HEADS * HEAD_DIM
SWA_KV_W = SWA_KV_HEADS * HEAD_DIM
IN_SPLITS = (DIFF_Q_W, DIFF_K_W, DIFF_V_W, SWA_Q_W, SWA_KV_W, SWA_KV_W, D_MODEL, D_MODEL)
IN_W = sum(IN_SPLITS)

kernel_name = "hybrid_diff_swa_ecmoe_dit"


def rmsnorm(x, g):
    xf = x.astype(F32)
    y = xf * lax.rsqrt(jnp.mean(xf * xf, axis=-1, keepdims=True) + EPS)
    return (y * g.astype(F32)).astype(x.dtype)


def modulate(x, g, shift, scale):
    return rmsnorm(x, g) * (1 + scale) + shift


def axial_rope_tables(n):
    rows = n // GRID_W
    row = jnp.repeat(jnp.arange(rows), GRID_W).astype(F32)
    col = jnp.tile(jnp.arange(GRID_W), rows).astype(F32)
    half = HEAD_DIM // 2
    inv_freq = ROPE_THETA ** (-jnp.arange(0, half, 2, dtype=F32) / half)
    ang = jnp.stack([row[:, None] * inv_freq, col[:, None] * inv_freq], axis=1)
    return jnp.cos(ang), jnp.sin(ang)


def apply_rope(x, rope):
    cos, sin = rope
    shp = x.shape
    xr = x.astype(F32).reshape(shp[:-1] + (2, 2, HEAD_DIM // 4))
    bshape = (shp[1],) + (1,) * (x.ndim - 3) + (2, HEAD_DIM // 4)
    cs, sn = cos.reshape(bshape), sin.reshape(bshape)
    x1, x2 = xr[..., 0, :], xr[..., 1, :]
    out = jnp.stack([x1 * cs - x2 * sn, x1 * sn + x2 * cs], axis=-2)
    return out.reshape(shp).astype(x.dtype)


def split_in(p):
    offs = tuple(int(o) for o in np.cumsum(IN_SPLITS)[:-1])
    return jnp.split(p, offs, axis=-1)


def diff_qkv(q, k, v, qg, kg, rope):
    B, n = q.shape[:2]
    q = rmsnorm(q.reshape(B, n, DIFF_HEADS, 2, HEAD_DIM), qg)
    k = rmsnorm(k.reshape(B, n, DIFF_HEADS, 2, HEAD_DIM), kg)
    if rope is not None:
        q, k = apply_rope(q, rope), apply_rope(k, rope)
    return q, k, v.reshape(B, n, DIFF_HEADS, DIFF_V_DIM)


def diff_attend(q, k, v, lam):
    s = jnp.einsum('bqhcd,bkhcd->bhcqk', q, k).astype(F32) * (HEAD_DIM ** -0.5)
    p = jax.nn.softmax(s, axis=-1)
    a = p[:, :, 0] - lam * p[:, :, 1]
    return jnp.einsum('bhqk,bkhd->bqhd', a.astype(v.dtype), v)


def diff_attn_latent(q, k_all, v_all, lam):
    B, N = q.shape[:2]
    nb = N // Q_BLOCK
    qb = jnp.swapaxes(q.reshape((B, nb, Q_BLOCK) + q.shape[2:]), 0, 1)
    o = lax.map(lambda qi: diff_attend(qi, k_all, v_all, lam), qb)
    return jnp.swapaxes(o, 0, 1).reshape(B, N, DIFF_HEADS, DIFF_V_DIM)


def swa_qkv(q, k, v, qg, kg, rope):
    B, n = q.shape[:2]
    q = rmsnorm(q.reshape(B, n, SWA_Q_HEADS, HEAD_DIM), qg)
    k = rmsnorm(k.reshape(B, n, SWA_KV_HEADS, HEAD_DIM), kg)
    if rope is not None:
        q, k = apply_rope(q, rope), apply_rope(k, rope)
    return q, k, v.reshape(B, n, SWA_KV_HEADS, HEAD_DIM)


def sink_attend(q, k, v, sink):
    B, L = q.shape[:2]
    qg = q.reshape(B, L, SWA_KV_HEADS, SWA_GROUP, HEAD_DIM)
    s = jnp.einsum('bqhgd,bkhd->bhgqk', qg, k).astype(F32) * (HEAD_DIM ** -0.5)
    s_sink = jnp.broadcast_to(sink.astype(F32).reshape(1, SWA_KV_HEADS, SWA_GROUP, 1, 1), s.shape[:-1] + (1,))
    p = jax.nn.softmax(jnp.concatenate([s, s_sink], axis=-1), axis=-1)[..., :-1]
    o = jnp.einsum('bhgqk,bkhd->bqhgd', p.astype(v.dtype), v)
    return o.reshape(B, L, SWA_Q_HEADS, HEAD_DIM)


def swa_latent(q, k, v, k_ctx, v_ctx, sink):
    B, N = q.shape[:2]
    nb = N // Q_BLOCK
    qb = q.reshape(B, nb, Q_BLOCK, SWA_KV_HEADS, SWA_GROUP, HEAD_DIM)

    def band(t):
        tp = jnp.pad(t, ((0, 0), (WINDOW, WINDOW), (0, 0), (0, 0)))
        tp = tp.reshape(B, nb + 2, Q_BLOCK, SWA_KV_HEADS, HEAD_DIM)
        return jnp.concatenate([tp[:, :-2], tp[:, 1:-1], tp[:, 2:]], axis=2)

    kw, vw = band(k), band(v)
    blk = jnp.arange(nb)[:, None, None] * Q_BLOCK
    qpos = blk + jnp.arange(Q_BLOCK)[None, :, None]
    kpos = blk - WINDOW + jnp.arange(3 * Q_BLOCK)[None, None, :]
    valid = (jnp.abs(qpos - kpos) <= WINDOW) & (kpos >= 0) & (kpos < N)
    scale = HEAD_DIM ** -0.5
    s_win = jnp.einsum('bnqhgd,bnkhd->bnhgqk', qb, kw).astype(F32) * scale
    s_win = jnp.where(valid[None, :, None, None], s_win, NEG_INF)
    s_ctx = jnp.einsum('bnqhgd,bkhd->bnhgqk', qb, k_ctx).astype(F32) * scale
    s_sink = jnp.broadcast_to(sink.astype(F32).reshape(1, 1, SWA_KV_HEADS, SWA_GROUP, 1, 1),
                              s_ctx.shape[:-1] + (1,))
    p = jax.nn.softmax(jnp.concatenate([s_win, s_ctx, s_sink], axis=-1), axis=-1)
    kwl = 3 * Q_BLOCK
    p_win = p[..., :kwl].astype(v.dtype)
    p_ctx = p[..., kwl:kwl + k_ctx.shape[1]].astype(v.dtype)
    o = (jnp.einsum('bnhgqk,bnkhd->bnqhgd', p_win, vw)
         + jnp.einsum('bnhgqk,bkhd->bnqhgd', p_ctx, v_ctx))
    return o.reshape(B, N, SWA_Q_HEADS, HEAD_DIM)


def expert_choice_ffn(h, w_router, w1, w3, w2):
    B, n, D = h.shape
    cap = CAPACITY_FACTOR * n // N_EXPERTS
    aff = jax.nn.softmax(jnp.einsum('bnd,de->bne', h, w_router).astype(F32), axis=-1)
    gate, idx = lax.top_k(jnp.swapaxes(aff, 1, 2), cap)
    xe = jax.vmap(lambda hb, ib: hb[ib])(h, idx)

    def expert(args):
        xi, a1, a3, a2 = args
        return (jax.nn.silu(xi @ a1) * (xi @ a3)) @ a2

    ye = lax.map(expert, (jnp.swapaxes(xe, 0, 1), w1, w3, w2))
    ye = jnp.swapaxes(ye, 0, 1) * gate[..., None].astype(h.dtype)
    return jax.vmap(lambda ib, yb: jnp.zeros((n, D), h.dtype).at[ib.reshape(-1)].add(yb.reshape(-1, D)))(idx, ye)


def hybrid_layer(xl, xc, sc_l, sc_c, rope, lam_init, last,
                 w_ada, b_ada, norm1_g, w_in, b_gate, diff_q_g, diff_k_g, diff_lambda, diff_subln_g,
                 swa_q_g, swa_k_g, swa_sink, w_branch_a, w_branch_b, w_out, norm2_g,
                 w_router, w_e1, w_e3, w_e2):
    B, N, D = xl.shape
    L = xc.shape[1]
    mod_l = (sc_l @ w_ada + b_ada)[:, None, :]
    mod_c = (sc_c @ w_ada + b_ada)[None, None, :]
    sh1_l, s1_l, g1_l, sh2_l, s2_l, g2_l = jnp.split(mod_l, 6, axis=-1)
    sh1_c, s1_c, g1_c, sh2_c, s2_c, g2_c = jnp.split(mod_c, 6, axis=-1)

    hl = modulate(xl, norm1_g, sh1_l, s1_l)
    hc = modulate(xc, norm1_g, sh1_c, s1_c)
    qa_l, ka_l, va_l, qb_l, kb_l, vb_l, ga_l, gb_l = split_in(hl @ w_in)
    qa_c, ka_c, va_c, qb_c, kb_c, vb_c, ga_c, gb_c = split_in(hc @ w_in)

    lam = (jnp.exp(jnp.sum(diff_lambda[0].astype(F32) * diff_lambda[1].astype(F32)))
           - jnp.exp(jnp.sum(diff_lambda[2].astype(F32) * diff_lambda[3].astype(F32))) + lam_init)

    qa_l, ka_l, va_l = diff_qkv(qa_l, ka_l, va_l, diff_q_g, diff_k_g, rope)
    qa_c, ka_c, va_c = diff_qkv(qa_c, ka_c, va_c, diff_q_g, diff_k_g, None)
    qb_l, kb_l, vb_l = swa_qkv(qb_l, kb_l, vb_l, swa_q_g, swa_k_g, rope)
    qb_c, kb_c, vb_c = swa_qkv(qb_c, kb_c, vb_c, swa_q_g, swa_k_g, None)

    def merge(ya, yb, ga, gb):
        ga = jax.nn.sigmoid(ga + b_gate[:D])
        gb = jax.nn.sigmoid(gb + b_gate[D:])
        return (ga * (ya @ w_branch_a) + gb * (yb @ w_branch_b)) @ w_out

    ya_l = diff_attn_latent(qa_l, jnp.concatenate([ka_l, ka_c], axis=1),
                            jnp.concatenate([va_l, va_c], axis=1), lam)
    ya_l = (rmsnorm(ya_l, diff_subln_g) * (1 - lam_init)).reshape(B, N, DIFF_V_W)
    yb_l = swa_latent(qb_l, kb_l, vb_l, kb_c, vb_c, swa_sink).reshape(B, N, SWA_Q_W)
    xl = xl + g1_l * merge(ya_l, yb_l, ga_l, gb_l)

    if not last:
        ya_c = (rmsnorm(diff_attend(qa_c, ka_c, va_c, lam), diff_subln_g) * (1 - lam_init)).reshape(B, L, DIFF_V_W)
        yb_c = sink_attend(qb_c, kb_c, vb_c, swa_sink).reshape(B, L, SWA_Q_W)
        xc = xc + g1_c * merge(ya_c, yb_c, ga_c, gb_c)

    hl2 = modulate(xl, norm2_g, sh2_l, s2_l)
    xl = xl + g2_l * expert_choice_ffn(hl2, w_router, w_e1, w_e3, w_e2)
    if not last:
        hc2 = modulate(xc, norm2_g, sh2_c, s2_c)
        xc = xc + g2_c * expert_choice_ffn(hc2, w_router, w_e1, w_e3, w_e2)
    return xl, xc


def setup_inputs(seed: int = 0) -> dict:
    key = jax.random.key(seed)
    ks = jax.random.split(key, 24)
    D = D_MODEL

    def nrm(k, shape, scale):
        return jax.random.normal(k, shape, F32) * scale

    return {
        "x": nrm(ks[0], (BATCH, SEQ, D), 1.0),
        "c": nrm(ks[1], (BATCH, D), 1.0),
        "ctx": nrm(ks[2], (BATCH, CTX_LEN, D), 1.0),
        "c_ctx": nrm(ks[3], (D,), 1.0),
        "w_ada": nrm(ks[4], (DEPTH, D, 6 * D), ADA_INIT_SCALE * D ** -0.5),
        "b_ada": nrm(ks[5], (DEPTH, 6 * D), 0.02),
        "norm1_g": 1 + nrm(ks[6], (DEPTH, D), 0.02),
        "w_in": nrm(ks[7], (DEPTH, D, IN_W), D ** -0.5),
        "b_gate": nrm(ks[8], (DEPTH, 2 * D), 0.02),
        "diff_q_g": 1 + nrm(ks[9], (DEPTH, HEAD_DIM), 0.02),
        "diff_k_g": 1 + nrm(ks[10], (DEPTH, HEAD_DIM), 0.02),
        "diff_lambda": nrm(ks[11], (DEPTH, 4, HEAD_DIM), 0.1),
        "diff_subln_g": 1 + nrm(ks[12], (DEPTH, DIFF_V_DIM), 0.02),
        "swa_q_g": 1 + nrm(ks[13], (DEPTH, HEAD_DIM), 0.02),
        "swa_k_g": 1 + nrm(ks[14], (DEPTH, HEAD_DIM), 0.02),
        "swa_sink": nrm(ks[15], (DEPTH, SWA_Q_HEADS), 0.5),
        "w_branch_a": nrm(ks[16], (DEPTH, DIFF_V_W, D), DIFF_V_W ** -0.5),
        "w_branch_b": nrm(ks[17], (DEPTH, SWA_Q_W, D), SWA_Q_W ** -0.5),
        "w_out": nrm(ks[18], (DEPTH, D, D), D ** -0.5),
        "norm2_g": 1 + nrm(ks[19], (DEPTH, D), 0.02),
        "w_router": nrm(ks[20], (DEPTH, D, N_EXPERTS), D ** -0.5),
        "w_e1": nrm(ks[21], (DEPTH, N_EXPERTS, D, EXPERT_FF), D ** -0.5),
        "w_e3": nrm(ks[22], (DEPTH, N_EXPERTS, D, EXPERT_FF), D ** -0.5),
        "w_e2": nrm(ks[23], (DEPTH, N_EXPERTS, EXPERT_FF, D), EXPERT_FF ** -0.5),
    }


def reference(x, c, ctx, c_ctx, w_ada, b_ada, norm1_g, w_in, b_gate, diff_q_g, diff_k_g, diff_lambda,
              diff_subln_g, swa_q_g, swa_k_g, swa_sink, w_branch_a, w_branch_b, w_out, norm2_g,
              w_router, w_e1, w_e3, w_e2):
    rope = axial_rope_tables(x.shape[1])
    sc_l = jax.nn.silu(c)
    sc_c = jax.nn.silu(c_ctx)
    xl, xc = x, ctx
    for i in range(DEPTH):
        lam_init = 0.8 - 0.6 * math.exp(-0.3 * i)
        xl, xc = hybrid_layer(
            xl, xc, sc_l, sc_c, rope, lam_init, i == DEPTH - 1,
            w_ada[i], b_ada[i], norm1_g[i], w_in[i], b_gate[i], diff_q_g[i], diff_k_g[i], diff_lambda[i],
            diff_subln_g[i], swa_q_g[i], swa_k_g[i], swa_sink[i], w_branch_a[i], w_branch_b[i], w_out[i],
            norm2_g[i], w_router[i], w_e1[i], w_e3[i], w_e2[i])
    return xl
```

```python
import numpy as np
import concourse.bass as bass
import concourse.mybir as mybir
from concourse.bass_utils import run_bass_kernel_spmd
from contextlib import ExitStack

F32 = mybir.dt.float32
BF16 = mybir.dt.bfloat16
U32 = mybir.dt.uint32
I32 = mybir.dt.int32
ALU = mybir.AluOpType
ACT = mybir.ActivationFunctionType
AX = mybir.AxisListType

D = 1024
NL = 4096
NCX = 256
NT = NL + NCX
TL = NL // 128
TC = NCX // 128
TT = TL + TC
DEPTH = 4
HD = 64
IN_W = 6656
NE = 16
FF = 2816
FFC = FF // 128
CAP_L = 512
CAP_C = 32
EPS = 1e-6
NB = 2
QKW = 3328

ENGS = ("pe", "act", "dve", "pool", "sp")
SAME_ENG_SYNC = True
DBG = {}


class Res:
    __slots__ = ("name", "w", "r_eng", "r_dma", "excl")

    def __init__(self, name="", excl=False):
        self.name = name
        self.excl = excl
        self.w = None
        self.r_eng = {}
        self.r_dma = []


class Prog:
    def __init__(self, nc, n_dma_slots=None):
        self.nc = nc
        self.eng_obj = {"pe": nc.tensor, "act": nc.scalar, "dve": nc.vector,
                        "pool": nc.gpsimd, "sp": nc.sync}
        self.sem = {e: nc.alloc_semaphore(name=f"sem_{e}") for e in ENGS}
        self.cnt = {e: 0 for e in ENGS}
        n_dma_slots = n_dma_slots or {"sp": 40, "pool": 32, "act": 16}
        self.slots = {q: [[nc.alloc_semaphore(name=f"dq_{q}{i}"), 0] for i in range(n)]
                      for q, n in n_dma_slots.items()}
        self.slot_i = {q: 0 for q in self.slots}
        self.seen = {e: {} for e in ENGS}
        self.ops = {e: [] for e in ENGS}
        self.semobj = {}
        for e in ENGS:
            self.semobj[("E", e)] = self.sem[e]
        self.n_ops = 0

    def _need(self, eng, tok, waits):
        if tok is None:
            return
        if tok[0] == "E":
            if tok[1] == eng and (eng == "pe" or not SAME_ENG_SYNC):
                return
            key = ("E", tok[1])
        else:
            key = ("D",) + tok[1]
        if self.seen[eng].get(key, 0) >= tok[2]:
            return
        self.seen[eng][key] = tok[2]
        waits[key] = max(waits.get(key, 0), tok[2])

    def op(self, eng, fn, reads=(), writes=(), dma=False):
        if DBG.get("max_ops") is not None and self.n_ops >= DBG["max_ops"]:
            return None
        waits = {}
        if any(r.excl for r in reads):
            writes = list(writes) + [r for r in reads if r.excl]
            reads = [r for r in reads if not r.excl]
        for r in reads:
            self._need(eng, r.w, waits)
        for w in writes:
            self._need(eng, w.w, waits)
            for e2, c in w.r_eng.items():
                self._need(eng, ("E", e2, c), waits)
            for t in w.r_dma:
                self._need(eng, t, waits)
        if dma:
            q = eng
            i = self.slot_i[q]
            self.slot_i[q] = (i + 1) % len(self.slots[q])
            slot = self.slots[q][i]
            if slot[1] > 0:
                self._need(eng, ("D", (q, i), slot[1]), waits)
            slot[1] += 16
            tok = ("D", (q, i), slot[1])
            inc = (slot[0], 16)
        else:
            self.cnt[eng] += 1
            tok = ("E", eng, self.cnt[eng])
            inc = (self.sem[eng], 1)
        wl = []
        for key, val in waits.items():
            s = self.sem[key[1]] if key[0] == "E" else self.slots[key[1]][key[2]][0]
            wl.append((s, val))
        self.ops[eng].append((wl, fn, inc))
        self.n_ops += 1
        for r in reads:
            if dma:
                r.r_dma.append(tok)
            else:
                r.r_eng[eng] = tok[2]
        for w in writes:
            w.w = tok
            w.r_eng = {}
            w.r_dma = []
        return tok

    def barrier(self):
        waits = {}
        for e in ENGS:
            if e != "sp" and self.cnt[e] > 0:
                self._need("sp", ("E", e, self.cnt[e]), waits)
        for q, sl in self.slots.items():
            for i, (s, v) in enumerate(sl):
                if v > 0:
                    self._need("sp", ("D", (q, i), v), waits)
        wl = []
        for key, val in waits.items():
            s = self.sem[key[1]] if key[0] == "E" else self.slots[key[1]][key[2]][0]
            wl.append((s, val))
        self.cnt["sp"] += 1
        tok = ("E", "sp", self.cnt["sp"])
        self.ops["sp"].append((wl, lambda e: e.nop(), (self.sem["sp"], 1)))
        for e in ENGS:
            if e == "sp":
                continue
            self.seen[e][("E", "sp")] = tok[2]
            self.cnt[e] += 1
            self.ops[e].append(([(self.sem["sp"], tok[2])], lambda e_: e_.nop(), (self.sem[e], 1)))
            for key in list(self.seen["sp"].keys()):
                self.seen[e][key] = max(self.seen[e].get(key, 0), self.seen["sp"][key])

    def emit(self):
        nc = self.nc
        ops = self.ops
        self.ops = {e: [] for e in ENGS}

        def run(e, lst):
            for wl, fn, inc in lst:
                for s, v in wl:
                    e.wait_ge(s, v)
                ins = fn(e)
                ins.then_inc(inc[0], inc[1])

        with nc.Block() as block:
            @block.tensor
            def _(e):
                run(e, ops["pe"])

            @block.scalar
            def _(e):
                run(e, ops["act"])

            @block.vector
            def _(e):
                run(e, ops["dve"])

            @block.gpsimd
            def _(e):
                run(e, ops["pool"])

            @block.sync
            def _(e):
                run(e, ops["sp"])


class T:
    __slots__ = ("t", "r")

    def __init__(self, t, name=""):
        self.t = t
        self.r = Res(name)


def RS(*tiles):
    return [x.r for x in tiles]


class Ctx:
    pass


class Stage:
    def __init__(self, C, name):
        self.C = C
        self.name = name
        self.es = ExitStack()
        self.n = 0

    def __enter__(self):
        self.es.__enter__()
        return self

    def __exit__(self, *a):
        if a[0] is None:
            self.C.P.barrier()
            self.C.P.emit()
        return self.es.__exit__(*a)

    def sb(self, shape, dt, name=None):
        self.n += 1
        nm = f"{self.name}_{name or 's'}{self.n}"
        return T(self.es.enter_context(self.C.nc.sbuf_tensor(nm, list(shape), dt)), nm)

    def ps(self, shape, dt, name=None):
        self.n += 1
        nm = f"{self.name}_{name or 'p'}{self.n}"
        t = T(self.es.enter_context(self.C.nc.psum_tensor(nm, list(shape), dt)), nm)
        t.r.excl = True
        return t


def bc_mid(ap2d, n):
    p, w = ap2d.shape
    return ap2d.rearrange("p (o w) -> p o w", o=1).to_broadcast([p, n, w])


def bc_last(ap2d, w):
    p, n = ap2d.shape
    return ap2d.rearrange("p (n o) -> p n o", o=1).to_broadcast([p, n, w])


WEIGHT_SPECS = [
    ("w_ada", [DEPTH, D, 6 * D]), ("b_ada", [DEPTH, 6 * D]), ("norm1_g", [DEPTH, D]),
    ("w_in", [DEPTH, D, IN_W]), ("b_gate", [DEPTH, 2 * D]), ("diff_q_g", [DEPTH, HD]),
    ("diff_k_g", [DEPTH, HD]), ("diff_lambda", [DEPTH, 4 * HD]), ("diff_subln_g", [DEPTH, 128]),
    ("swa_q_g", [DEPTH, HD]), ("swa_k_g", [DEPTH, HD]), ("swa_sink", [DEPTH, 16]),
    ("w_branch_a", [DEPTH, D, D]), ("w_branch_b", [DEPTH, D, D]), ("w_out", [DEPTH, D, D]),
    ("norm2_g", [DEPTH, D]), ("w_router", [DEPTH, D, NE]),
    ("w_e1", [DEPTH, NE, D, FF]), ("w_e3", [DEPTH, NE, D, FF]), ("w_e2", [DEPTH, NE, FF, D]),
]


def setup(nc, dbg_outs=(), n_layers=DEPTH, no_experts=False):
    C = Ctx()
    C.n_layers = n_layers
    C.nc = nc
    C.P = Prog(nc)
    di = lambda n, s, dt=F32: nc.dram_tensor(n, list(s), dt, kind="ExternalInput").ap()
    dn = lambda n, s, dt: nc.dram_tensor(n, list(s), dt, kind=("ExternalOutput" if n in dbg_outs else "Internal")).ap()
    C.x = di("x", [NB, NL, D])
    C.ctxin = di("ctxin", [NB, NCX, D])
    C.cT = di("cT", [128, 8, 3])
    C.rope = di("rope", [NL, 96])
    C.W = {}
    for n, s in WEIGHT_SPECS:
        s = [n_layers] + list(s[1:])
        if no_experts and n.startswith("w_e"):
            s = [1, 1, 2, 2]
        C.W[n] = di(n, s)
    C.out = [nc.dram_tensor(f"out{b}", [NL, D], F32, kind="ExternalOutput").ap() for b in range(NB)]
    C.XC = [dn(f"XC{b}", [NCX, D], F32) for b in range(NB)]
    C.QKT = dn("QKT", [NB, QKW, NT], BF16)
    C.VA = dn("VA", [NB, NT, D], BF16)
    C.VB = dn("VB", [NB, NT, 256], BF16)
    C.GT = dn("GT", [NB, NT, 2 * D], BF16)
    C.YA = dn("YA", [NB, NT, D], BF16)
    C.YB = dn("YB", [NB, NT, D], BF16)
    C.H2 = [dn(f"H2_{b}", [NL, D], BF16) for b in range(NB)]
    C.MODV = dn("MODV", [DEPTH, 3, 6 * D], F32)
    C.AFF = dn("AFF", [NB, NE, NT], F32)
    C.H2C = [dn(f"H2C_{b}", [NCX, D], BF16) for b in range(NB)]
    C.es = ExitStack()
    return C


def xres(C, b, t):
    if t < TL:
        return C.out[b][t * 128:(t + 1) * 128, :]
    return C.XC[b][(t - TL) * 128:(t - TL + 1) * 128, :]


def lam_init(l):
    import math
    return 0.8 - 0.6 * math.exp(-0.3 * l)


def stage_consts(C):
    nc, P = C.nc, C.P
    g = C.es
    pers = lambda shape, dt, nm: T(g.enter_context(nc.sbuf_tensor("c_" + nm, list(shape), dt)), nm)
    C.ident_b = pers([128, 128], BF16, "identb")
    C.ident_f = pers([128, 128], F32, "identf")
    C.gain = pers([128, DEPTH, 4, HD], F32, "gain")
    C.subln = pers([128, DEPTH, 128], F32, "subln")
    C.esink = pers([128, DEPTH, 16], F32, "esink")
    C.nlam = pers([128, DEPTH], F32, "nlam")
    C.mprev = pers([128, 4, 128], BF16, "mprev")
    C.mnext = pers([128, 4, 128], BF16, "mnext")
    W = C.W
    with Stage(C, "cst") as S:
        for b in range(NB):
            for k in range(8):
                P.op("sp", lambda e, b=b, k=k: e.dma_start(out=C.out[b][k * 512:(k + 1) * 512, :], in_=C.x[b, k * 512:(k + 1) * 512, :]), dma=True)
            P.op("sp", lambda e, b=b: e.dma_start(out=C.XC[b], in_=C.ctxin[b]), dma=True)
        P.op("pool", lambda e: e.memset(C.ident_f.t[:], 0.0), writes=RS(C.ident_f))
        P.op("pool", lambda e: e.affine_select(out=C.ident_f.t[:], in_=C.ident_f.t[:], pattern=[[-1, 128]],
                                               compare_op=ALU.not_equal, fill=1.0, base=0, channel_multiplier=1),
             reads=RS(C.ident_f), writes=RS(C.ident_f))
        P.op("dve", lambda e: e.tensor_copy(C.ident_b.t[:], C.ident_f.t[:]), reads=RS(C.ident_f), writes=RS(C.ident_b))
        mf = S.sb([128, 4, 128], F32, "mf")
        P.op("pool", lambda e: e.memset(mf.t[:], 1.0), writes=RS(mf))
        P.op("pool", lambda e: e.affine_select(out=mf.t[:], in_=mf.t[:], pattern=[[0, 4], [-1, 128]],
                                               compare_op=ALU.is_ge, fill=0.0, base=0, channel_multiplier=1),
             reads=RS(mf), writes=RS(mf))
        P.op("dve", lambda e: e.tensor_copy(C.mprev.t[:], mf.t[:]), reads=RS(mf), writes=RS(C.mprev))
        mf2 = S.sb([128, 4, 128], F32, "mf2")
        P.op("pool", lambda e: e.memset(mf2.t[:], 1.0), writes=RS(mf2))
        P.op("pool", lambda e: e.affine_select(out=mf2.t[:], in_=mf2.t[:], pattern=[[0, 4], [1, 128]],
                                               compare_op=ALU.is_ge, fill=0.0, base=0, channel_multiplier=-1),
             reads=RS(mf2), writes=RS(mf2))
        P.op("dve", lambda e: e.tensor_copy(C.mnext.t[:], mf2.t[:]), reads=RS(mf2), writes=RS(C.mnext))
        for k, nm in enumerate(["diff_q_g", "diff_k_g", "swa_q_g", "swa_k_g"]):
            for l in range(C.n_layers):
                P.op("sp", lambda e, k=k, nm=nm, l=l: e.dma_start(out=C.gain.t[:, l, k, :], in_=W[nm][l:l + 1, :].to_broadcast([128, HD])),
                     writes=RS(C.gain), dma=True)
        for l in range(C.n_layers):
            P.op("sp", lambda e, l=l: e.dma_start(out=C.subln.t[:, l, :], in_=W["diff_subln_g"][l:l + 1, :].to_broadcast([128, 128])),
                 writes=RS(C.subln), dma=True)
        for l in range(C.n_layers):
            P.op("dve", lambda e, l=l: e.tensor_scalar(C.subln.t[:, l, :], C.subln.t[:, l, :], 1.0 - lam_init(l), None, op0=ALU.mult),
                 reads=RS(C.subln), writes=RS(C.subln))
        for l in range(C.n_layers):
            P.op("sp", lambda e, l=l: e.dma_start(out=C.esink.t[:, l, :], in_=W["swa_sink"][l:l + 1, :].to_broadcast([128, 16])),
                 writes=RS(C.esink), dma=True)
        P.op("act", lambda e: e.activation(out=C.esink.t[:], in_=C.esink.t[:], func=ACT.Exp), reads=RS(C.esink), writes=RS(C.esink))
        dl = S.sb([128, DEPTH, 4, HD], F32, "dl")
        for l in range(C.n_layers):
            P.op("sp", lambda e, l=l: e.dma_start(out=dl.t[:, l].rearrange("p a d -> p (a d)"), in_=W["diff_lambda"][l:l + 1, :].to_broadcast([128, 4 * HD])),
                 writes=RS(dl), dma=True)
        pr = S.sb([128, DEPTH, 2, HD], F32, "pr")
        P.op("dve", lambda e: e.tensor_tensor(out=pr.t[:], in0=dl.t[:, :, 0:4:2, :], in1=dl.t[:, :, 1:4:2, :], op=ALU.mult), reads=RS(dl), writes=RS(pr))
        sm = S.sb([128, DEPTH, 2], F32, "sm")
        P.op("dve", lambda e: e.tensor_reduce(out=sm.t[:], in_=pr.t[:], axis=AX.X, op=ALU.add), reads=RS(pr), writes=RS(sm))
        P.op("act", lambda e: e.activation(out=sm.t[:], in_=sm.t[:], func=ACT.Exp), reads=RS(sm), writes=RS(sm))
        P.op("dve", lambda e: e.tensor_tensor(out=C.nlam.t[:], in0=sm.t[:, :, 1], in1=sm.t[:, :, 0], op=ALU.subtract), reads=RS(sm), writes=RS(C.nlam))
        for l in range(C.n_layers):
            P.op("dve", lambda e, l=l: e.tensor_scalar(C.nlam.t[:, l:l + 1], C.nlam.t[:, l:l + 1], -lam_init(l), None, op0=ALU.add),
                 reads=RS(C.nlam), writes=RS(C.nlam))
        sc = S.sb([128, 8, 3], F32, "sc")
        P.op("sp", lambda e: e.dma_start(out=sc.t[:], in_=C.cT), writes=RS(sc), dma=True)
        P.op("act", lambda e: e.activation(out=sc.t[:], in_=sc.t[:], func=ACT.Silu), reads=RS(sc), writes=RS(sc))
        wa = [S.sb([128, 3072], F32, f"wa{i}") for i in range(2)]
        pm = [S.ps([3, 512], F32, f"pm{i}") for i in range(6)]
        msb = S.sb([3, 6 * D], F32, "msb")
        bsb = S.sb([3, 6 * D], F32, "bsb")
        gsb = S.sb([3, 2, D], F32, "gsb")
        it = 0
        for l in range(C.n_layers):
            P.op("sp", lambda e, l=l: e.dma_start(out=bsb.t[:], in_=W["b_ada"][l:l + 1, :].to_broadcast([3, 6 * D])), writes=RS(bsb), dma=True)
            P.op("sp", lambda e, l=l: e.dma_start(out=gsb.t[:, 0, :], in_=W["norm1_g"][l:l + 1, :].to_broadcast([3, D])), writes=RS(gsb), dma=True)
            P.op("sp", lambda e, l=l: e.dma_start(out=gsb.t[:, 1, :], in_=W["norm2_g"][l:l + 1, :].to_broadcast([3, D])), writes=RS(gsb), dma=True)
            for half in range(2):
                for j in range(8):
                    w = wa[it % 2]
                    it += 1
                    P.op("sp", lambda e, w=w, l=l, j=j, half=half: e.dma_start(out=w.t[:], in_=W["w_ada"][l, j * 128:(j + 1) * 128, half * 3072:(half + 1) * 3072]),
                         writes=RS(w), dma=True)
                    for n in range(6):
                        P.op("pe", lambda e, w=w, j=j, n=n: e.matmul(pm[n].t[:], sc.t[:, j, :], w.t[:, n * 512:(n + 1) * 512], start=(j == 0), stop=(j == 7)),
                             reads=RS(sc, w), writes=RS(pm[n]))
                for n in range(6):
                    c0 = half * 3072 + n * 512
                    P.op("dve", lambda e, n=n, c0=c0: e.tensor_tensor(out=msb.t[:, c0:c0 + 512], in0=pm[n].t[:], in1=bsb.t[:, c0:c0 + 512], op=ALU.add),
                         reads=RS(pm[n], bsb), writes=RS(msb))
            for k, c0 in enumerate((D, 4 * D)):
                P.op("dve", lambda e, k=k, c0=c0: e.scalar_tensor_tensor(out=msb.t[:, c0:c0 + D], in0=msb.t[:, c0:c0 + D], scalar=1.0, in1=gsb.t[:, k, :], op0=ALU.add, op1=ALU.mult),
                     reads=RS(msb, gsb), writes=RS(msb))
            P.op("sp", lambda e, l=l: e.dma_start(out=C.MODV[l], in_=msb.t[:]), reads=RS(msb), dma=True)


def modrow(C, l, r, k):
    return C.MODV[l, r:r + 1, k * D:(k + 1) * D]


def stage_inproj(C, l):
    nc, P, W = C.nc, C.P, C.W
    with Stage(C, f"s1_{l}") as S:
        win = S.sb([128, 8, IN_W], BF16, "win")
        winr = [Res(f"win{j}") for j in range(8)]
        for j in range(0 if not DBG.get("no_win") else 8, 8):
            for h in range(2):
                P.op("pool", lambda e, j=j, h=h: e.dma_start(out=win.t[:, j, h * 3328:(h + 1) * 3328], in_=W["w_in"][l, j * 128:(j + 1) * 128, h * 3328:(h + 1) * 3328]),
                     writes=[winr[j]], dma=True)
        BG = S.sb([128, 2 * D], BF16, "BG")
        P.op("pool", lambda e: e.dma_start(out=BG.t[:], in_=W["b_gate"][l:l + 1, :].to_broadcast([128, 2 * D])), writes=RS(BG), dma=True)
        G1 = S.sb([128, D], F32, "G1")
        SH1 = S.sb([128, D], F32, "SH1")
        xt = [S.sb([128, D], F32, f"xt{i}") for i in range(2)]
        junk = S.sb([128, QKW], F32, "junk")
        ssq = S.sb([128, 1], F32, "ssq")
        rstd = S.sb([128, 1], F32, "rstd")
        hbs = [S.sb([128, D], BF16, f"hb{i}") for i in range(2)]
        hTs = [S.sb([128, 8, 128], BF16, f"hT{i}") for i in range(2)]
        QF = S.sb([128, QKW], F32, "QF")
        QB = S.sb([128, QKW], BF16, "QB")
        QT = S.sb([128, 26, 128], BF16, "QT")
        vout = S.sb([128, 1280], BF16, "vout")
        gout = S.sb([128, 2 * D], BF16, "gout")
        gtmp = [S.sb([128, 512], F32, f"gtmp{i}") for i in range(2)]
        ss = S.sb([128, 52], F32, "ss")
        rs = S.sb([128, 52], F32, "rs")
        rp = S.sb([128, 96], F32, "rp")
        pT = S.ps([128, 8, 128], BF16, "pT")
        pacc = [S.ps([128, 512], F32, f"pacc{i}") for i in range(3)]
        pQT = [S.ps([128, 8, 128], BF16, f"pQT{i}") for i in range(2)]
        gain = C.gain
        ipa = 0
        it = 0
        for b in range(NB):
            for seg in range(2):
                r = b if seg == 0 else 2
                P.op("sp", lambda e, r=r: e.dma_start(out=G1.t[:], in_=modrow(C, l, r, 1).to_broadcast([128, D])), writes=RS(G1), dma=True)
                P.op("sp", lambda e, r=r: e.dma_start(out=SH1.t[:], in_=modrow(C, l, r, 0).to_broadcast([128, D])), writes=RS(SH1), dma=True)
                tiles = range(TL) if seg == 0 else range(TL, TT)
                if "s1_tiles" in DBG:
                    tiles = list(tiles)[:DBG["s1_tiles"]]
                for t in tiles:
                    x = xt[it % 2]
                    hb = hbs[it % 2]
                    hT = hTs[it % 2]
                    it += 1
                    P.op("sp", lambda e, x=x, b=b, t=t: e.dma_start(out=x.t[:], in_=xres(C, b, t)), writes=RS(x), dma=True)
                    if seg == 0:
                        P.op("sp", lambda e, t=t: e.dma_start(out=rp.t[:], in_=C.rope[t * 128:(t + 1) * 128, :]), writes=RS(rp), dma=True)
                    if DBG.get("s1_stop", 99) <= 0:
                        continue
                    P.op("act", lambda e, x=x: e.activation(out=junk.t[:, 0:D], in_=x.t[:], func=ACT.Square, accum_out=ssq.t[:]),
                         reads=RS(x), writes=RS(junk, ssq))
                    P.op("act", lambda e: e.activation(out=rstd.t[:], in_=ssq.t[:], func=ACT.Sqrt, scale=1.0 / D, bias=EPS), reads=RS(ssq), writes=RS(rstd))
                    P.op("dve", lambda e: e.reciprocal(rstd.t[:], rstd.t[:]), reads=RS(rstd), writes=RS(rstd))
                    P.op("dve", lambda e, x=x: e.scalar_tensor_tensor(out=x.t[:], in0=x.t[:], scalar=rstd.t[:, 0:1], in1=G1.t[:], op0=ALU.mult, op1=ALU.mult),
                         reads=RS(x, rstd, G1), writes=RS(x))
                    P.op("dve", lambda e, x=x, hb=hb: e.tensor_tensor(out=hb.t[:], in0=x.t[:], in1=SH1.t[:], op=ALU.add), reads=RS(x, SH1), writes=RS(hb))
                    if DBG.get("s1_stop", 99) <= 1:
                        continue
                    for j in range(8):
                        P.op("pe", lambda e, j=j, hb=hb: e.transpose(pT.t[:, j, :], hb.t[:, j * 128:(j + 1) * 128], C.ident_b.t[:]), reads=RS(hb, C.ident_b), writes=RS(pT))
                    P.op("act", lambda e, hT=hT: e.copy(hT.t[:], pT.t[:]), reads=RS(pT), writes=RS(hT))
                    if DBG.get("s1_stop", 99) <= 2:
                        continue
                    for n in (4, 5, 9, 10, 11, 12, 0, 1, 2, 3, 6, 7, 8):
                        pa = pacc[ipa % 3]
                        ipa += 1
                        for j in range(8):
                            P.op("pe", lambda e, pa=pa, j=j, n=n, hT=hT: e.matmul(pa.t[:], hT.t[:, j, :], win.t[:, j, n * 512:(n + 1) * 512], start=(j == 0), stop=(j == 7)),
                                 reads=[hT.r, winr[j]], writes=RS(pa))
                        if n in (4, 5):
                            P.op("act", lambda e, pa=pa, n=n: e.copy(vout.t[:, (n - 4) * 512:(n - 3) * 512], pa.t[:]), reads=RS(pa), writes=RS(vout))
                        elif n >= 9:
                            gt_ = gtmp[n % 2]
                            c0 = (n - 9) * 512
                            P.op("dve", lambda e, pa=pa, gt_=gt_, c0=c0: e.tensor_tensor(out=gt_.t[:], in0=pa.t[:], in1=BG.t[:, c0:c0 + 512], op=ALU.add),
                                 reads=RS(pa, BG), writes=RS(gt_))
                            P.op("act", lambda e, gt_=gt_, c0=c0: e.activation(out=gout.t[:, c0:c0 + 512], in_=gt_.t[:], func=ACT.Sigmoid), reads=RS(gt_), writes=RS(gout))
                        elif n <= 3:
                            P.op("act", lambda e, pa=pa, n=n: e.copy(QF.t[:, n * 512:(n + 1) * 512], pa.t[:]), reads=RS(pa), writes=RS(QF))
                        elif n in (6, 7):
                            P.op("dve", lambda e, pa=pa, n=n: e.tensor_copy(QF.t[:, 2048 + (n - 6) * 512:2048 + (n - 5) * 512], pa.t[:]), reads=RS(pa), writes=RS(QF))
                        else:
                            P.op("act", lambda e, pa=pa: e.copy(QF.t[:, 3072:3328], pa.t[:, 0:256]), reads=RS(pa), writes=RS(QF))
                            P.op("act", lambda e, pa=pa: e.copy(vout.t[:, 1024:1280], pa.t[:, 256:512]), reads=RS(pa), writes=RS(vout))
                    if DBG.get("s1_stop", 99) <= 3:
                        continue
                    P.op("act", lambda e: e.activation(out=junk.t[:], in_=QF.t[:], func=ACT.Square), reads=RS(QF), writes=RS(junk))
                    P.op("dve", lambda e: e.tensor_reduce(out=ss.t[:], in_=junk.t[:].rearrange("p (g d) -> p g d", d=HD), axis=AX.X, op=ALU.add),
                         reads=RS(junk), writes=RS(ss))
                    P.op("act", lambda e: e.activation(out=rs.t[:], in_=ss.t[:], func=ACT.Sqrt, scale=1.0 / HD, bias=EPS), reads=RS(ss), writes=RS(rs))
                    P.op("dve", lambda e: e.reciprocal(rs.t[:], rs.t[:]), reads=RS(rs), writes=RS(rs))
                    QF3 = QF.t[:].rearrange("p (g d) -> p g d", d=HD)
                    P.op("pool", lambda e, QF3=QF3: e.tensor_tensor(out=QF3, in0=QF3, in1=bc_last(rs.t[:], HD), op=ALU.mult), reads=RS(QF, rs), writes=RS(QF))
                    for k, (g0, ng) in enumerate(((0, 16), (16, 16), (32, 16), (48, 4))):
                        P.op("pool", lambda e, QF3=QF3, k=k, g0=g0, ng=ng: e.tensor_tensor(out=QF3[:, g0:g0 + ng, :], in0=QF3[:, g0:g0 + ng, :], in1=bc_mid(gain.t[:, l, k, :], ng), op=ALU.mult),
                             reads=RS(QF, gain), writes=RS(QF))
                    if DBG.get("s1_stop", 99) <= 4:
                        continue
                    if seg == 0:
                        QF5 = QF.t[:].rearrange("p (g a r i) -> p g a r i", a=2, r=2, i=16)
                        J5 = junk.t[:].rearrange("p (g a r i) -> p g a r i", a=2, r=2, i=16)
                        QB5 = QB.t[:].rearrange("p (g a r i) -> p g a r i", a=2, r=2, i=16)
                        SN = rp.t[:, 64:96].rearrange("p (o a i) -> p o a i", o=1, a=2).to_broadcast([128, 52, 2, 16])
                        P.op("pool", lambda e, QF5=QF5, J5=J5, SN=SN: e.tensor_tensor(out=J5[:, :, :, 0, :], in0=QF5[:, :, :, 1, :], in1=SN, op=ALU.mult),
                             reads=RS(QF, rp), writes=RS(junk))
                        P.op("pool", lambda e, QF5=QF5, J5=J5, SN=SN: e.tensor_tensor(out=J5[:, :, :, 1, :], in0=QF5[:, :, :, 0, :], in1=SN, op=ALU.mult),
                             reads=RS(QF, rp), writes=RS(junk))
                        P.op("dve", lambda e, QF3=QF3: e.tensor_tensor(out=QF3, in0=QF3, in1=bc_mid(rp.t[:, 0:64], 52), op=ALU.mult), reads=RS(QF, rp), writes=RS(QF))
                        P.op("dve", lambda e, QF5=QF5, J5=J5, QB5=QB5: e.tensor_tensor(out=QB5[:, :, :, 0, :], in0=QF5[:, :, :, 0, :], in1=J5[:, :, :, 0, :], op=ALU.subtract),
                             reads=RS(QF, junk), writes=RS(QB))
                        P.op("dve", lambda e, QF5=QF5, J5=J5, QB5=QB5: e.tensor_tensor(out=QB5[:, :, :, 1, :], in0=QF5[:, :, :, 1, :], in1=J5[:, :, :, 1, :], op=ALU.add),
                             reads=RS(QF, junk), writes=RS(QB))
                    else:
                        P.op("dve", lambda e: e.tensor_copy(QB.t[:], QF.t[:]), reads=RS(QF), writes=RS(QB))
                    if DBG.get("s1_stop", 99) <= 5:
                        continue
                    for jb in range(4):
                        pq = pQT[jb % 2]
                        nj = 8 if jb < 3 else 2
                        for jj in range(nj):
                            j = jb * 8 + jj
                            P.op("pe", lambda e, pq=pq, jj=jj, j=j: e.transpose(pq.t[:, jj, :], QB.t[:, j * 128:(j + 1) * 128], C.ident_b.t[:]), reads=RS(QB, C.ident_b), writes=RS(pq))
                        eng = "act" if jb % 2 == 0 else "dve"
                        if eng == "act":
                            P.op("act", lambda e, pq=pq, jb=jb, nj=nj: e.copy(QT.t[:, jb * 8:jb * 8 + nj, :], pq.t[:, 0:nj, :]), reads=RS(pq), writes=RS(QT))
                        else:
                            P.op("dve", lambda e, pq=pq, jb=jb, nj=nj: e.tensor_copy(QT.t[:, jb * 8:jb * 8 + nj, :], pq.t[:, 0:nj, :]), reads=RS(pq), writes=RS(QT))
                    if DBG.get("s1_stop", 99) <= 6:
                        continue
                    tok0 = t * 128
                    if not DBG.get("no_qkt"):
                        for jq in range(0, 26, 2):
                            P.op("sp", lambda e, b=b, tok0=tok0, jq=jq: e.dma_start(out=C.QKT[b, jq * 128:(jq + 2) * 128, tok0:tok0 + 128].rearrange("(j p) n -> p j n", p=128), in_=QT.t[:, jq:jq + 2, :]), reads=RS(QT), dma=True)
                    P.op("sp", lambda e, b=b, tok0=tok0: e.dma_start(out=C.VA[b, tok0:tok0 + 128, :], in_=vout.t[:, 0:D]), reads=RS(vout), dma=True)
                    P.op("sp", lambda e, b=b, tok0=tok0: e.dma_start(out=C.VB[b, tok0:tok0 + 128, :], in_=vout.t[:, D:1280]), reads=RS(vout), dma=True)
                    P.op("sp", lambda e, b=b, tok0=tok0: e.dma_start(out=C.GT[b, tok0:tok0 + 128, :], in_=gout.t[:]), reads=RS(gout), dma=True)


def stage_diff(C, l):
    nc, P = C.nc, C.P
    last = (l == DEPTH - 1)
    with Stage(C, f"s2_{l}") as S:
        KT = [S.sb([128, NT], BF16, f"KT{i}") for i in range(2)]
        QT = [S.sb([128, NT], BF16, f"QT{i}") for i in range(2)]
        V1 = [S.sb([128, TT, 129], BF16, f"V1{i}") for i in range(2)]
        PT = [S.sb([128, 512], BF16, f"PT{i}") for i in range(3)]
        o0 = S.sb([128, 128], F32, "o0")
        av = S.sb([128, 128], F32, "av")
        junk = S.sb([128, 128], F32, "junk")
        rec = S.sb([128, 2], F32, "rec")
        s1 = S.sb([128, 1], F32, "s1")
        ssq = S.sb([128, 1], F32, "ssq")
        rstd = S.sb([128, 1], F32, "rstd")
        yo = [S.sb([128, 4, 128], BF16, f"yo{i}") for i in range(2)]
        ps = [S.ps([128, 512], F32, f"ps{i}") for i in range(2)]
        accs = [[S.ps([128, 3, 129], F32, f"acc{k}_{i}") for i in range(3)] for k in range(2)]
        for v in V1:
            P.op("pool", lambda e, v=v: e.memset(v.t[:, :, 128:129], 1.0), writes=RS(v))

        def acc_ap(accb, c, qs):
            a = c * 4 + qs
            return accb[a // 3], accb[a // 3].t[:, a % 3, :]

        ih = 0
        iyo = 0
        ich = 0
        for b in range(NB):
            for h in range(8):
                kt_, qt_, v1_ = KT[ih % 2], QT[ih % 2], V1[ih % 2]
                ih += 1
                P.op("sp", lambda e, kt_=kt_, b=b, h=h: e.dma_start(out=kt_.t[:], in_=C.QKT[b, 1024 + h * 128:1024 + (h + 1) * 128, :]), writes=RS(kt_), dma=True)
                P.op("sp", lambda e, qt_=qt_, b=b, h=h: e.dma_start(out=qt_.t[:], in_=C.QKT[b, h * 128:(h + 1) * 128, :]), writes=RS(qt_), dma=True)
                vsrc = C.VA[b].rearrange("(kt p) c -> p kt c", p=128)
                for k0 in range(0, TT, 6):
                    k1 = min(TT, k0 + 6)
                    P.op("sp", lambda e, v1_=v1_, k0=k0, k1=k1, h=h, vsrc=vsrc: e.dma_start(out=v1_.t[:, k0:k1, 0:128], in_=vsrc[:, k0:k1, h * 128:(h + 1) * 128]),
                         writes=RS(v1_), dma=True)
                chunks = [(qc * 512, 4, list(range(TT))) for qc in range(8)]
                if not last:
                    chunks.append((NL, 2, [TL, TL + 1]))
                if "s2_chunks" in DBG:
                    chunks = chunks[:DBG["s2_chunks"]] + chunks[8:]
                for (q0, nqs, kts) in chunks:
                    nq = nqs * 128
                    accb = accs[ich % 2]
                    ich += 1
                    started = set()
                    units = [(c, ki, kt) for c in range(2) for ki, kt in enumerate(kts)]

                    def qk(i, units=units, kt_=kt_, qt_=qt_, q0=q0, nq=nq):
                        c, ki, kt = units[i]
                        p_ = ps[i % 2]
                        P.op("pe", lambda e, p_=p_, c=c, kt=kt: e.matmul(
                            p_.t[:, 0:nq], kt_.t[c * 64:(c + 1) * 64, kt * 128:(kt + 1) * 128], qt_.t[c * 64:(c + 1) * 64, q0:q0 + nq], start=True, stop=True),
                            reads=RS(kt_, qt_), writes=RS(p_))

                    qk(0)
                    for i, (c, ki, kt) in enumerate(units):
                        p_ = ps[i % 2]
                        pt_ = PT[i % 3]
                        if i + 1 < len(units):
                            qk(i + 1)
                        P.op("act", lambda e, p_=p_, pt_=pt_, nq=nq: e.activation(out=pt_.t[:, 0:nq], in_=p_.t[:, 0:nq], func=ACT.Exp, scale=0.125),
                             reads=RS(p_), writes=RS(pt_))
                        for qs in range(nqs):
                            at, aap = acc_ap(accb, c, qs)
                            st = (ki == 0) and (id(at) not in started)
                            started.add(id(at))
                            P.op("pe", lambda e, aap=aap, pt_=pt_, v1_=v1_, qs=qs, kt=kt, st=st: e.matmul(
                                aap, pt_.t[:, qs * 128:(qs + 1) * 128], v1_.t[:, kt, :], start=st, stop=False, skip_group_check=True),
                                reads=RS(pt_, v1_), writes=RS(at))
                    y_ = yo[iyo % 2]
                    iyo += 1
                    for qs in range(nqs):
                        a0t, a0 = acc_ap(accb, 0, qs)
                        a1t, a1 = acc_ap(accb, 1, qs)
                        P.op("dve", lambda e, a0=a0: e.reciprocal(rec.t[:, 0:1], a0[:, 128:129]), reads=RS(a0t), writes=RS(rec))
                        P.op("dve", lambda e, a1=a1: e.reciprocal(rec.t[:, 1:2], a1[:, 128:129]), reads=RS(a1t), writes=RS(rec))
                        P.op("dve", lambda e: e.tensor_tensor(out=s1.t[:], in0=rec.t[:, 1:2], in1=C.nlam.t[:, l:l + 1], op=ALU.mult), reads=RS(rec, C.nlam), writes=RS(s1))
                        P.op("dve", lambda e, a0=a0: e.tensor_scalar(o0.t[:], a0[:, 0:128], rec.t[:, 0:1], None, op0=ALU.mult), reads=RS(a0t, rec), writes=RS(o0))
                        P.op("dve", lambda e, a1=a1: e.scalar_tensor_tensor(out=av.t[:], in0=a1[:, 0:128], scalar=s1.t[:, 0:1], in1=o0.t[:], op0=ALU.mult, op1=ALU.add),
                             reads=RS(a1t, s1, o0), writes=RS(av))
                        P.op("pool", lambda e: e.tensor_tensor(out=junk.t[:], in0=av.t[:], in1=av.t[:], op=ALU.mult), reads=RS(av), writes=RS(junk))
                        P.op("dve", lambda e: e.tensor_reduce(out=ssq.t[:], in_=junk.t[:], axis=AX.X, op=ALU.add), reads=RS(junk), writes=RS(ssq))
                        P.op("act", lambda e: e.activation(out=rstd.t[:], in_=ssq.t[:], func=ACT.Ln, scale=1.0 / 128, bias=EPS), reads=RS(ssq), writes=RS(rstd))
                        P.op("act", lambda e: e.activation(out=rstd.t[:], in_=rstd.t[:], func=ACT.Exp, scale=-0.5), reads=RS(rstd), writes=RS(rstd))
                        P.op("dve", lambda e, y_=y_, qs=qs: e.scalar_tensor_tensor(out=y_.t[:, qs, :], in0=av.t[:], scalar=rstd.t[:, 0:1], in1=C.subln.t[:, l, :], op0=ALU.mult, op1=ALU.mult),
                             reads=RS(av, rstd, C.subln), writes=RS(y_))
                    P.op("sp", lambda e, y_=y_, b=b, q0=q0, nqs=nqs, nq=nq, h=h: e.dma_start(
                        out=C.YA[b, q0:q0 + nq, h * 128:(h + 1) * 128].rearrange("(s p) c -> p s c", p=128), in_=y_.t[:, 0:nqs, :]), reads=RS(y_), dma=True)


def stage_swa(C, l):
    nc, P = C.nc, C.P
    last = (l == DEPTH - 1)
    with Stage(C, f"s3_{l}") as S:
        KT = [S.sb([64, NT], BF16, f"KT{i}") for i in range(2)]
        QT = [S.sb([64, 4, NT], BF16, f"QT{i}") for i in range(2)]
        V1 = [S.sb([128, TT, 65], BF16, f"V1{i}") for i in range(2)]
        PT = [S.sb([128, 4, 128], BF16, f"PT{i}") for i in range(3)]
        den = S.sb([128, 4], F32, "den")
        yo = [S.sb([128, 4, 64], BF16, f"yo{i}") for i in range(2)]
        ps = [S.ps([128, 512], F32, f"ps{i}") for i in range(3)]
        acc = [S.ps([128, 4, 65], F32, f"acc{i}") for i in range(2)]
        for v in V1:
            P.op("pool", lambda e, v=v: e.memset(v.t[:, :, 64:65], 1.0), writes=RS(v))
        ig = 0
        ips = 0
        ipt = 0
        iacc = 0
        iyo = 0
        for b in range(NB):
            for g in range(4):
                kt_, qt_, v1_ = KT[ig % 2], QT[ig % 2], V1[ig % 2]
                ig += 1
                P.op("sp", lambda e, kt_=kt_, b=b, g=g: e.dma_start(out=kt_.t[:], in_=C.QKT[b, 3072 + g * 64:3072 + (g + 1) * 64, :]), writes=RS(kt_), dma=True)
                P.op("sp", lambda e, qt_=qt_, b=b, g=g: e.dma_start(out=qt_.t[:], in_=C.QKT[b, 2048 + g * 256:2048 + (g + 1) * 256, :].rearrange("(i d) n -> d i n", d=64)),
                     writes=RS(qt_), dma=True)
                vsrc = C.VB[b].rearrange("(kt p) c -> p kt c", p=128)
                for k0 in range(0, TT, 6):
                    k1 = min(TT, k0 + 6)
                    P.op("sp", lambda e, v1_=v1_, k0=k0, k1=k1, g=g, vsrc=vsrc: e.dma_start(out=v1_.t[:, k0:k1, 0:64], in_=vsrc[:, k0:k1, g * 64:(g + 1) * 64]),
                         writes=RS(v1_), dma=True)
                blocks = list(range(TL)) + ([] if last else [TL, TL + 1])
                if "s3_blocks" in DBG:
                    blocks = blocks[:DBG["s3_blocks"]] + blocks[TL:]
                for n in blocks:
                    if n < TL:
                        kts = ([(n - 1, C.mprev)] if n > 0 else []) + [(n, None)] + ([(n + 1, C.mnext)] if n < TL - 1 else []) + [(TL, None), (TL + 1, None)]
                    else:
                        kts = [(TL, None), (TL + 1, None)]
                    a_ = acc[iacc % 2]
                    iacc += 1

                    def qk(i, kts=kts, kt_=kt_, qt_=qt_, n=n, base=ips):
                        kt = kts[i][0]
                        p_ = ps[(base + i) % 3]
                        P.op("pe", lambda e, p_=p_, kt=kt: e.matmul(
                            p_.t[:].rearrange("p (i q) -> p i q", i=4), kt_.t[:, kt * 128:(kt + 1) * 128], qt_.t[:, :, n * 128:(n + 1) * 128], start=True, stop=True),
                            reads=RS(kt_, qt_), writes=RS(p_))

                    qk(0)
                    for ki, (kt, mask) in enumerate(kts):
                        p_ = ps[ips % 3]
                        ips += 1
                        pt_ = PT[ipt % 3]
                        ipt += 1
                        if ki + 1 < len(kts):
                            qk(ki + 1)
                        P.op("act", lambda e, p_=p_, pt_=pt_: e.activation(out=pt_.t[:].rearrange("p i q -> p (i q)"), in_=p_.t[:], func=ACT.Exp, scale=0.125),
                             reads=RS(p_), writes=RS(pt_))
                        if mask is not None:
                            P.op("pool", lambda e, pt_=pt_, mask=mask: e.tensor_tensor(out=pt_.t[:], in0=pt_.t[:], in1=mask.t[:], op=ALU.mult), reads=RS(pt_, mask), writes=RS(pt_))
                        for i in range(4):
                            P.op("pe", lambda e, a_=a_, pt_=pt_, v1_=v1_, i=i, kt=kt, ki=ki: e.matmul(
                                a_.t[:, i, :], pt_.t[:, i, :], v1_.t[:, kt, :], start=(ki == 0 and i == 0), stop=False, skip_group_check=True),
                                reads=RS(pt_, v1_), writes=RS(a_))
                    y_ = yo[iyo % 2]
                    iyo += 1
                    P.op("dve", lambda e, a_=a_, g=g: e.tensor_tensor(out=den.t[:], in0=a_.t[:, :, 64], in1=C.esink.t[:, l, 4 * g:4 * g + 4], op=ALU.add), reads=RS(a_, C.esink), writes=RS(den))
                    P.op("dve", lambda e: e.reciprocal(den.t[:], den.t[:]), reads=RS(den), writes=RS(den))
                    P.op("dve", lambda e, a_=a_, y_=y_: e.tensor_tensor(out=y_.t[:], in0=a_.t[:, :, 0:64], in1=bc_last(den.t[:], 64), op=ALU.mult), reads=RS(a_, den), writes=RS(y_))
                    P.op("sp", lambda e, y_=y_, b=b, n=n, g=g: e.dma_start(out=C.YB[b, n * 128:(n + 1) * 128, g * 256:(g + 1) * 256], in_=y_.t[:].rearrange("p i d -> p (i d)")),
                         reads=RS(y_), dma=True)


def stage_merge(C, l):
    nc, P, W = C.nc, C.P, C.W
    last = (l == DEPTH - 1)
    with Stage(C, f"s4_{l}") as S:
        wts = {}
        for nm in ("w_branch_a", "w_branch_b", "w_out"):
            wt = S.sb([128, 8, D], BF16, nm)
            for j0 in (0, 4):
                P.op("pool", lambda e, wt=wt, nm=nm, j0=j0: e.dma_start(out=wt.t[:, j0:j0 + 4, :], in_=W[nm][l, j0 * 128:(j0 + 4) * 128, :].rearrange("(j p) n -> p j n", p=128)),
                     writes=RS(wt), dma=True)
            wts[nm] = wt
        wa, wb, wo = wts["w_branch_a"], wts["w_branch_b"], wts["w_out"]
        wr = S.sb([128, 8, NE], F32, "wr")
        P.op("sp", lambda e: e.dma_start(out=wr.t[:], in_=W["w_router"][l].rearrange("(j p) n -> p j n", p=128)), writes=RS(wr), dma=True)
        GATE1 = S.sb([128, D], F32, "GATE1")
        G2 = S.sb([128, D], F32, "G2")
        SH2 = S.sb([128, D], F32, "SH2")
        ya = [S.sb([128, D], BF16, f"ya{i}") for i in range(2)]
        yb = [S.sb([128, D], BF16, f"yb{i}") for i in range(2)]
        gt = [S.sb([128, 2 * D], BF16, f"gt{i}") for i in range(2)]
        xt = [S.sb([128, D], F32, f"xt{i}") for i in range(2)]
        yaT = S.sb([128, 8, 128], BF16, "yaT")
        ybT = S.sb([128, 8, 128], BF16, "ybT")
        msum = S.sb([128, D], F32, "msum")
        tmp = [S.sb([128, 512], F32, f"tmp{i}") for i in range(2)]
        mb = S.sb([128, D], BF16, "mb")
        mT = S.sb([128, 8, 128], BF16, "mT")
        xn = S.sb([128, D], F32, "xn")
        junk = S.sb([128, D], F32, "junk")
        h2f = S.sb([128, D], F32, "h2f")
        h2b = S.sb([128, D], BF16, "h2b")
        h2T = S.sb([128, 8, 128], F32, "h2T")
        ssq = S.sb([128, 1], F32, "ssq")
        rstd = S.sb([128, 1], F32, "rstd")
        mx = S.sb([128, 1], F32, "mx")
        se = S.sb([128, 1], F32, "se")
        ex = S.sb([128, NE], F32, "ex")
        aff = S.sb([128, NE], F32, "aff")
        affT = S.sb([NE, 128], F32, "affT")
        pT = [S.ps([128, 8, 128], BF16, f"pT{i}") for i in range(2)]
        pacc = [S.ps([128, 512], F32, f"pacc{i}") for i in range(2)]
        pTf = [S.ps([128, 4, 128], F32, f"pTf{i}") for i in range(2)]
        plog = S.ps([128, NE], F32, "plog")
        paT = S.ps([NE, 128], F32, "paT")
        it = 0
        for b in range(NB):
            for seg in range(2):
                if seg == 1 and last:
                    continue
                r = b if seg == 0 else 2
                for tl, k in ((GATE1, 2), (G2, 4), (SH2, 3)):
                    P.op("sp", lambda e, tl=tl, r=r, k=k: e.dma_start(out=tl.t[:], in_=modrow(C, l, r, k).to_broadcast([128, D])), writes=RS(tl), dma=True)
                tiles = list(range(TL) if seg == 0 else range(TL, TT))
                if "s4_tiles" in DBG:
                    tiles = tiles[:DBG["s4_tiles"]]
                for t in tiles:
                    tok0 = t * 128
                    ya_, yb_, gt_, x_ = ya[it % 2], yb[it % 2], gt[it % 2], xt[it % 2]
                    it += 1
                    P.op("sp", lambda e, ya_=ya_, b=b, tok0=tok0: e.dma_start(out=ya_.t[:], in_=C.YA[b, tok0:tok0 + 128, :]), writes=RS(ya_), dma=True)
                    P.op("sp", lambda e, yb_=yb_, b=b, tok0=tok0: e.dma_start(out=yb_.t[:], in_=C.YB[b, tok0:tok0 + 128, :]), writes=RS(yb_), dma=True)
                    P.op("sp", lambda e, gt_=gt_, b=b, tok0=tok0: e.dma_start(out=gt_.t[:], in_=C.GT[b, tok0:tok0 + 128, :]), writes=RS(gt_), dma=True)
                    P.op("sp", lambda e, x_=x_, b=b, t=t: e.dma_start(out=x_.t[:], in_=xres(C, b, t)), writes=RS(x_), dma=True)
                    for j in range(8):
                        P.op("pe", lambda e, j=j, ya_=ya_: e.transpose(pT[0].t[:, j, :], ya_.t[:, j * 128:(j + 1) * 128], C.ident_b.t[:]), reads=RS(ya_, C.ident_b), writes=RS(pT[0]))
                    P.op("act", lambda e: e.copy(yaT.t[:], pT[0].t[:]), reads=RS(pT[0]), writes=RS(yaT))
                    for j in range(8):
                        P.op("pe", lambda e, j=j, yb_=yb_: e.transpose(pT[1].t[:, j, :], yb_.t[:, j * 128:(j + 1) * 128], C.ident_b.t[:]), reads=RS(yb_, C.ident_b), writes=RS(pT[1]))
                    P.op("dve", lambda e: e.tensor_copy(ybT.t[:], pT[1].t[:]), reads=RS(pT[1]), writes=RS(ybT))
                    for n in range(2):
                        cs = slice(n * 512, (n + 1) * 512)
                        for j in range(8):
                            P.op("pe", lambda e, j=j, cs=cs: e.matmul(pacc[0].t[:], yaT.t[:, j, :], wa.t[:, j, cs], start=(j == 0), stop=(j == 7)), reads=RS(yaT, wa), writes=RS(pacc[0]))
                        P.op("dve", lambda e, cs=cs, gt_=gt_: e.tensor_tensor(out=msum.t[:, cs], in0=pacc[0].t[:], in1=gt_.t[:, cs], op=ALU.mult), reads=RS(pacc[0], gt_), writes=RS(msum))
                        for j in range(8):
                            P.op("pe", lambda e, j=j, cs=cs: e.matmul(pacc[1].t[:], ybT.t[:, j, :], wb.t[:, j, cs], start=(j == 0), stop=(j == 7)), reads=RS(ybT, wb), writes=RS(pacc[1]))
                        tm = tmp[n]
                        P.op("dve", lambda e, n=n, tm=tm, gt_=gt_: e.tensor_tensor(out=tm.t[:], in0=pacc[1].t[:], in1=gt_.t[:, D + n * 512:D + (n + 1) * 512], op=ALU.mult), reads=RS(pacc[1], gt_), writes=RS(tm))
                        P.op("pool", lambda e, cs=cs, tm=tm: e.tensor_tensor(out=mb.t[:, cs], in0=msum.t[:, cs], in1=tm.t[:], op=ALU.add), reads=RS(msum, tm), writes=RS(mb))
                    for j in range(8):
                        P.op("pe", lambda e, j=j: e.transpose(pT[0].t[:, j, :], mb.t[:, j * 128:(j + 1) * 128], C.ident_b.t[:]), reads=RS(mb, C.ident_b), writes=RS(pT[0]))
                    P.op("act", lambda e: e.copy(mT.t[:], pT[0].t[:]), reads=RS(pT[0]), writes=RS(mT))
                    for n in range(2):
                        cs = slice(n * 512, (n + 1) * 512)
                        pa = pacc[n]
                        tm = tmp[n]
                        for j in range(8):
                            P.op("pe", lambda e, j=j, cs=cs, pa=pa: e.matmul(pa.t[:], mT.t[:, j, :], wo.t[:, j, cs], start=(j == 0), stop=(j == 7)), reads=RS(mT, wo), writes=RS(pa))
                        P.op("dve", lambda e, cs=cs, pa=pa, tm=tm: e.tensor_tensor(out=tm.t[:], in0=pa.t[:], in1=GATE1.t[:, cs], op=ALU.mult), reads=RS(pa, GATE1), writes=RS(tm))
                        P.op("pool", lambda e, cs=cs, tm=tm, x_=x_: e.tensor_tensor(out=xn.t[:, cs], in0=tm.t[:], in1=x_.t[:, cs], op=ALU.add), reads=RS(tm, x_), writes=RS(xn))
                    P.op("sp", lambda e, b=b, t=t: e.dma_start(out=xres(C, b, t), in_=xn.t[:]), reads=RS(xn), dma=True)
                    P.op("act", lambda e: e.activation(out=junk.t[:], in_=xn.t[:], func=ACT.Square, accum_out=ssq.t[:]), reads=RS(xn), writes=RS(junk, ssq))
                    P.op("act", lambda e: e.activation(out=rstd.t[:], in_=ssq.t[:], func=ACT.Sqrt, scale=1.0 / D, bias=EPS), reads=RS(ssq), writes=RS(rstd))
                    P.op("dve", lambda e: e.reciprocal(rstd.t[:], rstd.t[:]), reads=RS(rstd), writes=RS(rstd))
                    P.op("dve", lambda e: e.scalar_tensor_tensor(out=h2f.t[:], in0=xn.t[:], scalar=rstd.t[:, 0:1], in1=G2.t[:], op0=ALU.mult, op1=ALU.mult), reads=RS(xn, rstd, G2), writes=RS(h2f))
                    P.op("pool", lambda e: e.tensor_tensor(out=h2f.t[:], in0=h2f.t[:], in1=SH2.t[:], op=ALU.add), reads=RS(h2f, SH2), writes=RS(h2f))
                    P.op("act", lambda e: e.copy(h2b.t[:], h2f.t[:]), reads=RS(h2f), writes=RS(h2b))
                    if seg == 0:
                        P.op("sp", lambda e, b=b, tok0=tok0: e.dma_start(out=C.H2[b][tok0:tok0 + 128, :], in_=h2b.t[:]), reads=RS(h2b), dma=True)
                    else:
                        P.op("sp", lambda e, b=b, tok0=tok0: e.dma_start(out=C.H2C[b][tok0 - NL:tok0 - NL + 128, :], in_=h2b.t[:]), reads=RS(h2b), dma=True)
                    for half in range(2):
                        for jj in range(4):
                            j = half * 4 + jj
                            P.op("pe", lambda e, half=half, jj=jj, j=j: e.transpose(pTf[half].t[:, jj, :], h2f.t[:, j * 128:(j + 1) * 128], C.ident_f.t[:]), reads=RS(h2f, C.ident_f), writes=RS(pTf[half]))
                    P.op("act", lambda e: e.copy(h2T.t[:, 0:4, :], pTf[0].t[:]), reads=RS(pTf[0]), writes=RS(h2T))
                    P.op("dve", lambda e: e.tensor_copy(h2T.t[:, 4:8, :], pTf[1].t[:]), reads=RS(pTf[1]), writes=RS(h2T))
                    for j in range(8):
                        P.op("pe", lambda e, j=j: e.matmul(plog.t[:], h2T.t[:, j, :], wr.t[:, j, :], start=(j == 0), stop=(j == 7)), reads=RS(h2T, wr), writes=RS(plog))
                    P.op("dve", lambda e: e.tensor_reduce(out=mx.t[:], in_=plog.t[:], axis=AX.X, op=ALU.max), reads=RS(plog), writes=RS(mx))
                    P.op("dve", lambda e: e.tensor_scalar(mx.t[:], mx.t[:], -1.0, None, op0=ALU.mult), reads=RS(mx), writes=RS(mx))
                    P.op("act", lambda e: e.activation(out=ex.t[:], in_=plog.t[:], func=ACT.Exp, bias=mx.t[:, 0:1], accum_out=se.t[:]), reads=RS(plog, mx), writes=RS(ex, se))
                    P.op("dve", lambda e: e.reciprocal(se.t[:], se.t[:]), reads=RS(se), writes=RS(se))
                    P.op("dve", lambda e: e.tensor_scalar(aff.t[:], ex.t[:], se.t[:, 0:1], None, op0=ALU.mult), reads=RS(ex, se), writes=RS(aff))
                    P.op("pe", lambda e: e.transpose(paT.t[:], aff.t[:], C.ident_f.t[:]), reads=RS(aff, C.ident_f), writes=RS(paT))
                    P.op("act", lambda e: e.copy(affT.t[:], paT.t[:]), reads=RS(paT), writes=RS(affT))
                    P.op("sp", lambda e, b=b, tok0=tok0: e.dma_start(out=C.AFF[b, :, tok0:tok0 + 128], in_=affT.t[:]), reads=RS(affT), dma=True)


def alloc_route_tiles(C):
    g = C.es
    nc = C.nc
    pers = lambda shape, dt, nm: T(g.enter_context(nc.sbuf_tensor("c_" + nm, list(shape), dt)), nm)
    C.idxT = pers([128, 4, 32], U32, "idxT")
    C.valsT = pers([128, 4, 32], F32, "valsT")
    C.idxcT = pers([32, 32], U32, "idxcT")
    C.valscT = pers([32, 32], F32, "valscT")


def stage_topk(C, l):
    nc, P = C.nc, C.P
    last = (l == DEPTH - 1)
    with Stage(C, f"s5_{l}") as S:
        work = S.sb([32, NL], F32, "work")
        vals = S.sb([32, CAP_L], F32, "vals")
        idx = S.sb([32, CAP_L], U32, "idx")
        idxf = S.sb([32, CAP_L], F32, "idxf")
        pt = [S.ps([128, 32], F32, f"pt{i}") for i in range(2)]
        for b in range(NB):
            P.op("sp", lambda e, b=b: e.dma_start(out=work.t[b * 16:(b + 1) * 16, :], in_=C.AFF[b, :, 0:NL]), writes=RS(work), dma=True)
        for r in range(CAP_L // 8):
            rs_ = slice(r * 8, (r + 1) * 8)
            P.op("dve", lambda e, rs_=rs_: e.max(out=vals.t[:, rs_], in_=work.t[:]), reads=RS(work), writes=RS(vals))
            P.op("dve", lambda e, rs_=rs_: e.max_index(out=idx.t[:, rs_], in_max=vals.t[:, rs_], in_values=work.t[:]), reads=RS(work, vals), writes=RS(idx))
            P.op("dve", lambda e, rs_=rs_: e.match_replace(out=work.t[:], in_to_replace=vals.t[:, rs_], in_values=work.t[:], imm_value=-1.0), reads=RS(work, vals), writes=RS(work))
        P.op("dve", lambda e: e.tensor_copy(idxf.t[:], idx.t[:]), reads=RS(idx), writes=RS(idxf))
        k = 0
        for s in range(4):
            for src, dst in ((idxf, C.idxT), (vals, C.valsT)):
                p_ = pt[k % 2]
                k += 1
                P.op("pe", lambda e, p_=p_, src=src, s=s: e.transpose(p_.t[:], src.t[:, s * 128:(s + 1) * 128], C.ident_f.t[0:32, 0:32]), reads=RS(src, C.ident_f), writes=RS(p_))
                P.op("dve", lambda e, p_=p_, dst=dst, s=s: e.tensor_copy(dst.t[:, s, :], p_.t[:]), reads=RS(p_), writes=RS(dst))
        if not last:
            workc = S.sb([32, NCX], F32, "workc")
            valsc = S.sb([32, CAP_C], F32, "valsc")
            idxc = S.sb([32, CAP_C], U32, "idxc")
            idxcf = S.sb([32, CAP_C], F32, "idxcf")
            for b in range(NB):
                P.op("sp", lambda e, b=b: e.dma_start(out=workc.t[b * 16:(b + 1) * 16, :], in_=C.AFF[b, :, NL:NT]), writes=RS(workc), dma=True)
            for r in range(CAP_C // 8):
                rs_ = slice(r * 8, (r + 1) * 8)
                P.op("dve", lambda e, rs_=rs_: e.max(out=valsc.t[:, rs_], in_=workc.t[:]), reads=RS(workc), writes=RS(valsc))
                P.op("dve", lambda e, rs_=rs_: e.max_index(out=idxc.t[:, rs_], in_max=valsc.t[:, rs_], in_values=workc.t[:]), reads=RS(workc, valsc), writes=RS(idxc))
                P.op("dve", lambda e, rs_=rs_: e.match_replace(out=workc.t[:], in_to_replace=valsc.t[:, rs_], in_values=workc.t[:], imm_value=-1.0), reads=RS(workc, valsc), writes=RS(workc))
            P.op("dve", lambda e: e.tensor_copy(idxcf.t[:], idxc.t[:]), reads=RS(idxc), writes=RS(idxcf))
            for src, dst in ((idxcf, C.idxcT), (valsc, C.valscT)):
                p_ = pt[k % 2]
                k += 1
                P.op("pe", lambda e, p_=p_, src=src: e.transpose(p_.t[0:32, :], src.t[:], C.ident_f.t[0:32, 0:32]), reads=RS(src, C.ident_f), writes=RS(p_))
                P.op("dve", lambda e, p_=p_, dst=dst: e.tensor_copy(dst.t[:], p_.t[0:32, :]), reads=RS(p_), writes=RS(dst))
        if "dump_route" in DBG:
            P.op("sp", lambda e: e.dma_start(out=C.DIDX[l], in_=C.idxT.t[:].rearrange("p s c -> p (s c)")), reads=RS(C.idxT), dma=True)
            P.op("sp", lambda e: e.dma_start(out=C.DVAL[l], in_=C.valsT.t[:].rearrange("p s c -> p (s c)")), reads=RS(C.valsT), dma=True)


def stage_experts(C, l):
    nc, P, W = C.nc, C.P, C.W
    last = (l == DEPTH - 1)
    ncx = 0 if last else 2 * CAP_C
    NTOK = NB * CAP_L + ncx
    with Stage(C, f"s6_{l}") as S:
        xgT = S.sb([128, 8, NB * CAP_L + 2 * CAP_C], BF16, "xgT")
        hT = S.sb([128, FFC, NB * CAP_L + 2 * CAP_C], BF16, "hT")
        w2 = S.sb([128, FFC, D], BF16, "w2")
        wblk = [S.sb([128, 2, 8, 256], BF16, f"wblk{i}") for i in range(4)]
        xg = [S.sb([128, D], BF16, f"xg{i}") for i in range(2)]
        ye = [S.sb([128, D], F32, f"ye{i}") for i in range(2)]
        stmp = [S.sb([128, 512], F32, f"stmp{i}") for i in range(2)]
        ytmp = [S.sb([128, 512], F32, f"ytmp{i}") for i in range(2)]
        G2g = [S.sb([128, D], F32, f"G2g{i}") for i in range(3)]
        pT = [S.ps([128, 8, 128], BF16, f"pT{i}") for i in range(2)]
        ps1 = [S.ps([128, 512], F32, f"ps1{i}") for i in range(2)]
        ps3 = [S.ps([128, 512], F32, f"ps3{i}") for i in range(2)]
        py = [S.ps([128, 512], F32, f"py{i}") for i in range(2)]
        for r in range(3):
            P.op("sp", lambda e, r=r: e.dma_start(out=G2g[r].t[:], in_=modrow(C, l, r, 5).to_broadcast([128, D])), writes=RS(G2g[r]), dma=True)
        xres_r = [Res(f"xres{b}") for b in range(NB)]
        xcres_r = [Res(f"xcres{b}") for b in range(NB)]
        ixg = ipT = iw = ips = iy = iye = 0
        experts = range(NE) if "s6_experts" not in DBG else range(DBG["s6_experts"])
        for ex in experts:
            for b in range(NB):
                be = b * 16 + ex
                for s in range(4):
                    g_ = xg[ixg % 2]
                    ixg += 1
                    P.op("pool", lambda e, g_=g_, b=b, s=s, be=be: e.indirect_dma_start(
                        out=g_.t[:], out_offset=None, in_=C.H2[b], in_offset=bass.IndirectOffsetOnAxis(ap=C.idxT.t[:, s, be:be + 1], axis=0)),
                        reads=RS(C.idxT), writes=RS(g_), dma=True)
                    p_ = pT[ipT % 2]
                    ipT += 1
                    for j in range(8):
                        P.op("pe", lambda e, p_=p_, g_=g_, j=j: e.transpose(p_.t[:, j, :], g_.t[:, j * 128:(j + 1) * 128], C.ident_b.t[:]), reads=RS(g_, C.ident_b), writes=RS(p_))
                    c0 = b * CAP_L + s * 128
                    P.op("act", lambda e, p_=p_, c0=c0: e.copy(xgT.t[:, :, c0:c0 + 128], p_.t[:]), reads=RS(p_), writes=RS(xgT))
            if not last:
                for b in range(NB):
                    be = b * 16 + ex
                    g_ = xg[ixg % 2]
                    ixg += 1
                    P.op("pool", lambda e, g_=g_, b=b, be=be: e.indirect_dma_start(
                        out=g_.t[0:CAP_C, :], out_offset=None, in_=C.H2C[b], in_offset=bass.IndirectOffsetOnAxis(ap=C.idxcT.t[0:CAP_C, be:be + 1], axis=0)),
                        reads=RS(C.idxcT), writes=RS(g_), dma=True)
                    p_ = pT[ipT % 2]
                    ipT += 1
                    for j in range(8):
                        P.op("pe", lambda e, p_=p_, g_=g_, j=j: e.transpose(p_.t[:, j, 0:CAP_C], g_.t[0:CAP_C, j * 128:(j + 1) * 128], C.ident_b.t[0:CAP_C, 0:CAP_C]), reads=RS(g_, C.ident_b), writes=RS(p_))
                    c0 = NB * CAP_L + b * CAP_C
                    P.op("act", lambda e, p_=p_, c0=c0: e.copy(xgT.t[:, :, c0:c0 + CAP_C], p_.t[:, :, 0:CAP_C]), reads=RS(p_), writes=RS(xgT))
            for f0, f1 in ((0, 6), (6, 12), (12, 17), (17, 22)):
                P.op("pool", lambda e, f0=f0, f1=f1, ex=ex: e.dma_start(out=w2.t[:, f0:f1, :], in_=W["w_e2"][l, ex, f0 * 128:f1 * 128, :].rearrange("(f p) d -> p f d", p=128)),
                     writes=RS(w2), dma=True)
            groups = [(0, 512), (512, 512)] + ([(1024, ncx)] if ncx else [])
            for fg in range(FFC // 2):
                wk = wblk[iw % 4]
                iw += 1
                for k, nm in enumerate(("w_e1", "w_e3")):
                    P.op("pool", lambda e, wk=wk, k=k, nm=nm, fg=fg, ex=ex: e.dma_start(out=wk.t[:, k, :, :], in_=W[nm][l, ex, :, fg * 256:(fg + 1) * 256].rearrange("(j p) f -> p j f", p=128)),
                         writes=RS(wk), dma=True)
                for fc in range(2):
                    f = fg * 2 + fc
                    for (c0, n) in groups:
                        p1, p3 = ps1[ips % 2], ps3[ips % 2]
                        st_ = stmp[ips % 2]
                        ips += 1
                        for j in range(8):
                            P.op("pe", lambda e, p1=p1, wk=wk, j=j, fc=fc, c0=c0, n=n: e.matmul(p1.t[:, 0:n], wk.t[:, 0, j, fc * 128:(fc + 1) * 128], xgT.t[:, j, c0:c0 + n], start=(j == 0), stop=(j == 7)),
                                 reads=RS(wk, xgT), writes=RS(p1))
                        for j in range(8):
                            P.op("pe", lambda e, p3=p3, wk=wk, j=j, fc=fc, c0=c0, n=n: e.matmul(p3.t[:, 0:n], wk.t[:, 1, j, fc * 128:(fc + 1) * 128], xgT.t[:, j, c0:c0 + n], start=(j == 0), stop=(j == 7)),
                                 reads=RS(wk, xgT), writes=RS(p3))
                        P.op("act", lambda e, p1=p1, st_=st_, n=n: e.activation(out=st_.t[:, 0:n], in_=p1.t[:, 0:n], func=ACT.Silu), reads=RS(p1), writes=RS(st_))
                        P.op("dve", lambda e, p3=p3, st_=st_, f=f, c0=c0, n=n: e.tensor_tensor(out=hT.t[:, f, c0:c0 + n], in0=p3.t[:, 0:n], in1=st_.t[:, 0:n], op=ALU.mult), reads=RS(p3, st_), writes=RS(hT))
            subt = [(b, s, b * CAP_L + s * 128, 128) for b in range(NB) for s in range(4)]
            if not last:
                subt += [(b, None, NB * CAP_L + b * CAP_C, CAP_C) for b in range(NB)]
            for (b, s, c0, m) in subt:
                be = b * 16 + ex
                ye_ = ye[iye % 2]
                iye += 1
                if s is not None:
                    gate_ap = C.valsT.t[:, s, be:be + 1]
                    gres = C.valsT
                    g2_ = G2g[b]
                else:
                    gate_ap = C.valscT.t[0:CAP_C, be:be + 1]
                    gres = C.valscT
                    g2_ = G2g[2]
                for n in range(2):
                    p_ = py[iy % 2]
                    yt_ = ytmp[iy % 2]
                    iy += 1
                    for f in range(FFC):
                        P.op("pe", lambda e, p_=p_, f=f, c0=c0, m=m, n=n: e.matmul(p_.t[0:m, :], hT.t[:, f, c0:c0 + m], w2.t[:, f, n * 512:(n + 1) * 512], start=(f == 0), stop=(f == FFC - 1)),
                             reads=RS(hT, w2), writes=RS(p_))
                    P.op("act", lambda e, p_=p_, yt_=yt_, m=m, gate_ap=gate_ap: e.activation(out=yt_.t[0:m, :], in_=p_.t[0:m, :], func=ACT.Copy, scale=gate_ap), reads=RS(p_, gres), writes=RS(yt_))
                    P.op("dve", lambda e, yt_=yt_, ye_=ye_, g2_=g2_, m=m, n=n: e.tensor_tensor(out=ye_.t[0:m, n * 512:(n + 1) * 512], in0=yt_.t[0:m, :], in1=g2_.t[0:m, n * 512:(n + 1) * 512], op=ALU.mult),
                         reads=RS(yt_, g2_), writes=RS(ye_))
                if s is not None:
                    P.op("pool", lambda e, ye_=ye_, b=b, s=s, be=be: e.indirect_dma_start(
                        out=C.out[b], out_offset=bass.IndirectOffsetOnAxis(ap=C.idxT.t[:, s, be:be + 1], axis=0), in_=ye_.t[:], in_offset=None, compute_op=ALU.add),
                        reads=RS(ye_, C.idxT), writes=[xres_r[b]], dma=True)
                else:
                    P.op("pool", lambda e, ye_=ye_, b=b, be=be: e.indirect_dma_start(
                        out=C.XC[b], out_offset=bass.IndirectOffsetOnAxis(ap=C.idxcT.t[0:CAP_C, be:be + 1], axis=0), in_=ye_.t[0:CAP_C, :], in_offset=None, compute_op=ALU.add),
                        reads=RS(ye_, C.idxcT), writes=[xcres_r[b]], dma=True)


def build_program(nc, n_layers=DEPTH, dbg_outs=(), no_experts=False, stages=None):
    C = setup(nc, dbg_outs=dbg_outs, n_layers=n_layers, no_experts=no_experts)
    if "dump_route" in DBG:
        C.DIDX = nc.dram_tensor("DIDX", [DEPTH, 128, 128], U32, kind="ExternalOutput").ap()
        C.DVAL = nc.dram_tensor("DVAL", [DEPTH, 128, 128], F32, kind="ExternalOutput").ap()
    stage_consts(C)
    alloc_route_tiles(C)
    fns = {"inproj": stage_inproj, "diff": stage_diff, "swa": stage_swa, "merge": stage_merge, "topk": stage_topk, "experts": stage_experts}
    order = ["inproj", "diff", "swa", "merge", "topk", "experts"]
    for l in range(n_layers):
        for nm in order:
            if stages is None or nm in stages:
                fns[nm](C, l)
    C.es.close()
    return C


def _rope_table():
    rows = NL // 64
    row = np.repeat(np.arange(rows), 64).astype(np.float32)
    col = np.tile(np.arange(64), rows).astype(np.float32)
    inv = (np.float32(10000.0) ** (-np.arange(0, 32, 2, dtype=np.float32) / np.float32(32))).astype(np.float32)
    ang = np.stack([row[:, None] * inv, col[:, None] * inv], axis=1).astype(np.float32)
    cs, sn = np.cos(ang).astype(np.float32), np.sin(ang).astype(np.float32)
    csx = np.repeat(cs[:, :, None, :], 2, axis=2).reshape(NL, 64)
    return np.ascontiguousarray(np.concatenate([csx, sn.reshape(NL, 32)], axis=1).astype(np.float32))


_NC_CACHE = {}


def kernel(**inputs):
    n_cores = 8
    if "nc" not in _NC_CACHE:
        nc = bass.Bass("TRN2", target_bir_lowering=False)
        build_program(nc)
        _NC_CACHE["nc"] = nc
    nc = _NC_CACHE["nc"]
    f32 = lambda a: np.ascontiguousarray(np.asarray(a, dtype=np.float32))
    x, c, ctx, c_ctx = f32(inputs["x"]), f32(inputs["c"]), f32(inputs["ctx"]), f32(inputs["c_ctx"])
    rope = _rope_table()
    shared = {}
    for n, s in WEIGHT_SPECS:
        shared[n] = f32(inputs[n]).reshape(s)
    in_maps = []
    for core in range(n_cores):
        b0 = core * NB
        cc = np.concatenate([c[b0:b0 + NB], c_ctx[None, :]], axis=0)
        m = {"x": x[b0:b0 + NB], "ctxin": ctx[b0:b0 + NB],
             "cT": np.ascontiguousarray(cc.reshape(3, 8, 128).transpose(2, 1, 0)), "rope": rope}
        m.update(shared)
        in_maps.append(m)
    res = run_bass_kernel_spmd(nc, in_maps, core_ids=list(range(n_cores)))
    out = np.empty((n_cores * NB, NL, D), np.float32)
    for core in range(n_cores):
        for b in range(NB):
            out[core * NB + b] = res.results[core][f"out{b}"]
    return out
```

```python
import numpy as np
import concourse.bass as bass
import concourse.mybir as mybir
from concourse.bass_utils import run_bass_kernel_spmd
from contextlib import ExitStack

F32 = mybir.dt.float32
BF16 = mybir.dt.bfloat16
U32 = mybir.dt.uint32
I32 = mybir.dt.int32
ALU = mybir.AluOpType
ACT = mybir.ActivationFunctionType
AX = mybir.AxisListType

D = 1024
NL = 4096
NCX = 256
NT = NL + NCX
TL = NL // 128
TC = NCX // 128
TT = TL + TC
DEPTH = 4
HD = 64
IN_W = 6656
NE = 16
FF = 2816
FFC = FF // 128
CAP_L = 512
CAP_C = 32
EPS = 1e-6
NB = 2
QKW = 3328

ENGS = ("pe", "act", "dve", "pool", "sp")
SAME_ENG_SYNC = True
DBG = {}


class Res:
    __slots__ = ("name", "w", "r_eng", "r_dma", "excl")

    def __init__(self, name="", excl=False):
        self.name = name
        self.excl = excl
        self.w = None
        self.r_eng = {}
        self.r_dma = []


class Prog:
    def __init__(self, nc, n_dma_slots=None):
        self.nc = nc
        self.eng_obj = {"pe": nc.tensor, "act": nc.scalar, "dve": nc.vector,
                        "pool": nc.gpsimd, "sp": nc.sync}
        self.sem = {e: nc.alloc_semaphore(name=f"sem_{e}") for e in ENGS}
        self.cnt = {e: 0 for e in ENGS}
        n_dma_slots = n_dma_slots or {"sp": 40, "pool": 32, "act": 16}
        self.slots = {q: [[nc.alloc_semaphore(name=f"dq_{q}{i}"), 0] for i in range(n)]
                      for q, n in n_dma_slots.items()}
        self.slot_i = {q: 0 for q in self.slots}
        self.seen = {e: {} for e in ENGS}
        self.ops = {e: [] for e in ENGS}
        self.semobj = {}
        for e in ENGS:
            self.semobj[("E", e)] = self.sem[e]
        self.n_ops = 0

    def _need(self, eng, tok, waits):
        if tok is None:
            return
        if tok[0] == "E":
            if tok[1] == eng and (eng == "pe" or not SAME_ENG_SYNC):
                return
            key = ("E", tok[1])
        else:
            key = ("D",) + tok[1]
        if self.seen[eng].get(key, 0) >= tok[2]:
            return
        self.seen[eng][key] = tok[2]
        waits[key] = max(waits.get(key, 0), tok[2])

    def op(self, eng, fn, reads=(), writes=(), dma=False):
        if DBG.get("max_ops") is not None and self.n_ops >= DBG["max_ops"]:
            return None
        waits = {}
        if any(r.excl for r in reads):
            writes = list(writes) + [r for r in reads if r.excl]
            reads = [r for r in reads if not r.excl]
        for r in reads:
            self._need(eng, r.w, waits)
        for w in writes:
            self._need(eng, w.w, waits)
            for e2, c in w.r_eng.items():
                self._need(eng, ("E", e2, c), waits)
            for t in w.r_dma:
                self._need(eng, t, waits)
        if dma:
            q = eng
            i = self.slot_i[q]
            self.slot_i[q] = (i + 1) % len(self.slots[q])
            slot = self.slots[q][i]
            if slot[1] > 0:
                self._need(eng, ("D", (q, i), slot[1]), waits)
            slot[1] += 16
            tok = ("D", (q, i), slot[1])
            inc = (slot[0], 16)
        else:
            self.cnt[eng] += 1
            tok = ("E", eng, self.cnt[eng])
            inc = (self.sem[eng], 1)
        wl = []
        for key, val in waits.items():
            s = self.sem[key[1]] if key[0] == "E" else self.slots[key[1]][key[2]][0]
            wl.append((s, val))
        self.ops[eng].append((wl, fn, inc))
        self.n_ops += 1
        for r in reads:
            if dma:
                r.r_dma.append(tok)
            else:
                r.r_eng[eng] = tok[2]
        for w in writes:
            w.w = tok
            w.r_eng = {}
            w.r_dma = []
        return tok

    def barrier(self):
        waits = {}
        for e in ENGS:
            if e != "sp" and self.cnt[e] > 0:
                self._need("sp", ("E", e, self.cnt[e]), waits)
        for q, sl in self.slots.items():
            for i, (s, v) in enumerate(sl):
                if v > 0:
                    self._need("sp", ("D", (q, i), v), waits)
        wl = []
        for key, val in waits.items():
            s = self.sem[key[1]] if key[0] == "E" else self.slots[key[1]][key[2]][0]
            wl.append((s, val))
        self.cnt["sp"] += 1
        tok = ("E", "sp", self.cnt["sp"])
        self.ops["sp"].append((wl, lambda e: e.nop(), (self.sem["sp"], 1)))
        for e in ENGS:
            if e == "sp":
                continue
            self.seen[e][("E", "sp")] = tok[2]
            self.cnt[e] += 1
            self.ops[e].append(([(self.sem["sp"], tok[2])], lambda e_: e_.nop(), (self.sem[e], 1)))
            for key in list(self.seen["sp"].keys()):
                self.seen[e][key] = max(self.seen[e].get(key, 0), self.seen["sp"][key])

    def emit(self):
        nc = self.nc
        ops = self.ops
        self.ops = {e: [] for e in ENGS}

        def run(e, lst):
            for wl, fn, inc in lst:
                for s, v in wl:
                    e.wait_ge(s, v)
                ins = fn(e)
                ins.then_inc(inc[0], inc[1])

        with nc.Block() as block:
            @block.tensor
            def _(e):
                run(e, ops["pe"])

            @block.scalar
            def _(e):
                run(e, ops["act"])

            @block.vector
            def _(e):
                run(e, ops["dve"])

            @block.gpsimd
            def _(e):
                run(e, ops["pool"])

            @block.sync
            def _(e):
                run(e, ops["sp"])


class T:
    __slots__ = ("t", "r")

    def __init__(self, t, name=""):
        self.t = t
        self.r = Res(name)


def RS(*tiles):
    return [x.r for x in tiles]


class Ctx:
    pass


class Stage:
    def __init__(self, C, name):
        self.C = C
        self.name = name
        self.es = ExitStack()
        self.n = 0

    def __enter__(self):
        self.es.__enter__()
        return self

    def __exit__(self, *a):
        if a[0] is None:
            self.C.P.barrier()
            self.C.P.emit()
        return self.es.__exit__(*a)

    def sb(self, shape, dt, name=None):
        self.n += 1
        nm = f"{self.name}_{name or 's'}{self.n}"
        return T(self.es.enter_context(self.C.nc.sbuf_tensor(nm, list(shape), dt)), nm)

    def ps(self, shape, dt, name=None):
        self.n += 1
        nm = f"{self.name}_{name or 'p'}{self.n}"
        t = T(self.es.enter_context(self.C.nc.psum_tensor(nm, list(shape), dt)), nm)
        t.r.excl = True
        return t


def bc_mid(ap2d, n):
    p, w = ap2d.shape
    return ap2d.rearrange("p (o w) -> p o w", o=1).to_broadcast([p, n, w])


def bc_last(ap2d, w):
    p, n = ap2d.shape
    return ap2d.rearrange("p (n o) -> p n o", o=1).to_broadcast([p, n, w])


WEIGHT_SPECS = [
    ("w_ada", [DEPTH, D, 6 * D]), ("b_ada", [DEPTH, 6 * D]), ("norm1_g", [DEPTH, D]),
    ("w_in", [DEPTH, D, IN_W]), ("b_gate", [DEPTH, 2 * D]), ("diff_q_g", [DEPTH, HD]),
    ("diff_k_g", [DEPTH, HD]), ("diff_lambda", [DEPTH, 4 * HD]), ("diff_subln_g", [DEPTH, 128]),
    ("swa_q_g", [DEPTH, HD]), ("swa_k_g", [DEPTH, HD]), ("swa_sink", [DEPTH, 16]),
    ("w_branch_a", [DEPTH, D, D]), ("w_branch_b", [DEPTH, D, D]), ("w_out", [DEPTH, D, D]),
    ("norm2_g", [DEPTH, D]), ("w_router", [DEPTH, D, NE]),
    ("w_e1", [DEPTH, NE, D, FF]), ("w_e3", [DEPTH, NE, D, FF]), ("w_e2", [DEPTH, NE, FF, D]),
]


def setup(nc, dbg_outs=(), n_layers=DEPTH, no_experts=False):
    C = Ctx()
    C.n_layers = n_layers
    C.nc = nc
    C.P = Prog(nc)
    di = lambda n, s, dt=F32: nc.dram_tensor(n, list(s), dt, kind="ExternalInput").ap()
    dn = lambda n, s, dt: nc.dram_tensor(n, list(s), dt, kind=("ExternalOutput" if n in dbg_outs else "Internal")).ap()
    C.x = di("x", [NB, NL, D])
    C.ctxin = di("ctxin", [NB, NCX, D])
    C.cT = di("cT", [128, 8, 3])
    C.rope = di("rope", [NL, 96])
    C.W = {}
    for n, s in WEIGHT_SPECS:
        s = [n_layers] + list(s[1:])
        if no_experts and n.startswith("w_e"):
            s = [1, 1, 2, 2]
        C.W[n] = di(n, s)
    C.out = [nc.dram_tensor(f"out{b}", [NL, D], F32, kind="ExternalOutput").ap() for b in range(NB)]
    C.XC = [dn(f"XC{b}", [NCX, D], F32) for b in range(NB)]
    C.QKT = dn("QKT", [NB, QKW, NT], BF16)
    C.VA = dn("VA", [NB, NT, D], BF16)
    C.VB = dn("VB", [NB, NT, 256], BF16)
    C.GT = dn("GT", [NB, NT, 2 * D], BF16)
    C.YA = dn("YA", [NB, NT, D], BF16)
    C.YB = dn("YB", [NB, NT, D], BF16)
    C.H2 = [dn(f"H2_{b}", [NL, D], BF16) for b in range(NB)]
    C.MODV = dn("MODV", [DEPTH, 3, 6 * D], F32)
    C.AFF = dn("AFF", [NB, NE, NT], F32)
    C.H2C = [dn(f"H2C_{b}", [NCX, D], BF16) for b in range(NB)]
    C.es = ExitStack()
    return C


def xres(C, b, t):
    if t < TL:
        return C.out[b][t * 128:(t + 1) * 128, :]
    return C.XC[b][(t - TL) * 128:(t - TL + 1) * 128, :]


def lam_init(l):
    import math
    return 0.8 - 0.6 * math.exp(-0.3 * l)


def stage_consts(C):
    nc, P = C.nc, C.P
    g = C.es
    pers = lambda shape, dt, nm: T(g.enter_context(nc.sbuf_tensor("c_" + nm, list(shape), dt)), nm)
    C.ident_b = pers([128, 128], BF16, "identb")
    C.ident_f = pers([128, 128], F32, "identf")
    C.gain = pers([128, DEPTH, 4, HD], F32, "gain")
    C.subln = pers([128, DEPTH, 128], F32, "subln")
    C.esink = pers([128, DEPTH, 16], F32, "esink")
    C.nlam = pers([128, DEPTH], F32, "nlam")
    C.mprev = pers([128, 4, 128], BF16, "mprev")
    C.mnext = pers([128, 4, 128], BF16, "mnext")
    W = C.W
    with Stage(C, "cst") as S:
        for b in range(NB):
            for k in range(8):
                P.op("sp", lambda e, b=b, k=k: e.dma_start(out=C.out[b][k * 512:(k + 1) * 512, :], in_=C.x[b, k * 512:(k + 1) * 512, :]), dma=True)
            P.op("sp", lambda e, b=b: e.dma_start(out=C.XC[b], in_=C.ctxin[b]), dma=True)
        P.op("pool", lambda e: e.memset(C.ident_f.t[:], 0.0), writes=RS(C.ident_f))
        P.op("pool", lambda e: e.affine_select(out=C.ident_f.t[:], in_=C.ident_f.t[:], pattern=[[-1, 128]],
                                               compare_op=ALU.not_equal, fill=1.0, base=0, channel_multiplier=1),
             reads=RS(C.ident_f), writes=RS(C.ident_f))
        P.op("dve", lambda e: e.tensor_copy(C.ident_b.t[:], C.ident_f.t[:]), reads=RS(C.ident_f), writes=RS(C.ident_b))
        mf = S.sb([128, 4, 128], F32, "mf")
        P.op("pool", lambda e: e.memset(mf.t[:], 1.0), writes=RS(mf))
        P.op("pool", lambda e: e.affine_select(out=mf.t[:], in_=mf.t[:], pattern=[[0, 4], [-1, 128]],
                                               compare_op=ALU.is_ge, fill=0.0, base=0, channel_multiplier=1),
             reads=RS(mf), writes=RS(mf))
        P.op("dve", lambda e: e.tensor_copy(C.mprev.t[:], mf.t[:]), reads=RS(mf), writes=RS(C.mprev))
        mf2 = S.sb([128, 4, 128], F32, "mf2")
        P.op("pool", lambda e: e.memset(mf2.t[:], 1.0), writes=RS(mf2))
        P.op("pool", lambda e: e.affine_select(out=mf2.t[:], in_=mf2.t[:], pattern=[[0, 4], [1, 128]],
                                               compare_op=ALU.is_ge, fill=0.0, base=0, channel_multiplier=-1),
             reads=RS(mf2), writes=RS(mf2))
        P.op("dve", lambda e: e.tensor_copy(C.mnext.t[:], mf2.t[:]), reads=RS(mf2), writes=RS(C.mnext))
        for k, nm in enumerate(["diff_q_g", "diff_k_g", "swa_q_g", "swa_k_g"]):
            for l in range(C.n_layers):
                P.op("sp", lambda e, k=k, nm=nm, l=l: e.dma_start(out=C.gain.t[:, l, k, :], in_=W[nm][l:l + 1, :].to_broadcast([128, HD])),
                     writes=RS(C.gain), dma=True)
        for l in range(C.n_layers):
            P.op("sp", lambda e, l=l: e.dma_start(out=C.subln.t[:, l, :], in_=W["diff_subln_g"][l:l + 1, :].to_broadcast([128, 128])),
                 writes=RS(C.subln), dma=True)
        for l in range(C.n_layers):
            P.op("dve", lambda e, l=l: e.tensor_scalar(C.subln.t[:, l, :], C.subln.t[:, l, :], 1.0 - lam_init(l), None, op0=ALU.mult),
                 reads=RS(C.subln), writes=RS(C.subln))
        for l in range(C.n_layers):
            P.op("sp", lambda e, l=l: e.dma_start(out=C.esink.t[:, l, :], in_=W["swa_sink"][l:l + 1, :].to_broadcast([128, 16])),
                 writes=RS(C.esink), dma=True)
        P.op("act", lambda e: e.activation(out=C.esink.t[:], in_=C.esink.t[:], func=ACT.Exp), reads=RS(C.esink), writes=RS(C.esink))
        dl = S.sb([128, DEPTH, 4, HD], F32, "dl")
        for l in range(C.n_layers):
            P.op("sp", lambda e, l=l: e.dma_start(out=dl.t[:, l].rearrange("p a d -> p (a d)"), in_=W["diff_lambda"][l:l + 1, :].to_broadcast([128, 4 * HD])),
                 writes=RS(dl), dma=True)
        pr = S.sb([128, DEPTH, 2, HD], F32, "pr")
        P.op("dve", lambda e: e.tensor_tensor(out=pr.t[:], in0=dl.t[:, :, 0:4:2, :], in1=dl.t[:, :, 1:4:2, :], op=ALU.mult), reads=RS(dl), writes=RS(pr))
        sm = S.sb([128, DEPTH, 2], F32, "sm")
        P.op("dve", lambda e: e.tensor_reduce(out=sm.t[:], in_=pr.t[:], axis=AX.X, op=ALU.add), reads=RS(pr), writes=RS(sm))
        P.op("act", lambda e: e.activation(out=sm.t[:], in_=sm.t[:], func=ACT.Exp), reads=RS(sm), writes=RS(sm))
        P.op("dve", lambda e: e.tensor_tensor(out=C.nlam.t[:], in0=sm.t[:, :, 1], in1=sm.t[:, :, 0], op=ALU.subtract), reads=RS(sm), writes=RS(C.nlam))
        for l in range(C.n_layers):
            P.op("dve", lambda e, l=l: e.tensor_scalar(C.nlam.t[:, l:l + 1], C.nlam.t[:, l:l + 1], -lam_init(l), None, op0=ALU.add),
                 reads=RS(C.nlam), writes=RS(C.nlam))
        sc = S.sb([128, 8, 3], F32, "sc")
        P.op("sp", lambda e: e.dma_start(out=sc.t[:], in_=C.cT), writes=RS(sc), dma=True)
        P.op("act", lambda e: e.activation(out=sc.t[:], in_=sc.t[:], func=ACT.Silu), reads=RS(sc), writes=RS(sc))
        wa = [S.sb([128, 3072], F32, f"wa{i}") for i in range(2)]
        pm = [S.ps([3, 512], F32, f"pm{i}") for i in range(6)]
        msb = S.sb([3, 6 * D], F32, "msb")
        bsb = S.sb([3, 6 * D], F32, "bsb")
        gsb = S.sb([3, 2, D], F32, "gsb")
        it = 0
        for l in range(C.n_layers):
            P.op("sp", lambda e, l=l: e.dma_start(out=bsb.t[:], in_=W["b_ada"][l:l + 1, :].to_broadcast([3, 6 * D])), writes=RS(bsb), dma=True)
            P.op("sp", lambda e, l=l: e.dma_start(out=gsb.t[:, 0, :], in_=W["norm1_g"][l:l + 1, :].to_broadcast([3, D])), writes=RS(gsb), dma=True)
            P.op("sp", lambda e, l=l: e.dma_start(out=gsb.t[:, 1, :], in_=W["norm2_g"][l:l + 1, :].to_broadcast([3, D])), writes=RS(gsb), dma=True)
            for half in range(2):
                for j in range(8):
                    w = wa[it % 2]
                    it += 1
                    P.op("sp", lambda e, w=w, l=l, j=j, half=half: e.dma_start(out=w.t[:], in_=W["w_ada"][l, j * 128:(j + 1) * 128, half * 3072:(half + 1) * 3072]),
                         writes=RS(w), dma=True)
                    for n in range(6):
                        P.op("pe", lambda e, w=w, j=j, n=n: e.matmul(pm[n].t[:], sc.t[:, j, :], w.t[:, n * 512:(n + 1) * 512], start=(j == 0), stop=(j == 7)),
                             reads=RS(sc, w), writes=RS(pm[n]))
                for n in range(6):
                    c0 = half * 3072 + n * 512
                    P.op("dve", lambda e, n=n, c0=c0: e.tensor_tensor(out=msb.t[:, c0:c0 + 512], in0=pm[n].t[:], in1=bsb.t[:, c0:c0 + 512], op=ALU.add),
                         reads=RS(pm[n], bsb), writes=RS(msb))
            for k, c0 in enumerate((D, 4 * D)):
                P.op("dve", lambda e, k=k, c0=c0: e.scalar_tensor_tensor(out=msb.t[:, c0:c0 + D], in0=msb.t[:, c0:c0 + D], scalar=1.0, in1=gsb.t[:, k, :], op0=ALU.add, op1=ALU.mult),
                     reads=RS(msb, gsb), writes=RS(msb))
            P.op("sp", lambda e, l=l: e.dma_start(out=C.MODV[l], in_=msb.t[:]), reads=RS(msb), dma=True)


def modrow(C, l, r, k):
    return C.MODV[l, r:r + 1, k * D:(k + 1) * D]


def stage_inproj(C, l):
    nc, P, W = C.nc, C.P, C.W
    with Stage(C, f"s1_{l}") as S:
        win = S.sb([128, 8, IN_W], BF16, "win")
        winr = [Res(f"win{j}") for j in range(8)]
        for j in range(0 if not DBG.get("no_win") else 8, 8):
            for h in range(2):
                P.op("pool", lambda e, j=j, h=h: e.dma_start(out=win.t[:, j, h * 3328:(h + 1) * 3328], in_=W["w_in"][l, j * 128:(j + 1) * 128, h * 3328:(h + 1) * 3328]),
                     writes=[winr[j]], dma=True)
        BG = S.sb([128, 2 * D], BF16, "BG")
        P.op("pool", lambda e: e.dma_start(out=BG.t[:], in_=W["b_gate"][l:l + 1, :].to_broadcast([128, 2 * D])), writes=RS(BG), dma=True)
        G1 = S.sb([128, D], F32, "G1")
        SH1 = S.sb([128, D], F32, "SH1")
        xt = [S.sb([128, D], F32, f"xt{i}") for i in range(2)]
        junk = S.sb([128, QKW], F32, "junk")
        ssq = S.sb([128, 1], F32, "ssq")
        rstd = S.sb([128, 1], F32, "rstd")
        hbs = [S.sb([128, D], BF16, f"hb{i}") for i in range(2)]
        hTs = [S.sb([128, 8, 128], BF16, f"hT{i}") for i in range(2)]
        QF = S.sb([128, QKW], F32, "QF")
        QB = S.sb([128, QKW], BF16, "QB")
        QT = S.sb([128, 26, 128], BF16, "QT")
        vout = S.sb([128, 1280], BF16, "vout")
        gout = S.sb([128, 2 * D], BF16, "gout")
        gtmp = [S.sb([128, 512], F32, f"gtmp{i}") for i in range(2)]
        ss = S.sb([128, 52], F32, "ss")
        rs = S.sb([128, 52], F32, "rs")
        rp = S.sb([128, 96], F32, "rp")
        pT = S.ps([128, 8, 128], BF16, "pT")
        pacc = [S.ps([128, 512], F32, f"pacc{i}") for i in range(3)]
        pQT = [S.ps([128, 8, 128], BF16, f"pQT{i}") for i in range(2)]
        gain = C.gain
        ipa = 0
        it = 0
        for b in range(NB):
            for seg in range(2):
                r = b if seg == 0 else 2
                P.op("sp", lambda e, r=r: e.dma_start(out=G1.t[:], in_=modrow(C, l, r, 1).to_broadcast([128, D])), writes=RS(G1), dma=True)
                P.op("sp", lambda e, r=r: e.dma_start(out=SH1.t[:], in_=modrow(C, l, r, 0).to_broadcast([128, D])), writes=RS(SH1), dma=True)
                tiles = range(TL) if seg == 0 else range(TL, TT)
                if "s1_tiles" in DBG:
                    tiles = list(tiles)[:DBG["s1_tiles"]]
                for t in tiles:
                    x = xt[it % 2]
                    hb = hbs[it % 2]
                    hT = hTs[it % 2]
                    it += 1
                    P.op("sp", lambda e, x=x, b=b, t=t: e.dma_start(out=x.t[:], in_=xres(C, b, t)), writes=RS(x), dma=True)
                    if seg == 0:
                        P.op("sp", lambda e, t=t: e.dma_start(out=rp.t[:], in_=C.rope[t * 128:(t + 1) * 128, :]), writes=RS(rp), dma=True)
                    if DBG.get("s1_stop", 99) <= 0:
                        continue
                    P.op("act", lambda e, x=x: e.activation(out=junk.t[:, 0:D], in_=x.t[:], func=ACT.Square, accum_out=ssq.t[:]),
                         reads=RS(x), writes=RS(junk, ssq))
                    P.op("act", lambda e: e.activation(out=rstd.t[:], in_=ssq.t[:], func=ACT.Sqrt, scale=1.0 / D, bias=EPS), reads=RS(ssq), writes=RS(rstd))
                    P.op("dve", lambda e: e.reciprocal(rstd.t[:], rstd.t[:]), reads=RS(rstd), writes=RS(rstd))
                    P.op("dve", lambda e, x=x: e.scalar_tensor_tensor(out=x.t[:], in0=x.t[:], scalar=rstd.t[:, 0:1], in1=G1.t[:], op0=ALU.mult, op1=ALU.mult),
                         reads=RS(x, rstd, G1), writes=RS(x))
                    P.op("dve", lambda e, x=x, hb=hb: e.tensor_tensor(out=hb.t[:], in0=x.t[:], in1=SH1.t[:], op=ALU.add), reads=RS(x, SH1), writes=RS(hb))
                    if DBG.get("s1_stop", 99) <= 1:
                        continue
                    for j in range(8):
                        P.op("pe", lambda e, j=j, hb=hb: e.transpose(pT.t[:, j, :], hb.t[:, j * 128:(j + 1) * 128], C.ident_b.t[:]), reads=RS(hb, C.ident_b), writes=RS(pT))
                    P.op("act", lambda e, hT=hT: e.copy(hT.t[:], pT.t[:]), reads=RS(pT), writes=RS(hT))
                    if DBG.get("s1_stop", 99) <= 2:
                        continue
                    for n in (4, 5, 9, 10, 11, 12, 0, 1, 2, 3, 6, 7, 8):
                        pa = pacc[ipa % 3]
                        ipa += 1
                        for j in range(8):
                            P.op("pe", lambda e, pa=pa, j=j, n=n, hT=hT: e.matmul(pa.t[:], hT.t[:, j, :], win.t[:, j, n * 512:(n + 1) * 512], start=(j == 0), stop=(j == 7)),
                                 reads=[hT.r, winr[j]], writes=RS(pa))
                        if n in (4, 5):
                            P.op("act", lambda e, pa=pa, n=n: e.copy(vout.t[:, (n - 4) * 512:(n - 3) * 512], pa.t[:]), reads=RS(pa), writes=RS(vout))
                        elif n >= 9:
                            gt_ = gtmp[n % 2]
                            c0 = (n - 9) * 512
                            P.op("dve", lambda e, pa=pa, gt_=gt_, c0=c0: e.tensor_tensor(out=gt_.t[:], in0=pa.t[:], in1=BG.t[:, c0:c0 + 512], op=ALU.add),
                                 reads=RS(pa, BG), writes=RS(gt_))
                            P.op("act", lambda e, gt_=gt_, c0=c0: e.activation(out=gout.t[:, c0:c0 + 512], in_=gt_.t[:], func=ACT.Sigmoid), reads=RS(gt_), writes=RS(gout))
                        elif n <= 3:
                            P.op("act", lambda e, pa=pa, n=n: e.copy(QF.t[:, n * 512:(n + 1) * 512], pa.t[:]), reads=RS(pa), writes=RS(QF))
                        elif n in (6, 7):
                            P.op("dve", lambda e, pa=pa, n=n: e.tensor_copy(QF.t[:, 2048 + (n - 6) * 512:2048 + (n - 5) * 512], pa.t[:]), reads=RS(pa), writes=RS(QF))
                        else:
                            P.op("act", lambda e, pa=pa: e.copy(QF.t[:, 3072:3328], pa.t[:, 0:256]), reads=RS(pa), writes=RS(QF))
                            P.op("act", lambda e, pa=pa: e.copy(vout.t[:, 1024:1280], pa.t[:, 256:512]), reads=RS(pa), writes=RS(vout))
                    if DBG.get("s1_stop", 99) <= 3:
                        continue
                    P.op("act", lambda e: e.activation(out=junk.t[:], in_=QF.t[:], func=ACT.Square), reads=RS(QF), writes=RS(junk))
                    P.op("dve", lambda e: e.tensor_reduce(out=ss.t[:], in_=junk.t[:].rearrange("p (g d) -> p g d", d=HD), axis=AX.X, op=ALU.add),
                         reads=RS(junk), writes=RS(ss))
                    P.op("act", lambda e: e.activation(out=rs.t[:], in_=ss.t[:], func=ACT.Sqrt, scale=1.0 / HD, bias=EPS), reads=RS(ss), writes=RS(rs))
                    P.op("dve", lambda e: e.reciprocal(rs.t[:], rs.t[:]), reads=RS(rs), writes=RS(rs))
                    QF3 = QF.t[:].rearrange("p (g d) -> p g d", d=HD)
                    P.op("pool", lambda e, QF3=QF3: e.tensor_tensor(out=QF3, in0=QF3, in1=bc_last(rs.t[:], HD), op=ALU.mult), reads=RS(QF, rs), writes=RS(QF))
                    for k, (g0, ng) in enumerate(((0, 16), (16, 16), (32, 16), (48, 4))):
                        P.op("pool", lambda e, QF3=QF3, k=k, g0=g0, ng=ng: e.tensor_tensor(out=QF3[:, g0:g0 + ng, :], in0=QF3[:, g0:g0 + ng, :], in1=bc_mid(gain.t[:, l, k, :], ng), op=ALU.mult),
                             reads=RS(QF, gain), writes=RS(QF))
                    if DBG.get("s1_stop", 99) <= 4:
                        continue
                    if seg == 0:
                        QF5 = QF.t[:].rearrange("p (g a r i) -> p g a r i", a=2, r=2, i=16)
                        J5 = junk.t[:].rearrange("p (g a r i) -> p g a r i", a=2, r=2, i=16)
                        QB5 = QB.t[:].rearrange("p (g a r i) -> p g a r i", a=2, r=2, i=16)
                        SN = rp.t[:, 64:96].rearrange("p (o a i) -> p o a i", o=1, a=2).to_broadcast([128, 52, 2, 16])
                        P.op("pool", lambda e, QF5=QF5, J5=J5, SN=SN: e.tensor_tensor(out=J5[:, :, :, 0, :], in0=QF5[:, :, :, 1, :], in1=SN, op=ALU.mult),
                             reads=RS(QF, rp), writes=RS(junk))
                        P.op("pool", lambda e, QF5=QF5, J5=J5, SN=SN: e.tensor_tensor(out=J5[:, :, :, 1, :], in0=QF5[:, :, :, 0, :], in1=SN, op=ALU.mult),
                             reads=RS(QF, rp), writes=RS(junk))
                        P.op("dve", lambda e, QF3=QF3: e.tensor_tensor(out=QF3, in0=QF3, in1=bc_mid(rp.t[:, 0:64], 52), op=ALU.mult), reads=RS(QF, rp), writes=RS(QF))
                        P.op("dve", lambda e, QF5=QF5, J5=J5, QB5=QB5: e.tensor_tensor(out=QB5[:, :, :, 0, :], in0=QF5[:, :, :, 0, :], in1=J5[:, :, :, 0, :], op=ALU.subtract),
                             reads=RS(QF, junk), writes=RS(QB))
                        P.op("dve", lambda e, QF5=QF5, J5=J5, QB5=QB5: e.tensor_tensor(out=QB5[:, :, :, 1, :], in0=QF5[:, :, :, 1, :], in1=J5[:, :, :, 1, :], op=ALU.add),
                             reads=RS(QF, junk), writes=RS(QB))
                    else:
                        P.op("dve", lambda e: e.tensor_copy(QB.t[:], QF.t[:]), reads=RS(QF), writes=RS(QB))
                    if DBG.get("s1_stop", 99) <= 5:
                        continue
                    for jb in range(4):
                        pq = pQT[jb % 2]
                        nj = 8 if jb < 3 else 2
                        for jj in range(nj):
                            j = jb * 8 + jj
                            P.op("pe", lambda e, pq=pq, jj=jj, j=j: e.transpose(pq.t[:, jj, :], QB.t[:, j * 128:(j + 1) * 128], C.ident_b.t[:]), reads=RS(QB, C.ident_b), writes=RS(pq))
                        eng = "act" if jb % 2 == 0 else "dve"
                        if eng == "act":
                            P.op("act", lambda e, pq=pq, jb=jb, nj=nj: e.copy(QT.t[:, jb * 8:jb * 8 + nj, :], pq.t[:, 0:nj, :]), reads=RS(pq), writes=RS(QT))
                        else:
                            P.op("dve", lambda e, pq=pq, jb=jb, nj=nj: e.tensor_copy(QT.t[:, jb * 8:jb * 8 + nj, :], pq.t[:, 0:nj, :]), reads=RS(pq), writes=RS(QT))
                    if DBG.get("s1_stop", 99) <= 6:
                        continue
                    tok0 = t * 128
                    if not DBG.get("no_qkt"):
                        for jq in range(0, 26, 2):
                            P.op("sp", lambda e, b=b, tok0=tok0, jq=jq: e.dma_start(out=C.QKT[b, jq * 128:(jq + 2) * 128, tok0:tok0 + 128].rearrange("(j p) n -> p j n", p=128), in_=QT.t[:, jq:jq + 2, :]), reads=RS(QT), dma=True)
                    P.op("sp", lambda e, b=b, tok0=tok0: e.dma_start(out=C.VA[b, tok0:tok0 + 128, :], in_=vout.t[:, 0:D]), reads=RS(vout), dma=True)
                    P.op("sp", lambda e, b=b, tok0=tok0: e.dma_start(out=C.VB[b, tok0:tok0 + 128, :], in_=vout.t[:, D:1280]), reads=RS(vout), dma=True)
                    P.op("sp", lambda e, b=b, tok0=tok0: e.dma_start(out=C.GT[b, tok0:tok0 + 128, :], in_=gout.t[:]), reads=RS(gout), dma=True)


def stage_diff(C, l):
    nc, P = C.nc, C.P
    last = (l == DEPTH - 1)
    with Stage(C, f"s2_{l}") as S:
        KT = [S.sb([128, NT], BF16, f"KT{i}") for i in range(2)]
        QT = [[S.sb([128, NT], BF16, f"QZ{i}_{c}") for c in range(2)] for i in range(2)]
        V1 = [S.sb([128, TT, 129], BF16, f"V1{i}") for i in range(2)]
        PT = [S.sb([128, 512], BF16, f"PT{i}") for i in range(3)]
        o0 = S.sb([128, 128], F32, "o0")
        av = S.sb([128, 128], F32, "av")
        junk = S.sb([128, 128], F32, "junk")
        rec = S.sb([128, 2], F32, "rec")
        s1 = S.sb([128, 1], F32, "s1")
        ssq = S.sb([128, 1], F32, "ssq")
        rstd = S.sb([128, 1], F32, "rstd")
        yo = [S.sb([128, 4, 128], BF16, f"yo{i}") for i in range(2)]
        NPS = 4
        LOOK = 3
        ps = [S.ps([128, 512], F32, f"ps{i}") for i in range(NPS)]
        accb = [S.ps([128, 3, 129], F32, f"acc_{i}") for i in range(3)]
        accS = [[S.sb([128, 3, 129], F32, f"accS{k}_{i}") for i in range(3)] for k in range(2)]
        for v in V1:
            P.op("pool", lambda e, v=v: e.memset(v.t[:, :, 128:129], 1.0), writes=RS(v))
        for qz in QT:
            P.op("pool", lambda e, qz=qz: e.memset(qz[0].t[64:128, :], 0.0), writes=RS(qz[0]))
            P.op("dve", lambda e, qz=qz: e.memset(qz[1].t[0:64, :], 0.0), writes=RS(qz[1]))

        def acc_ap(accb, c, qs):
            a = c * 4 + qs
            return accb[a // 3], accb[a // 3].t[:, a % 3, :]

        ih = 0
        iyo = 0
        ich = 0
        for b in range(NB):
            for h in range(8):
                kt_, qt_, v1_ = KT[ih % 2], QT[ih % 2], V1[ih % 2]
                ih += 1
                P.op("sp", lambda e, kt_=kt_, b=b, h=h: e.dma_start(out=kt_.t[:], in_=C.QKT[b, 1024 + h * 128:1024 + (h + 1) * 128, :]), writes=RS(kt_), dma=True)
                P.op("sp", lambda e, qt_=qt_, b=b, h=h: e.dma_start(out=qt_[0].t[0:64, :], in_=C.QKT[b, h * 128:h * 128 + 64, :]), writes=RS(qt_[0]), dma=True)
                P.op("sp", lambda e, qt_=qt_, b=b, h=h: e.dma_start(out=qt_[1].t[64:128, :], in_=C.QKT[b, h * 128 + 64:(h + 1) * 128, :]), writes=RS(qt_[1]), dma=True)
                vsrc = C.VA[b].rearrange("(kt p) c -> p kt c", p=128)
                for k0 in range(0, TT, 6):
                    k1 = min(TT, k0 + 6)
                    P.op("sp", lambda e, v1_=v1_, k0=k0, k1=k1, h=h, vsrc=vsrc: e.dma_start(out=v1_.t[:, k0:k1, 0:128], in_=vsrc[:, k0:k1, h * 128:(h + 1) * 128]),
                         writes=RS(v1_), dma=True)
                chunks = [(qc * 512, 4, list(range(TT))) for qc in range(8)]
                if not last:
                    chunks.append((NL, 2, [TL, TL + 1]))
                if "s2_chunks" in DBG:
                    chunks = chunks[:DBG["s2_chunks"]] + chunks[8:]
                for (q0, nqs, kts) in chunks:
                    nq = nqs * 128
                    accs_ = accS[ich % 2]
                    ich += 1
                    started = set()
                    units = [(c, ki, kt) for c in range(2) for ki, kt in enumerate(kts)]

                    def qk(i, units=units, kt_=kt_, qt_=qt_, q0=q0, nq=nq):
                        c, ki, kt = units[i]
                        p_ = ps[i % NPS]
                        P.op("pe", lambda e, p_=p_, c=c, kt=kt: e.matmul(
                            p_.t[:, 0:nq], kt_.t[:, kt * 128:(kt + 1) * 128], qt_[c].t[:, q0:q0 + nq], start=True, stop=True),
                            reads=RS(kt_, qt_[c]), writes=RS(p_))

                    for i0 in range(min(LOOK, len(units))):
                        qk(i0)
                    for i, (c, ki, kt) in enumerate(units):
                        p_ = ps[i % NPS]
                        pt_ = PT[i % 3]
                        if i + LOOK < len(units):
                            qk(i + LOOK)
                        P.op("act", lambda e, p_=p_, pt_=pt_, nq=nq: e.activation(out=pt_.t[:, 0:nq], in_=p_.t[:, 0:nq], func=ACT.Exp, scale=0.125),
                             reads=RS(p_), writes=RS(pt_))
                        for qs in range(nqs):
                            at, aap = acc_ap(accb, c, qs)
                            st = (ki == 0) and (id(at) not in started)
                            started.add(id(at))
                            P.op("pe", lambda e, aap=aap, pt_=pt_, v1_=v1_, qs=qs, kt=kt, st=st: e.matmul(
                                aap, pt_.t[:, qs * 128:(qs + 1) * 128], v1_.t[:, kt, :], start=st, stop=False, skip_group_check=True),
                                reads=RS(pt_, v1_), writes=RS(at))
                    for k3 in range(3):
                        P.op("dve", lambda e, k3=k3, accs_=accs_: e.tensor_copy(accs_[k3].t[:], accb[k3].t[:]), reads=RS(accb[k3]), writes=RS(accs_[k3]))
                    y_ = yo[iyo % 2]
                    iyo += 1
                    for qs in range(nqs):
                        a0t, a0 = acc_ap(accs_, 0, qs)
                        a1t, a1 = acc_ap(accs_, 1, qs)
                        P.op("dve", lambda e, a0=a0: e.reciprocal(rec.t[:, 0:1], a0[:, 128:129]), reads=RS(a0t), writes=RS(rec))
                        P.op("dve", lambda e, a1=a1: e.reciprocal(rec.t[:, 1:2], a1[:, 128:129]), reads=RS(a1t), writes=RS(rec))
                        P.op("dve", lambda e: e.tensor_tensor(out=s1.t[:], in0=rec.t[:, 1:2], in1=C.nlam.t[:, l:l + 1], op=ALU.mult), reads=RS(rec, C.nlam), writes=RS(s1))
                        P.op("dve", lambda e, a0=a0: e.tensor_scalar(o0.t[:], a0[:, 0:128], rec.t[:, 0:1], None, op0=ALU.mult), reads=RS(a0t, rec), writes=RS(o0))
                        P.op("dve", lambda e, a1=a1: e.scalar_tensor_tensor(out=av.t[:], in0=a1[:, 0:128], scalar=s1.t[:, 0:1], in1=o0.t[:], op0=ALU.mult, op1=ALU.add),
                             reads=RS(a1t, s1, o0), writes=RS(av))
                        P.op("pool", lambda e: e.tensor_tensor(out=junk.t[:], in0=av.t[:], in1=av.t[:], op=ALU.mult), reads=RS(av), writes=RS(junk))
                        P.op("dve", lambda e: e.tensor_reduce(out=ssq.t[:], in_=junk.t[:], axis=AX.X, op=ALU.add), reads=RS(junk), writes=RS(ssq))
                        P.op("act", lambda e: e.activation(out=rstd.t[:], in_=ssq.t[:], func=ACT.Ln, scale=1.0 / 128, bias=EPS), reads=RS(ssq), writes=RS(rstd))
                        P.op("act", lambda e: e.activation(out=rstd.t[:], in_=rstd.t[:], func=ACT.Exp, scale=-0.5), reads=RS(rstd), writes=RS(rstd))
                        P.op("dve", lambda e, y_=y_, qs=qs: e.scalar_tensor_tensor(out=y_.t[:, qs, :], in0=av.t[:], scalar=rstd.t[:, 0:1], in1=C.subln.t[:, l, :], op0=ALU.mult, op1=ALU.mult),
                             reads=RS(av, rstd, C.subln), writes=RS(y_))
                    P.op("sp", lambda e, y_=y_, b=b, q0=q0, nqs=nqs, nq=nq, h=h: e.dma_start(
                        out=C.YA[b, q0:q0 + nq, h * 128:(h + 1) * 128].rearrange("(s p) c -> p s c", p=128), in_=y_.t[:, 0:nqs, :]), reads=RS(y_), dma=True)


def stage_swa(C, l):
    nc, P = C.nc, C.P
    last = (l == DEPTH - 1)
    with Stage(C, f"s3_{l}") as S:
        KT = [S.sb([64, NT], BF16, f"KT{i}") for i in range(2)]
        QT = [S.sb([64, 4, NT], BF16, f"QT{i}") for i in range(2)]
        V1 = [S.sb([128, TT, 65], BF16, f"V1{i}") for i in range(2)]
        PT = [S.sb([128, 4, 128], BF16, f"PT{i}") for i in range(3)]
        den = S.sb([128, 4], F32, "den")
        yo = [S.sb([128, 4, 64], BF16, f"yo{i}") for i in range(2)]
        ps = [S.ps([128, 512], F32, f"ps{i}") for i in range(3)]
        acc = [S.ps([128, 4, 65], F32, f"acc{i}") for i in range(2)]
        for v in V1:
            P.op("pool", lambda e, v=v: e.memset(v.t[:, :, 64:65], 1.0), writes=RS(v))
        ig = 0
        ips = 0
        ipt = 0
        iacc = 0
        iyo = 0
        for b in range(NB):
            for g in range(4):
                kt_, qt_, v1_ = KT[ig % 2], QT[ig % 2], V1[ig % 2]
                ig += 1
                P.op("sp", lambda e, kt_=kt_, b=b, g=g: e.dma_start(out=kt_.t[:], in_=C.QKT[b, 3072 + g * 64:3072 + (g + 1) * 64, :]), writes=RS(kt_), dma=True)
                P.op("sp", lambda e, qt_=qt_, b=b, g=g: e.dma_start(out=qt_.t[:], in_=C.QKT[b, 2048 + g * 256:2048 + (g + 1) * 256, :].rearrange("(i d) n -> d i n", d=64)),
                     writes=RS(qt_), dma=True)
                vsrc = C.VB[b].rearrange("(kt p) c -> p kt c", p=128)
                for k0 in range(0, TT, 6):
                    k1 = min(TT, k0 + 6)
                    P.op("sp", lambda e, v1_=v1_, k0=k0, k1=k1, g=g, vsrc=vsrc: e.dma_start(out=v1_.t[:, k0:k1, 0:64], in_=vsrc[:, k0:k1, g * 64:(g + 1) * 64]),
                         writes=RS(v1_), dma=True)
                blocks = list(range(TL)) + ([] if last else [TL, TL + 1])
                if "s3_blocks" in DBG:
                    blocks = blocks[:DBG["s3_blocks"]] + blocks[TL:]
                for n in blocks:
                    if n < TL:
                        kts = ([(n - 1, C.mprev)] if n > 0 else []) + [(n, None)] + ([(n + 1, C.mnext)] if n < TL - 1 else []) + [(TL, None), (TL + 1, None)]
                    else:
                        kts = [(TL, None), (TL + 1, None)]
                    a_ = acc[iacc % 2]
                    iacc += 1

                    def qk(i, kts=kts, kt_=kt_, qt_=qt_, n=n, base=ips):
                        kt = kts[i][0]
                        p_ = ps[(base + i) % 3]
                        P.op("pe", lambda e, p_=p_, kt=kt: e.matmul(
                            p_.t[:].rearrange("p (i q) -> p i q", i=4), kt_.t[:, kt * 128:(kt + 1) * 128], qt_.t[:, :, n * 128:(n + 1) * 128], start=True, stop=True),
                            reads=RS(kt_, qt_), writes=RS(p_))

                    qk(0)
                    qk(1)
                    for ki, (kt, mask) in enumerate(kts):
                        p_ = ps[ips % 3]
                        ips += 1
                        pt_ = PT[ipt % 3]
                        ipt += 1
                        if ki + 2 < len(kts):
                            qk(ki + 2)
                        P.op("act", lambda e, p_=p_, pt_=pt_: e.activation(out=pt_.t[:].rearrange("p i q -> p (i q)"), in_=p_.t[:], func=ACT.Exp, scale=0.125),
                             reads=RS(p_), writes=RS(pt_))
                        if mask is not None:
                            P.op("pool", lambda e, pt_=pt_, mask=mask: e.tensor_tensor(out=pt_.t[:], in0=pt_.t[:], in1=mask.t[:], op=ALU.mult), reads=RS(pt_, mask), writes=RS(pt_))
                        for i in range(4):
                            P.op("pe", lambda e, a_=a_, pt_=pt_, v1_=v1_, i=i, kt=kt, ki=ki: e.matmul(
                                a_.t[:, i, :], pt_.t[:, i, :], v1_.t[:, kt, :], start=(ki == 0 and i == 0), stop=False, skip_group_check=True),
                                reads=RS(pt_, v1_), writes=RS(a_))
                    y_ = yo[iyo % 2]
                    iyo += 1
                    P.op("dve", lambda e, a_=a_, g=g: e.tensor_tensor(out=den.t[:], in0=a_.t[:, :, 64], in1=C.esink.t[:, l, 4 * g:4 * g + 4], op=ALU.add), reads=RS(a_, C.esink), writes=RS(den))
                    P.op("dve", lambda e: e.reciprocal(den.t[:], den.t[:]), reads=RS(den), writes=RS(den))
                    P.op("dve", lambda e, a_=a_, y_=y_: e.tensor_tensor(out=y_.t[:], in0=a_.t[:, :, 0:64], in1=bc_last(den.t[:], 64), op=ALU.mult), reads=RS(a_, den), writes=RS(y_))
                    P.op("sp", lambda e, y_=y_, b=b, n=n, g=g: e.dma_start(out=C.YB[b, n * 128:(n + 1) * 128, g * 256:(g + 1) * 256], in_=y_.t[:].rearrange("p i d -> p (i d)")),
                         reads=RS(y_), dma=True)


def stage_merge(C, l):
    nc, P, W = C.nc, C.P, C.W
    last = (l == DEPTH - 1)
    with Stage(C, f"s4_{l}") as S:
        wts = {}
        for nm in ("w_branch_a", "w_branch_b", "w_out"):
            wt = S.sb([128, 8, D], BF16, nm)
            for j0 in (0, 4):
                P.op("pool", lambda e, wt=wt, nm=nm, j0=j0: e.dma_start(out=wt.t[:, j0:j0 + 4, :], in_=W[nm][l, j0 * 128:(j0 + 4) * 128, :].rearrange("(j p) n -> p j n", p=128)),
                     writes=RS(wt), dma=True)
            wts[nm] = wt
        wa, wb, wo = wts["w_branch_a"], wts["w_branch_b"], wts["w_out"]
        wr = S.sb([128, 8, NE], F32, "wr")
        P.op("sp", lambda e: e.dma_start(out=wr.t[:], in_=W["w_router"][l].rearrange("(j p) n -> p j n", p=128)), writes=RS(wr), dma=True)
        GATE1 = S.sb([128, D], F32, "GATE1")
        G2 = S.sb([128, D], F32, "G2")
        SH2 = S.sb([128, D], F32, "SH2")
        ya = [S.sb([128, D], BF16, f"ya{i}") for i in range(2)]
        yb = [S.sb([128, D], BF16, f"yb{i}") for i in range(2)]
        gt = [S.sb([128, 2 * D], BF16, f"gt{i}") for i in range(2)]
        xt = [S.sb([128, D], F32, f"xt{i}") for i in range(2)]
        yaT = S.sb([128, 8, 128], BF16, "yaT")
        ybT = S.sb([128, 8, 128], BF16, "ybT")
        msum = S.sb([128, D], F32, "msum")
        tmp = [S.sb([128, 512], F32, f"tmp{i}") for i in range(2)]
        mb = S.sb([128, D], BF16, "mb")
        mT = S.sb([128, 8, 128], BF16, "mT")
        xn = S.sb([128, D], F32, "xn")
        junk = S.sb([128, D], F32, "junk")
        h2f = S.sb([128, D], F32, "h2f")
        h2b = S.sb([128, D], BF16, "h2b")
        h2T = S.sb([128, 8, 128], F32, "h2T")
        ssq = S.sb([128, 1], F32, "ssq")
        rstd = S.sb([128, 1], F32, "rstd")
        mx = S.sb([128, 1], F32, "mx")
        se = S.sb([128, 1], F32, "se")
        ex = S.sb([128, NE], F32, "ex")
        aff = S.sb([128, NE], F32, "aff")
        affT = S.sb([NE, 128], F32, "affT")
        pT = [S.ps([128, 8, 128], BF16, f"pT{i}") for i in range(2)]
        pacc = [S.ps([128, 512], F32, f"pacc{i}") for i in range(2)]
        pTf = [S.ps([128, 4, 128], F32, f"pTf{i}") for i in range(2)]
        plog = S.ps([128, NE], F32, "plog")
        paT = S.ps([NE, 128], F32, "paT")
        it = 0
        for b in range(NB):
            for seg in range(2):
                if seg == 1 and last:
                    continue
                r = b if seg == 0 else 2
                for tl, k in ((GATE1, 2), (G2, 4), (SH2, 3)):
                    P.op("sp", lambda e, tl=tl, r=r, k=k: e.dma_start(out=tl.t[:], in_=modrow(C, l, r, k).to_broadcast([128, D])), writes=RS(tl), dma=True)
                tiles = list(range(TL) if seg == 0 else range(TL, TT))
                if "s4_tiles" in DBG:
                    tiles = tiles[:DBG["s4_tiles"]]
                for t in tiles:
                    tok0 = t * 128
                    ya_, yb_, gt_, x_ = ya[it % 2], yb[it % 2], gt[it % 2], xt[it % 2]
                    it += 1
                    P.op("sp", lambda e, ya_=ya_, b=b, tok0=tok0: e.dma_start(out=ya_.t[:], in_=C.YA[b, tok0:tok0 + 128, :]), writes=RS(ya_), dma=True)
                    P.op("sp", lambda e, yb_=yb_, b=b, tok0=tok0: e.dma_start(out=yb_.t[:], in_=C.YB[b, tok0:tok0 + 128, :]), writes=RS(yb_), dma=True)
                    P.op("sp", lambda e, gt_=gt_, b=b, tok0=tok0: e.dma_start(out=gt_.t[:], in_=C.GT[b, tok0:tok0 + 128, :]), writes=RS(gt_), dma=True)
                    P.op("sp", lambda e, x_=x_, b=b, t=t: e.dma_start(out=x_.t[:], in_=xres(C, b, t)), writes=RS(x_), dma=True)
                    for j in range(8):
                        P.op("pe", lambda e, j=j, ya_=ya_: e.transpose(pT[0].t[:, j, :], ya_.t[:, j * 128:(j + 1) * 128], C.ident_b.t[:]), reads=RS(ya_, C.ident_b), writes=RS(pT[0]))
                    P.op("act", lambda e: e.copy(yaT.t[:], pT[0].t[:]), reads=RS(pT[0]), writes=RS(yaT))
                    for j in range(8):
                        P.op("pe", lambda e, j=j, yb_=yb_: e.transpose(pT[1].t[:, j, :], yb_.t[:, j * 128:(j + 1) * 128], C.ident_b.t[:]), reads=RS(yb_, C.ident_b), writes=RS(pT[1]))
                    P.op("dve", lambda e: e.tensor_copy(ybT.t[:], pT[1].t[:]), reads=RS(pT[1]), writes=RS(ybT))
                    for n in range(2):
                        cs = slice(n * 512, (n + 1) * 512)
                        for j in range(8):
                            P.op("pe", lambda e, j=j, cs=cs: e.matmul(pacc[0].t[:], yaT.t[:, j, :], wa.t[:, j, cs], start=(j == 0), stop=(j == 7)), reads=RS(yaT, wa), writes=RS(pacc[0]))
                        P.op("dve", lambda e, cs=cs, gt_=gt_: e.tensor_tensor(out=msum.t[:, cs], in0=pacc[0].t[:], in1=gt_.t[:, cs], op=ALU.mult), reads=RS(pacc[0], gt_), writes=RS(msum))
                        for j in range(8):
                            P.op("pe", lambda e, j=j, cs=cs: e.matmul(pacc[1].t[:], ybT.t[:, j, :], wb.t[:, j, cs], start=(j == 0), stop=(j == 7)), reads=RS(ybT, wb), writes=RS(pacc[1]))
                        tm = tmp[n]
                        P.op("dve", lambda e, n=n, tm=tm, gt_=gt_: e.tensor_tensor(out=tm.t[:], in0=pacc[1].t[:], in1=gt_.t[:, D + n * 512:D + (n + 1) * 512], op=ALU.mult), reads=RS(pacc[1], gt_), writes=RS(tm))
                        P.op("pool", lambda e, cs=cs, tm=tm: e.tensor_tensor(out=mb.t[:, cs], in0=msum.t[:, cs], in1=tm.t[:], op=ALU.add), reads=RS(msum, tm), writes=RS(mb))
                    for j in range(8):
                        P.op("pe", lambda e, j=j: e.transpose(pT[0].t[:, j, :], mb.t[:, j * 128:(j + 1) * 128], C.ident_b.t[:]), reads=RS(mb, C.ident_b), writes=RS(pT[0]))
                    P.op("act", lambda e: e.copy(mT.t[:], pT[0].t[:]), reads=RS(pT[0]), writes=RS(mT))
                    for n in range(2):
                        cs = slice(n * 512, (n + 1) * 512)
                        pa = pacc[n]
                        tm = tmp[n]
                        for j in range(8):
                            P.op("pe", lambda e, j=j, cs=cs, pa=pa: e.matmul(pa.t[:], mT.t[:, j, :], wo.t[:, j, cs], start=(j == 0), stop=(j == 7)), reads=RS(mT, wo), writes=RS(pa))
                        P.op("dve", lambda e, cs=cs, pa=pa, tm=tm: e.tensor_tensor(out=tm.t[:], in0=pa.t[:], in1=GATE1.t[:, cs], op=ALU.mult), reads=RS(pa, GATE1), writes=RS(tm))
                        P.op("pool", lambda e, cs=cs, tm=tm, x_=x_: e.tensor_tensor(out=xn.t[:, cs], in0=tm.t[:], in1=x_.t[:, cs], op=ALU.add), reads=RS(tm, x_), writes=RS(xn))
                    P.op("sp", lambda e, b=b, t=t: e.dma_start(out=xres(C, b, t), in_=xn.t[:]), reads=RS(xn), dma=True)
                    P.op("act", lambda e: e.activation(out=junk.t[:], in_=xn.t[:], func=ACT.Square, accum_out=ssq.t[:]), reads=RS(xn), writes=RS(junk, ssq))
                    P.op("act", lambda e: e.activation(out=rstd.t[:], in_=ssq.t[:], func=ACT.Sqrt, scale=1.0 / D, bias=EPS), reads=RS(ssq), writes=RS(rstd))
                    P.op("dve", lambda e: e.reciprocal(rstd.t[:], rstd.t[:]), reads=RS(rstd), writes=RS(rstd))
                    P.op("dve", lambda e: e.scalar_tensor_tensor(out=h2f.t[:], in0=xn.t[:], scalar=rstd.t[:, 0:1], in1=G2.t[:], op0=ALU.mult, op1=ALU.mult), reads=RS(xn, rstd, G2), writes=RS(h2f))
                    P.op("pool", lambda e: e.tensor_tensor(out=h2f.t[:], in0=h2f.t[:], in1=SH2.t[:], op=ALU.add), reads=RS(h2f, SH2), writes=RS(h2f))
                    P.op("act", lambda e: e.copy(h2b.t[:], h2f.t[:]), reads=RS(h2f), writes=RS(h2b))
                    if seg == 0:
                        P.op("sp", lambda e, b=b, tok0=tok0: e.dma_start(out=C.H2[b][tok0:tok0 + 128, :], in_=h2b.t[:]), reads=RS(h2b), dma=True)
                    else:
                        P.op("sp", lambda e, b=b, tok0=tok0: e.dma_start(out=C.H2C[b][tok0 - NL:tok0 - NL + 128, :], in_=h2b.t[:]), reads=RS(h2b), dma=True)
                    for half in range(2):
                        for jj in range(4):
                            j = half * 4 + jj
                            P.op("pe", lambda e, half=half, jj=jj, j=j: e.transpose(pTf[half].t[:, jj, :], h2f.t[:, j * 128:(j + 1) * 128], C.ident_f.t[:]), reads=RS(h2f, C.ident_f), writes=RS(pTf[half]))
                    P.op("act", lambda e: e.copy(h2T.t[:, 0:4, :], pTf[0].t[:]), reads=RS(pTf[0]), writes=RS(h2T))
                    P.op("dve", lambda e: e.tensor_copy(h2T.t[:, 4:8, :], pTf[1].t[:]), reads=RS(pTf[1]), writes=RS(h2T))
                    for j in range(8):
                        P.op("pe", lambda e, j=j: e.matmul(plog.t[:], h2T.t[:, j, :], wr.t[:, j, :], start=(j == 0), stop=(j == 7)), reads=RS(h2T, wr), writes=RS(plog))
                    P.op("dve", lambda e: e.tensor_reduce(out=mx.t[:], in_=plog.t[:], axis=AX.X, op=ALU.max), reads=RS(plog), writes=RS(mx))
                    P.op("dve", lambda e: e.tensor_scalar(mx.t[:], mx.t[:], -1.0, None, op0=ALU.mult), reads=RS(mx), writes=RS(mx))
                    P.op("act", lambda e: e.activation(out=ex.t[:], in_=plog.t[:], func=ACT.Exp, bias=mx.t[:, 0:1], accum_out=se.t[:]), reads=RS(plog, mx), writes=RS(ex, se))
                    P.op("dve", lambda e: e.reciprocal(se.t[:], se.t[:]), reads=RS(se), writes=RS(se))
                    P.op("dve", lambda e: e.tensor_scalar(aff.t[:], ex.t[:], se.t[:, 0:1], None, op0=ALU.mult), reads=RS(ex, se), writes=RS(aff))
                    P.op("pe", lambda e: e.transpose(paT.t[:], aff.t[:], C.ident_f.t[:]), reads=RS(aff, C.ident_f), writes=RS(paT))
                    P.op("act", lambda e: e.copy(affT.t[:], paT.t[:]), reads=RS(paT), writes=RS(affT))
                    P.op("sp", lambda e, b=b, tok0=tok0: e.dma_start(out=C.AFF[b, :, tok0:tok0 + 128], in_=affT.t[:]), reads=RS(affT), dma=True)


def alloc_route_tiles(C):
    g = C.es
    nc = C.nc
    pers = lambda shape, dt, nm: T(g.enter_context(nc.sbuf_tensor("c_" + nm, list(shape), dt)), nm)
    C.idxT = pers([128, 4, 32], U32, "idxT")
    C.valsT = pers([128, 4, 32], F32, "valsT")
    C.idxcT = pers([32, 32], U32, "idxcT")
    C.valscT = pers([32, 32], F32, "valscT")


def stage_topk(C, l):
    nc, P = C.nc, C.P
    last = (l == DEPTH - 1)
    with Stage(C, f"s5_{l}") as S:
        work = S.sb([32, NL], F32, "work")
        vals = S.sb([32, CAP_L], F32, "vals")
        idx = S.sb([32, CAP_L], U32, "idx")
        idxf = S.sb([32, CAP_L], F32, "idxf")
        pt = [S.ps([128, 32], F32, f"pt{i}") for i in range(2)]
        for b in range(NB):
            P.op("sp", lambda e, b=b: e.dma_start(out=work.t[b * 16:(b + 1) * 16, :], in_=C.AFF[b, :, 0:NL]), writes=RS(work), dma=True)
        for r in range(CAP_L // 8):
            rs_ = slice(r * 8, (r + 1) * 8)
            P.op("dve", lambda e, rs_=rs_: e.max(out=vals.t[:, rs_], in_=work.t[:]), reads=RS(work), writes=RS(vals))
            P.op("dve", lambda e, rs_=rs_: e.max_index(out=idx.t[:, rs_], in_max=vals.t[:, rs_], in_values=work.t[:]), reads=RS(work, vals), writes=RS(idx))
            P.op("dve", lambda e, rs_=rs_: e.match_replace(out=work.t[:], in_to_replace=vals.t[:, rs_], in_values=work.t[:], imm_value=-1.0), reads=RS(work, vals), writes=RS(work))
        P.op("dve", lambda e: e.tensor_copy(idxf.t[:], idx.t[:]), reads=RS(idx), writes=RS(idxf))
        k = 0
        for s in range(4):
            for src, dst in ((idxf, C.idxT), (vals, C.valsT)):
                p_ = pt[k % 2]
                k += 1
                P.op("pe", lambda e, p_=p_, src=src, s=s: e.transpose(p_.t[:], src.t[:, s * 128:(s + 1) * 128], C.ident_f.t[0:32, 0:32]), reads=RS(src, C.ident_f), writes=RS(p_))
                P.op("dve", lambda e, p_=p_, dst=dst, s=s: e.tensor_copy(dst.t[:, s, :], p_.t[:]), reads=RS(p_), writes=RS(dst))
        if not last:
            workc = S.sb([32, NCX], F32, "workc")
            valsc = S.sb([32, CAP_C], F32, "valsc")
            idxc = S.sb([32, CAP_C], U32, "idxc")
            idxcf = S.sb([32, CAP_C], F32, "idxcf")
            for b in range(NB):
                P.op("sp", lambda e, b=b: e.dma_start(out=workc.t[b * 16:(b + 1) * 16, :], in_=C.AFF[b, :, NL:NT]), writes=RS(workc), dma=True)
            for r in range(CAP_C // 8):
                rs_ = slice(r * 8, (r + 1) * 8)
                P.op("dve", lambda e, rs_=rs_: e.max(out=valsc.t[:, rs_], in_=workc.t[:]), reads=RS(workc), writes=RS(valsc))
                P.op("dve", lambda e, rs_=rs_: e.max_index(out=idxc.t[:, rs_], in_max=valsc.t[:, rs_], in_values=workc.t[:]), reads=RS(workc, valsc), writes=RS(idxc))
                P.op("dve", lambda e, rs_=rs_: e.match_replace(out=workc.t[:], in_to_replace=valsc.t[:, rs_], in_values=workc.t[:], imm_value=-1.0), reads=RS(workc, valsc), writes=RS(workc))
            P.op("dve", lambda e: e.tensor_copy(idxcf.t[:], idxc.t[:]), reads=RS(idxc), writes=RS(idxcf))
            for src, dst in ((idxcf, C.idxcT), (valsc, C.valscT)):
                p_ = pt[k % 2]
                k += 1
                P.op("pe", lambda e, p_=p_, src=src: e.transpose(p_.t[0:32, :], src.t[:], C.ident_f.t[0:32, 0:32]), reads=RS(src, C.ident_f), writes=RS(p_))
                P.op("dve", lambda e, p_=p_, dst=dst: e.tensor_copy(dst.t[:], p_.t[0:32, :]), reads=RS(p_), writes=RS(dst))
        if "dump_route" in DBG:
            P.op("sp", lambda e: e.dma_start(out=C.DIDX[l], in_=C.idxT.t[:].rearrange("p s c -> p (s c)")), reads=RS(C.idxT), dma=True)
            P.op("sp", lambda e: e.dma_start(out=C.DVAL[l], in_=C.valsT.t[:].rearrange("p s c -> p (s c)")), reads=RS(C.valsT), dma=True)


def stage_experts(C, l):
    nc, P, W = C.nc, C.P, C.W
    last = (l == DEPTH - 1)
    ncx = 0 if last else 2 * CAP_C
    NTOK = NB * CAP_L + ncx
    with Stage(C, f"s6_{l}") as S:
        xgT = S.sb([128, 8, NB * CAP_L + 2 * CAP_C], BF16, "xgT")
        hT = S.sb([128, FFC, NB * CAP_L + 2 * CAP_C], BF16, "hT")
        w2 = S.sb([128, FFC, D], BF16, "w2")
        wblk = [S.sb([128, 2, 8, 256], BF16, f"wblk{i}") for i in range(4)]
        xgs = [S.sb([128, D], BF16, f"xg{i}") for i in range(NB * 4 + NB)]
        ye = [S.sb([128, D], F32, f"ye{i}") for i in range(2)]
        stmp = [S.sb([128, 512], F32, f"stmp{i}") for i in range(2)]
        ytmp = [S.sb([128, 512], F32, f"ytmp{i}") for i in range(2)]
        G2g = [S.sb([128, D], F32, f"G2g{i}") for i in range(3)]
        pT = [S.ps([128, 8, 128], BF16, f"pT{i}") for i in range(2)]
        ps1 = [S.ps([128, 512], F32, f"ps1{i}") for i in range(2)]
        ps3 = [S.ps([128, 512], F32, f"ps3{i}") for i in range(2)]
        py = [S.ps([128, 512], F32, f"py{i}") for i in range(2)]
        for r in range(3):
            P.op("sp", lambda e, r=r: e.dma_start(out=G2g[r].t[:], in_=modrow(C, l, r, 5).to_broadcast([128, D])), writes=RS(G2g[r]), dma=True)
        xres_r = [Res(f"xres{b}") for b in range(NB)]
        xcres_r = [Res(f"xcres{b}") for b in range(NB)]
        ipT = iw = ips = iy = iye = 0
        experts = list(range(NE) if "s6_experts" not in DBG else range(DBG["s6_experts"]))
        n_g = NB * 4 + (0 if last else NB)

        def gather(ex):
            k = 0
            for b in range(NB):
                be = b * 16 + ex
                for s in range(4):
                    g_ = xgs[k]
                    k += 1
                    P.op("pool", lambda e, g_=g_, b=b, s=s, be=be: e.indirect_dma_start(
                        out=g_.t[:], out_offset=None, in_=C.H2[b], in_offset=bass.IndirectOffsetOnAxis(ap=C.idxT.t[:, s, be:be + 1], axis=0)),
                        reads=RS(C.idxT), writes=RS(g_), dma=True)
            if not last:
                for b in range(NB):
                    be = b * 16 + ex
                    g_ = xgs[k]
                    k += 1
                    P.op("pool", lambda e, g_=g_, b=b, be=be: e.indirect_dma_start(
                        out=g_.t[0:CAP_C, :], out_offset=None, in_=C.H2C[b], in_offset=bass.IndirectOffsetOnAxis(ap=C.idxcT.t[0:CAP_C, be:be + 1], axis=0)),
                        reads=RS(C.idxcT), writes=RS(g_), dma=True)

        def to_feature_major(ex):
            nonlocal ipT
            k = 0
            for b in range(NB):
                for s in range(4):
                    g_ = xgs[k]
                    k += 1
                    p_ = pT[ipT % 2]
                    ipT += 1
                    for j in range(8):
                        P.op("pe", lambda e, p_=p_, g_=g_, j=j: e.transpose(p_.t[:, j, :], g_.t[:, j * 128:(j + 1) * 128], C.ident_b.t[:]), reads=RS(g_, C.ident_b), writes=RS(p_))
                    c0 = b * CAP_L + s * 128
                    P.op("act", lambda e, p_=p_, c0=c0: e.copy(xgT.t[:, :, c0:c0 + 128], p_.t[:]), reads=RS(p_), writes=RS(xgT))
            if not last:
                for b in range(NB):
                    g_ = xgs[k]
                    k += 1
                    p_ = pT[ipT % 2]
                    ipT += 1
                    for j in range(8):
                        P.op("pe", lambda e, p_=p_, g_=g_, j=j: e.transpose(p_.t[:, j, 0:CAP_C], g_.t[0:CAP_C, j * 128:(j + 1) * 128], C.ident_b.t[0:CAP_C, 0:CAP_C]), reads=RS(g_, C.ident_b), writes=RS(p_))
                    c0 = NB * CAP_L + b * CAP_C
                    P.op("act", lambda e, p_=p_, c0=c0: e.copy(xgT.t[:, :, c0:c0 + CAP_C], p_.t[:, :, 0:CAP_C]), reads=RS(p_), writes=RS(xgT))

        gather(experts[0])
        to_feature_major(experts[0])
        for iex, ex in enumerate(experts):
            nxt = experts[iex + 1] if iex + 1 < len(experts) else None
            for f0, f1 in ((0, 6), (6, 12), (12, 17), (17, 22)):
                P.op("pool", lambda e, f0=f0, f1=f1, ex=ex: e.dma_start(out=w2.t[:, f0:f1, :], in_=W["w_e2"][l, ex, f0 * 128:f1 * 128, :].rearrange("(f p) d -> p f d", p=128)),
                     writes=RS(w2), dma=True)
            groups = [(0, 512), (512, 512)] + ([(1024, ncx)] if ncx else [])
            for fg in range(FFC // 2):
                wk = wblk[iw % 4]
                iw += 1
                if fg == 4 and nxt is not None:
                    gather(nxt)
                for k, nm in enumerate(("w_e1", "w_e3")):
                    P.op("pool", lambda e, wk=wk, k=k, nm=nm, fg=fg, ex=ex: e.dma_start(out=wk.t[:, k, :, :], in_=W[nm][l, ex, :, fg * 256:(fg + 1) * 256].rearrange("(j p) f -> p j f", p=128)),
                         writes=RS(wk), dma=True)
                for fc in range(2):
                    f = fg * 2 + fc
                    for (c0, n) in groups:
                        p1, p3 = ps1[ips % 2], ps3[ips % 2]
                        st_ = stmp[ips % 2]
                        ips += 1
                        for j in range(8):
                            P.op("pe", lambda e, p1=p1, wk=wk, j=j, fc=fc, c0=c0, n=n: e.matmul(p1.t[:, 0:n], wk.t[:, 0, j, fc * 128:(fc + 1) * 128], xgT.t[:, j, c0:c0 + n], start=(j == 0), stop=(j == 7)),
                                 reads=RS(wk, xgT), writes=RS(p1))
                        for j in range(8):
                            P.op("pe", lambda e, p3=p3, wk=wk, j=j, fc=fc, c0=c0, n=n: e.matmul(p3.t[:, 0:n], wk.t[:, 1, j, fc * 128:(fc + 1) * 128], xgT.t[:, j, c0:c0 + n], start=(j == 0), stop=(j == 7)),
                                 reads=RS(wk, xgT), writes=RS(p3))
                        P.op("act", lambda e, p1=p1, st_=st_, n=n: e.activation(out=st_.t[:, 0:n], in_=p1.t[:, 0:n], func=ACT.Silu), reads=RS(p1), writes=RS(st_))
                        P.op("dve", lambda e, p3=p3, st_=st_, f=f, c0=c0, n=n: e.tensor_tensor(out=hT.t[:, f, c0:c0 + n], in0=p3.t[:, 0:n], in1=st_.t[:, 0:n], op=ALU.mult), reads=RS(p3, st_), writes=RS(hT))
            if nxt is not None:
                to_feature_major(nxt)
            subt = [(b, s, b * CAP_L + s * 128, 128) for b in range(NB) for s in range(4)]
            if not last:
                subt += [(b, None, NB * CAP_L + b * CAP_C, CAP_C) for b in range(NB)]
            for (b, s, c0, m) in subt:
                be = b * 16 + ex
                ye_ = ye[iye % 2]
                iye += 1
                if s is not None:
                    gate_ap = C.valsT.t[:, s, be:be + 1]
                    gres = C.valsT
                    g2_ = G2g[b]
                else:
                    gate_ap = C.valscT.t[0:CAP_C, be:be + 1]
                    gres = C.valscT
                    g2_ = G2g[2]
                for n in range(2):
                    p_ = py[iy % 2]
                    yt_ = ytmp[iy % 2]
                    iy += 1
                    for f in range(FFC):
                        P.op("pe", lambda e, p_=p_, f=f, c0=c0, m=m, n=n: e.matmul(p_.t[0:m, :], hT.t[:, f, c0:c0 + m], w2.t[:, f, n * 512:(n + 1) * 512], start=(f == 0), stop=(f == FFC - 1)),
                             reads=RS(hT, w2), writes=RS(p_))
                    P.op("act", lambda e, p_=p_, yt_=yt_, m=m, gate_ap=gate_ap: e.activation(out=yt_.t[0:m, :], in_=p_.t[0:m, :], func=ACT.Copy, scale=gate_ap), reads=RS(p_, gres), writes=RS(yt_))
                    P.op("dve", lambda e, yt_=yt_, ye_=ye_, g2_=g2_, m=m, n=n: e.tensor_tensor(out=ye_.t[0:m, n * 512:(n + 1) * 512], in0=yt_.t[0:m, :], in1=g2_.t[0:m, n * 512:(n + 1) * 512], op=ALU.mult),
                         reads=RS(yt_, g2_), writes=RS(ye_))
                if s is not None:
                    P.op("pool", lambda e, ye_=ye_, b=b, s=s, be=be: e.indirect_dma_start(
                        out=C.out[b], out_offset=bass.IndirectOffsetOnAxis(ap=C.idxT.t[:, s, be:be + 1], axis=0), in_=ye_.t[:], in_offset=None, compute_op=ALU.add),
                        reads=RS(ye_, C.idxT), writes=[xres_r[b]], dma=True)
                else:
                    P.op("pool", lambda e, ye_=ye_, b=b, be=be: e.indirect_dma_start(
                        out=C.XC[b], out_offset=bass.IndirectOffsetOnAxis(ap=C.idxcT.t[0:CAP_C, be:be + 1], axis=0), in_=ye_.t[0:CAP_C, :], in_offset=None, compute_op=ALU.add),
                        reads=RS(ye_, C.idxcT), writes=[xcres_r[b]], dma=True)


def build_program(nc, n_layers=DEPTH, dbg_outs=(), no_experts=False, stages=None):
    C = setup(nc, dbg_outs=dbg_outs, n_layers=n_layers, no_experts=no_experts)
    if "dump_route" in DBG:
        C.DIDX = nc.dram_tensor("DIDX", [DEPTH, 128, 128], U32, kind="ExternalOutput").ap()
        C.DVAL = nc.dram_tensor("DVAL", [DEPTH, 128, 128], F32, kind="ExternalOutput").ap()
    stage_consts(C)
    alloc_route_tiles(C)
    fns = {"inproj": stage_inproj, "diff": stage_diff, "swa": stage_swa, "merge": stage_merge, "topk": stage_topk, "experts": stage_experts}
    order = ["inproj", "diff", "swa", "merge", "topk", "experts"]
    for l in range(n_layers):
        for nm in order:
            if stages is None or nm in stages:
                fns[nm](C, l)
    C.es.close()
    return C


def _rope_table():
    rows = NL // 64
    row = np.repeat(np.arange(rows), 64).astype(np.float32)
    col = np.tile(np.arange(64), rows).astype(np.float32)
    inv = (np.float32(10000.0) ** (-np.arange(0, 32, 2, dtype=np.float32) / np.float32(32))).astype(np.float32)
    ang = np.stack([row[:, None] * inv, col[:, None] * inv], axis=1).astype(np.float32)
    cs, sn = np.cos(ang).astype(np.float32), np.sin(ang).astype(np.float32)
    csx = np.repeat(cs[:, :, None, :], 2, axis=2).reshape(NL, 64)
    return np.ascontiguousarray(np.concatenate([csx, sn.reshape(NL, 32)], axis=1).astype(np.float32))


_NC_CACHE = {}


def kernel(**inputs):
    n_cores = 8
    if "nc" not in _NC_CACHE:
        nc = bass.Bass("TRN2", target_bir_lowering=False)
        build_program(nc)
        _NC_CACHE["nc"] = nc
    nc = _NC_CACHE["nc"]
    f32 = lambda a: np.ascontiguousarray(np.asarray(a, dtype=np.float32))
    x, c, ctx, c_ctx = f32(inputs["x"]), f32(inputs["c"]), f32(inputs["ctx"]), f32(inputs["c_ctx"])
    rope = _rope_table()
    shared = {}
    for n, s in WEIGHT_SPECS:
        shared[n] = f32(inputs[n]).reshape(s)
    in_maps = []
    for core in range(n_cores):
        b0 = core * NB
        cc = np.concatenate([c[b0:b0 + NB], c_ctx[None, :]], axis=0)
        m = {"x": x[b0:b0 + NB], "ctxin": ctx[b0:b0 + NB],
             "cT": np.ascontiguousarray(cc.reshape(3, 8, 128).transpose(2, 1, 0)), "rope": rope}
        m.update(shared)
        in_maps.append(m)
    res = run_bass_kernel_spmd(nc, in_maps, core_ids=list(range(n_cores)))
    out = np.empty((n_cores * NB, NL, D), np.float32)
    for core in range(n_cores):
        for b in range(NB):
            out[core * NB + b] = res.results[core][f"out{b}"]
    return out
```

```python
import numpy as np
import concourse.bass as bass
import concourse.mybir as mybir
from concourse.bass_utils import run_bass_kernel_spmd
from contextlib import ExitStack

F32 = mybir.dt.float32
BF16 = mybir.dt.bfloat16
U32 = mybir.dt.uint32
I32 = mybir.dt.int32
ALU = mybir.AluOpType
ACT = mybir.ActivationFunctionType
AX = mybir.AxisListType

D = 1024
NL = 4096
NCX = 256
NT = NL + NCX
TL = NL // 128
TC = NCX // 128
TT = TL + TC
DEPTH = 4
HD = 64
IN_W = 6656
NE = 16
FF = 2816
FFC = FF // 128
CAP_L = 512
CAP_C = 32
EPS = 1e-6
NB = 2
QKW = 3328

ENGS = ("pe", "act", "dve", "pool", "sp")
SAME_ENG_SYNC = True
DBG = {}


class Res:
    __slots__ = ("name", "w", "r_eng", "r_dma", "excl")

    def __init__(self, name="", excl=False):
        self.name = name
        self.excl = excl
        self.w = None
        self.r_eng = {}
        self.r_dma = []


class Prog:
    def __init__(self, nc, n_dma_slots=None):
        self.nc = nc
        self.eng_obj = {"pe": nc.tensor, "act": nc.scalar, "dve": nc.vector,
                        "pool": nc.gpsimd, "sp": nc.sync}
        self.sem = {e: nc.alloc_semaphore(name=f"sem_{e}") for e in ENGS}
        self.cnt = {e: 0 for e in ENGS}
        n_dma_slots = n_dma_slots or {"sp": 40, "pool": 32, "act": 16}
        self.slots = {q: [[nc.alloc_semaphore(name=f"dq_{q}{i}"), 0] for i in range(n)]
                      for q, n in n_dma_slots.items()}
        self.slot_i = {q: 0 for q in self.slots}
        self.seen = {e: {} for e in ENGS}
        self.ops = {e: [] for e in ENGS}
        self.semobj = {}
        for e in ENGS:
            self.semobj[("E", e)] = self.sem[e]
        self.n_ops = 0

    def _need(self, eng, tok, waits):
        if tok is None:
            return
        if tok[0] == "E":
            if tok[1] == eng and (eng == "pe" or not SAME_ENG_SYNC):
                return
            key = ("E", tok[1])
        else:
            key = ("D",) + tok[1]
        if self.seen[eng].get(key, 0) >= tok[2]:
            return
        self.seen[eng][key] = tok[2]
        waits[key] = max(waits.get(key, 0), tok[2])

    def op(self, eng, fn, reads=(), writes=(), dma=False):
        if DBG.get("max_ops") is not None and self.n_ops >= DBG["max_ops"]:
            return None
        waits = {}
        if any(r.excl for r in reads):
            writes = list(writes) + [r for r in reads if r.excl]
            reads = [r for r in reads if not r.excl]
        for r in reads:
            self._need(eng, r.w, waits)
        for w in writes:
            self._need(eng, w.w, waits)
            for e2, c in w.r_eng.items():
                self._need(eng, ("E", e2, c), waits)
            for t in w.r_dma:
                self._need(eng, t, waits)
        if dma:
            q = eng
            i = self.slot_i[q]
            self.slot_i[q] = (i + 1) % len(self.slots[q])
            slot = self.slots[q][i]
            if slot[1] > 0:
                self._need(eng, ("D", (q, i), slot[1]), waits)
            slot[1] += 16
            tok = ("D", (q, i), slot[1])
            inc = (slot[0], 16)
        else:
            self.cnt[eng] += 1
            tok = ("E", eng, self.cnt[eng])
            inc = (self.sem[eng], 1)
        wl = []
        for key, val in waits.items():
            s = self.sem[key[1]] if key[0] == "E" else self.slots[key[1]][key[2]][0]
            wl.append((s, val))
        self.ops[eng].append((wl, fn, inc))
        self.n_ops += 1
        for r in reads:
            if dma:
                r.r_dma.append(tok)
            else:
                r.r_eng[eng] = tok[2]
        for w in writes:
            w.w = tok
            w.r_eng = {}
            w.r_dma = []
        return tok

    def barrier(self):
        waits = {}
        for e in ENGS:
            if e != "sp" and self.cnt[e] > 0:
                self._need("sp", ("E", e, self.cnt[e]), waits)
        for q, sl in self.slots.items():
            for i, (s, v) in enumerate(sl):
                if v > 0:
                    self._need("sp", ("D", (q, i), v), waits)
        wl = []
        for key, val in waits.items():
            s = self.sem[key[1]] if key[0] == "E" else self.slots[key[1]][key[2]][0]
            wl.append((s, val))
        self.cnt["sp"] += 1
        tok = ("E", "sp", self.cnt["sp"])
        self.ops["sp"].append((wl, lambda e: e.nop(), (self.sem["sp"], 1)))
        for e in ENGS:
            if e == "sp":
                continue
            self.seen[e][("E", "sp")] = tok[2]
            self.cnt[e] += 1
            self.ops[e].append(([(self.sem["sp"], tok[2])], lambda e_: e_.nop(), (self.sem[e], 1)))
            for key in list(self.seen["sp"].keys()):
                self.seen[e][key] = max(self.seen[e].get(key, 0), self.seen["sp"][key])

    def emit(self):
        nc = self.nc
        ops = self.ops
        self.ops = {e: [] for e in ENGS}

        def run(e, lst):
            for wl, fn, inc in lst:
                for s, v in wl:
                    e.wait_ge(s, v)
                ins = fn(e)
                ins.then_inc(inc[0], inc[1])

        with nc.Block() as block:
            @block.tensor
            def _(e):
                run(e, ops["pe"])

            @block.scalar
            def _(e):
                run(e, ops["act"])

            @block.vector
            def _(e):
                run(e, ops["dve"])

            @block.gpsimd
            def _(e):
                run(e, ops["pool"])

            @block.sync
            def _(e):
                run(e, ops["sp"])


class T:
    __slots__ = ("t", "r")

    def __init__(self, t, name=""):
        self.t = t
        self.r = Res(name)


def RS(*tiles):
    return [x.r for x in tiles]


class Ctx:
    pass


class Stage:
    def __init__(self, C, name):
        self.C = C
        self.name = name
        self.es = ExitStack()
        self.n = 0

    def __enter__(self):
        self.es.__enter__()
        return self

    def __exit__(self, *a):
        if a[0] is None:
            self.C.P.barrier()
            self.C.P.emit()
        return self.es.__exit__(*a)

    def sb(self, shape, dt, name=None):
        self.n += 1
        nm = f"{self.name}_{name or 's'}{self.n}"
        return T(self.es.enter_context(self.C.nc.sbuf_tensor(nm, list(shape), dt)), nm)

    def ps(self, shape, dt, name=None):
        self.n += 1
        nm = f"{self.name}_{name or 'p'}{self.n}"
        t = T(self.es.enter_context(self.C.nc.psum_tensor(nm, list(shape), dt)), nm)
        t.r.excl = True
        return t


def bc_mid(ap2d, n):
    p, w = ap2d.shape
    return ap2d.rearrange("p (o w) -> p o w", o=1).to_broadcast([p, n, w])


def bc_last(ap2d, w):
    p, n = ap2d.shape
    return ap2d.rearrange("p (n o) -> p n o", o=1).to_broadcast([p, n, w])


WEIGHT_SPECS = [
    ("w_ada", [DEPTH, D, 6 * D]), ("b_ada", [DEPTH, 6 * D]), ("norm1_g", [DEPTH, D]),
    ("w_in", [DEPTH, D, IN_W]), ("b_gate", [DEPTH, 2 * D]), ("diff_q_g", [DEPTH, HD]),
    ("diff_k_g", [DEPTH, HD]), ("diff_lambda", [DEPTH, 4 * HD]), ("diff_subln_g", [DEPTH, 128]),
    ("swa_q_g", [DEPTH, HD]), ("swa_k_g", [DEPTH, HD]), ("swa_sink", [DEPTH, 16]),
    ("w_branch_a", [DEPTH, D, D]), ("w_branch_b", [DEPTH, D, D]), ("w_out", [DEPTH, D, D]),
    ("norm2_g", [DEPTH, D]), ("w_router", [DEPTH, D, NE]),
    ("w_e1", [DEPTH, NE, D, FF]), ("w_e3", [DEPTH, NE, D, FF]), ("w_e2", [DEPTH, NE, FF, D]),
]


def setup(nc, dbg_outs=(), n_layers=DEPTH, no_experts=False):
    C = Ctx()
    C.n_layers = n_layers
    C.nc = nc
    C.P = Prog(nc)
    di = lambda n, s, dt=F32: nc.dram_tensor(n, list(s), dt, kind="ExternalInput").ap()
    dn = lambda n, s, dt: nc.dram_tensor(n, list(s), dt, kind=("ExternalOutput" if n in dbg_outs else "Internal")).ap()
    C.x = di("x", [NB, NL, D])
    C.ctxin = di("ctxin", [NB, NCX, D])
    C.cT = di("cT", [128, 8, 3])
    C.rope = di("rope", [NL, 96])
    C.W = {}
    for n, s in WEIGHT_SPECS:
        s = [n_layers] + list(s[1:])
        if no_experts and n.startswith("w_e"):
            s = [1, 1, 2, 2]
        C.W[n] = di(n, s)
    C.out = [nc.dram_tensor(f"out{b}", [NL, D], F32, kind="ExternalOutput").ap() for b in range(NB)]
    C.XC = [dn(f"XC{b}", [NCX, D], F32) for b in range(NB)]
    C.QKT = dn("QKT", [NB, QKW, NT], BF16)
    C.VA = dn("VA", [NB, NT, D], BF16)
    C.VB = dn("VB", [NB, NT, 256], BF16)
    C.GT = dn("GT", [NB, NT, 2 * D], BF16)
    C.YA = dn("YA", [NB, NT, D], BF16)
    C.YB = dn("YB", [NB, NT, D], BF16)
    C.H2 = [dn(f"H2_{b}", [NL, D], BF16) for b in range(NB)]
    C.MODV = dn("MODV", [DEPTH, 3, 6 * D], F32)
    C.AFF = dn("AFF", [NB, NE, NT], F32)
    C.H2C = [dn(f"H2C_{b}", [NCX, D], BF16) for b in range(NB)]
    C.es = ExitStack()
    return C


def xres(C, b, t):
    if t < TL:
        return C.out[b][t * 128:(t + 1) * 128, :]
    return C.XC[b][(t - TL) * 128:(t - TL + 1) * 128, :]


def lam_init(l):
    import math
    return 0.8 - 0.6 * math.exp(-0.3 * l)


def stage_consts(C):
    nc, P = C.nc, C.P
    g = C.es
    pers = lambda shape, dt, nm: T(g.enter_context(nc.sbuf_tensor("c_" + nm, list(shape), dt)), nm)
    C.ident_b = pers([128, 128], BF16, "identb")
    C.ident_f = pers([128, 128], F32, "identf")
    C.gain = pers([128, DEPTH, 4, HD], F32, "gain")
    C.subln = pers([128, DEPTH, 128], F32, "subln")
    C.esink = pers([128, DEPTH, 16], F32, "esink")
    C.nlam = pers([128, DEPTH], F32, "nlam")
    C.mprev = pers([128, 4, 128], BF16, "mprev")
    C.mnext = pers([128, 4, 128], BF16, "mnext")
    W = C.W
    with Stage(C, "cst") as S:
        for b in range(NB):
            for k in range(8):
                P.op("sp", lambda e, b=b, k=k: e.dma_start(out=C.out[b][k * 512:(k + 1) * 512, :], in_=C.x[b, k * 512:(k + 1) * 512, :]), dma=True)
            P.op("sp", lambda e, b=b: e.dma_start(out=C.XC[b], in_=C.ctxin[b]), dma=True)
        P.op("pool", lambda e: e.memset(C.ident_f.t[:], 0.0), writes=RS(C.ident_f))
        P.op("pool", lambda e: e.affine_select(out=C.ident_f.t[:], in_=C.ident_f.t[:], pattern=[[-1, 128]],
                                               compare_op=ALU.not_equal, fill=1.0, base=0, channel_multiplier=1),
             reads=RS(C.ident_f), writes=RS(C.ident_f))
        P.op("dve", lambda e: e.tensor_copy(C.ident_b.t[:], C.ident_f.t[:]), reads=RS(C.ident_f), writes=RS(C.ident_b))
        mf = S.sb([128, 4, 128], F32, "mf")
        P.op("pool", lambda e: e.memset(mf.t[:], 1.0), writes=RS(mf))
        P.op("pool", lambda e: e.affine_select(out=mf.t[:], in_=mf.t[:], pattern=[[0, 4], [-1, 128]],
                                               compare_op=ALU.is_ge, fill=0.0, base=0, channel_multiplier=1),
             reads=RS(mf), writes=RS(mf))
        P.op("dve", lambda e: e.tensor_copy(C.mprev.t[:], mf.t[:]), reads=RS(mf), writes=RS(C.mprev))
        mf2 = S.sb([128, 4, 128], F32, "mf2")
        P.op("pool", lambda e: e.memset(mf2.t[:], 1.0), writes=RS(mf2))
        P.op("pool", lambda e: e.affine_select(out=mf2.t[:], in_=mf2.t[:], pattern=[[0, 4], [1, 128]],
                                               compare_op=ALU.is_ge, fill=0.0, base=0, channel_multiplier=-1),
             reads=RS(mf2), writes=RS(mf2))
        P.op("dve", lambda e: e.tensor_copy(C.mnext.t[:], mf2.t[:]), reads=RS(mf2), writes=RS(C.mnext))
        for k, nm in enumerate(["diff_q_g", "diff_k_g", "swa_q_g", "swa_k_g"]):
            for l in range(C.n_layers):
                P.op("sp", lambda e, k=k, nm=nm, l=l: e.dma_start(out=C.gain.t[:, l, k, :], in_=W[nm][l:l + 1, :].to_broadcast([128, HD])),
                     writes=RS(C.gain), dma=True)
        for l in range(C.n_layers):
            P.op("sp", lambda e, l=l: e.dma_start(out=C.subln.t[:, l, :], in_=W["diff_subln_g"][l:l + 1, :].to_broadcast([128, 128])),
                 writes=RS(C.subln), dma=True)
        for l in range(C.n_layers):
            P.op("dve", lambda e, l=l: e.tensor_scalar(C.subln.t[:, l, :], C.subln.t[:, l, :], 1.0 - lam_init(l), None, op0=ALU.mult),
                 reads=RS(C.subln), writes=RS(C.subln))
        for l in range(C.n_layers):
            P.op("sp", lambda e, l=l: e.dma_start(out=C.esink.t[:, l, :], in_=W["swa_sink"][l:l + 1, :].to_broadcast([128, 16])),
                 writes=RS(C.esink), dma=True)
        P.op("act", lambda e: e.activation(out=C.esink.t[:], in_=C.esink.t[:], func=ACT.Exp), reads=RS(C.esink), writes=RS(C.esink))
        dl = S.sb([128, DEPTH, 4, HD], F32, "dl")
        for l in range(C.n_layers):
            P.op("sp", lambda e, l=l: e.dma_start(out=dl.t[:, l].rearrange("p a d -> p (a d)"), in_=W["diff_lambda"][l:l + 1, :].to_broadcast([128, 4 * HD])),
                 writes=RS(dl), dma=True)
        pr = S.sb([128, DEPTH, 2, HD], F32, "pr")
        P.op("dve", lambda e: e.tensor_tensor(out=pr.t[:], in0=dl.t[:, :, 0:4:2, :], in1=dl.t[:, :, 1:4:2, :], op=ALU.mult), reads=RS(dl), writes=RS(pr))
        sm = S.sb([128, DEPTH, 2], F32, "sm")
        P.op("dve", lambda e: e.tensor_reduce(out=sm.t[:], in_=pr.t[:], axis=AX.X, op=ALU.add), reads=RS(pr), writes=RS(sm))
        P.op("act", lambda e: e.activation(out=sm.t[:], in_=sm.t[:], func=ACT.Exp), reads=RS(sm), writes=RS(sm))
        P.op("dve", lambda e: e.tensor_tensor(out=C.nlam.t[:], in0=sm.t[:, :, 1], in1=sm.t[:, :, 0], op=ALU.subtract), reads=RS(sm), writes=RS(C.nlam))
        for l in range(C.n_layers):
            P.op("dve", lambda e, l=l: e.tensor_scalar(C.nlam.t[:, l:l + 1], C.nlam.t[:, l:l + 1], -lam_init(l), None, op0=ALU.add),
                 reads=RS(C.nlam), writes=RS(C.nlam))
        sc = S.sb([128, 8, 3], F32, "sc")
        P.op("sp", lambda e: e.dma_start(out=sc.t[:], in_=C.cT), writes=RS(sc), dma=True)
        P.op("act", lambda e: e.activation(out=sc.t[:], in_=sc.t[:], func=ACT.Silu), reads=RS(sc), writes=RS(sc))
        wa = [S.sb([128, 3072], F32, f"wa{i}") for i in range(2)]
        pm = [S.ps([3, 512], F32, f"pm{i}") for i in range(6)]
        msb = S.sb([3, 6 * D], F32, "msb")
        bsb = S.sb([3, 6 * D], F32, "bsb")
        gsb = S.sb([3, 2, D], F32, "gsb")
        it = 0
        for l in range(C.n_layers):
            P.op("sp", lambda e, l=l: e.dma_start(out=bsb.t[:], in_=W["b_ada"][l:l + 1, :].to_broadcast([3, 6 * D])), writes=RS(bsb), dma=True)
            P.op("sp", lambda e, l=l: e.dma_start(out=gsb.t[:, 0, :], in_=W["norm1_g"][l:l + 1, :].to_broadcast([3, D])), writes=RS(gsb), dma=True)
            P.op("sp", lambda e, l=l: e.dma_start(out=gsb.t[:, 1, :], in_=W["norm2_g"][l:l + 1, :].to_broadcast([3, D])), writes=RS(gsb), dma=True)
            for half in range(2):
                for j in range(8):
                    w = wa[it % 2]
                    it += 1
                    P.op("sp", lambda e, w=w, l=l, j=j, half=half: e.dma_start(out=w.t[:], in_=W["w_ada"][l, j * 128:(j + 1) * 128, half * 3072:(half + 1) * 3072]),
                         writes=RS(w), dma=True)
                    for n in range(6):
                        P.op("pe", lambda e, w=w, j=j, n=n: e.matmul(pm[n].t[:], sc.t[:, j, :], w.t[:, n * 512:(n + 1) * 512], start=(j == 0), stop=(j == 7)),
                             reads=RS(sc, w), writes=RS(pm[n]))
                for n in range(6):
                    c0 = half * 3072 + n * 512
                    P.op("dve", lambda e, n=n, c0=c0: e.tensor_tensor(out=msb.t[:, c0:c0 + 512], in0=pm[n].t[:], in1=bsb.t[:, c0:c0 + 512], op=ALU.add),
                         reads=RS(pm[n], bsb), writes=RS(msb))
            for k, c0 in enumerate((D, 4 * D)):
                P.op("dve", lambda e, k=k, c0=c0: e.scalar_tensor_tensor(out=msb.t[:, c0:c0 + D], in0=msb.t[:, c0:c0 + D], scalar=1.0, in1=gsb.t[:, k, :], op0=ALU.add, op1=ALU.mult),
                     reads=RS(msb, gsb), writes=RS(msb))
            P.op("sp", lambda e, l=l: e.dma_start(out=C.MODV[l], in_=msb.t[:]), reads=RS(msb), dma=True)


def modrow(C, l, r, k):
    return C.MODV[l, r:r + 1, k * D:(k + 1) * D]


def stage_inproj(C, l):
    nc, P, W = C.nc, C.P, C.W
    with Stage(C, f"s1_{l}") as S:
        win = S.sb([128, 8, IN_W], BF16, "win")
        winr = [Res(f"win{j}") for j in range(8)]
        for j in range(0 if not DBG.get("no_win") else 8, 8):
            for h in range(2):
                P.op("pool", lambda e, j=j, h=h: e.dma_start(out=win.t[:, j, h * 3328:(h + 1) * 3328], in_=W["w_in"][l, j * 128:(j + 1) * 128, h * 3328:(h + 1) * 3328]),
                     writes=[winr[j]], dma=True)
        BG = S.sb([128, 2 * D], BF16, "BG")
        P.op("pool", lambda e: e.dma_start(out=BG.t[:], in_=W["b_gate"][l:l + 1, :].to_broadcast([128, 2 * D])), writes=RS(BG), dma=True)
        G1 = S.sb([128, D], F32, "G1")
        SH1 = S.sb([128, D], F32, "SH1")
        xt = [S.sb([128, D], F32, f"xt{i}") for i in range(2)]
        junk = S.sb([128, QKW], F32, "junk")
        ssq = S.sb([128, 1], F32, "ssq")
        rstd = S.sb([128, 1], F32, "rstd")
        hbs = [S.sb([128, D], BF16, f"hb{i}") for i in range(2)]
        hTs = [S.sb([128, 8, 128], BF16, f"hT{i}") for i in range(2)]
        QF = S.sb([128, QKW], F32, "QF")
        QB = S.sb([128, QKW], BF16, "QB")
        QT = S.sb([128, 26, 128], BF16, "QT")
        vout = S.sb([128, 1280], BF16, "vout")
        gout = S.sb([128, 2 * D], BF16, "gout")
        gtmp = [S.sb([128, 512], F32, f"gtmp{i}") for i in range(2)]
        ss = S.sb([128, 52], F32, "ss")
        rs = S.sb([128, 52], F32, "rs")
        rp = S.sb([128, 96], F32, "rp")
        pT = S.ps([128, 8, 128], BF16, "pT")
        pacc = [S.ps([128, 512], F32, f"pacc{i}") for i in range(3)]
        pQT = [S.ps([128, 8, 128], BF16, f"pQT{i}") for i in range(2)]
        gain = C.gain
        sqj = S.sb([128, D], F32, "sqj")
        rps = [S.sb([128, 96], F32, f"rp{i}") for i in range(2)]
        state = {"ipa": 0, "seg": None}
        tl_all = []
        for b in range(NB):
            for seg in range(2):
                tiles = list(range(TL) if seg == 0 else range(TL, TT))
                if "s1_tiles" in DBG:
                    tiles = tiles[:DBG["s1_tiles"]]
                tl_all += [(b, seg, t) for t in tiles]

        def pre(i):
            b, seg, t = tl_all[i]
            x, hb, hT, rp = xt[i % 2], hbs[i % 2], hTs[i % 2], rps[i % 2]
            if state["seg"] != (b, seg):
                state["seg"] = (b, seg)
                r = b if seg == 0 else 2
                P.op("sp", lambda e, r=r: e.dma_start(out=G1.t[:], in_=modrow(C, l, r, 1).to_broadcast([128, D])), writes=RS(G1), dma=True)
                P.op("sp", lambda e, r=r: e.dma_start(out=SH1.t[:], in_=modrow(C, l, r, 0).to_broadcast([128, D])), writes=RS(SH1), dma=True)
            P.op("sp", lambda e, x=x, b=b, t=t: e.dma_start(out=x.t[:], in_=xres(C, b, t)), writes=RS(x), dma=True)
            if seg == 0:
                P.op("sp", lambda e, t=t, rp=rp: e.dma_start(out=rp.t[:], in_=C.rope[t * 128:(t + 1) * 128, :]), writes=RS(rp), dma=True)
            P.op("act", lambda e, x=x: e.activation(out=sqj.t[:], in_=x.t[:], func=ACT.Square, accum_out=ssq.t[:]), reads=RS(x), writes=RS(sqj, ssq))
            P.op("act", lambda e: e.activation(out=rstd.t[:], in_=ssq.t[:], func=ACT.Sqrt, scale=1.0 / D, bias=EPS), reads=RS(ssq), writes=RS(rstd))
            P.op("dve", lambda e: e.reciprocal(rstd.t[:], rstd.t[:]), reads=RS(rstd), writes=RS(rstd))
            P.op("dve", lambda e, x=x: e.scalar_tensor_tensor(out=x.t[:], in0=x.t[:], scalar=rstd.t[:, 0:1], in1=G1.t[:], op0=ALU.mult, op1=ALU.mult),
                 reads=RS(x, rstd, G1), writes=RS(x))
            P.op("dve", lambda e, x=x, hb=hb: e.tensor_tensor(out=hb.t[:], in0=x.t[:], in1=SH1.t[:], op=ALU.add), reads=RS(x, SH1), writes=RS(hb))
            for j in range(8):
                P.op("pe", lambda e, j=j, hb=hb: e.transpose(pT.t[:, j, :], hb.t[:, j * 128:(j + 1) * 128], C.ident_b.t[:]), reads=RS(hb, C.ident_b), writes=RS(pT))
            P.op("act", lambda e, hT=hT: e.copy(hT.t[:], pT.t[:]), reads=RS(pT), writes=RS(hT))

        def mm(i, chunks):
            hT = hTs[i % 2]
            for n in chunks:
                pa = pacc[state["ipa"] % 3]
                state["ipa"] += 1
                for j in range(8):
                    P.op("pe", lambda e, pa=pa, j=j, n=n, hT=hT: e.matmul(pa.t[:], hT.t[:, j, :], win.t[:, j, n * 512:(n + 1) * 512], start=(j == 0), stop=(j == 7)),
                         reads=[hT.r, winr[j]], writes=RS(pa))
                if n in (4, 5):
                    P.op("act", lambda e, pa=pa, n=n: e.copy(vout.t[:, (n - 4) * 512:(n - 3) * 512], pa.t[:]), reads=RS(pa), writes=RS(vout))
                elif n >= 9:
                    gt_ = gtmp[n % 2]
                    c0 = (n - 9) * 512
                    P.op("dve", lambda e, pa=pa, gt_=gt_, c0=c0: e.tensor_tensor(out=gt_.t[:], in0=pa.t[:], in1=BG.t[:, c0:c0 + 512], op=ALU.add),
                         reads=RS(pa, BG), writes=RS(gt_))
                    P.op("act", lambda e, gt_=gt_, c0=c0: e.activation(out=gout.t[:, c0:c0 + 512], in_=gt_.t[:], func=ACT.Sigmoid), reads=RS(gt_), writes=RS(gout))
                elif n <= 3:
                    P.op("act", lambda e, pa=pa, n=n: e.copy(QF.t[:, n * 512:(n + 1) * 512], pa.t[:]), reads=RS(pa), writes=RS(QF))
                elif n in (6, 7):
                    P.op("dve", lambda e, pa=pa, n=n: e.tensor_copy(QF.t[:, 2048 + (n - 6) * 512:2048 + (n - 5) * 512], pa.t[:]), reads=RS(pa), writes=RS(QF))
                else:
                    P.op("act", lambda e, pa=pa: e.copy(QF.t[:, 3072:3328], pa.t[:, 0:256]), reads=RS(pa), writes=RS(QF))
                    P.op("act", lambda e, pa=pa: e.copy(vout.t[:, 1024:1280], pa.t[:, 256:512]), reads=RS(pa), writes=RS(vout))

        def store_vg(i):
            b, seg, t = tl_all[i]
            tok0 = t * 128
            P.op("sp", lambda e, b=b, tok0=tok0: e.dma_start(out=C.VA[b, tok0:tok0 + 128, :], in_=vout.t[:, 0:D]), reads=RS(vout), dma=True)
            P.op("sp", lambda e, b=b, tok0=tok0: e.dma_start(out=C.VB[b, tok0:tok0 + 128, :], in_=vout.t[:, D:1280]), reads=RS(vout), dma=True)
            P.op("sp", lambda e, b=b, tok0=tok0: e.dma_start(out=C.GT[b, tok0:tok0 + 128, :], in_=gout.t[:]), reads=RS(gout), dma=True)

        def post_elem(i):
            b, seg, t = tl_all[i]
            rp = rps[i % 2]
            P.op("act", lambda e: e.activation(out=junk.t[:], in_=QF.t[:], func=ACT.Square), reads=RS(QF), writes=RS(junk))
            P.op("dve", lambda e: e.tensor_reduce(out=ss.t[:], in_=junk.t[:].rearrange("p (g d) -> p g d", d=HD), axis=AX.X, op=ALU.add),
                 reads=RS(junk), writes=RS(ss))
            P.op("act", lambda e: e.activation(out=rs.t[:], in_=ss.t[:], func=ACT.Sqrt, scale=1.0 / HD, bias=EPS), reads=RS(ss), writes=RS(rs))
            P.op("dve", lambda e: e.reciprocal(rs.t[:], rs.t[:]), reads=RS(rs), writes=RS(rs))
            QF3 = QF.t[:].rearrange("p (g d) -> p g d", d=HD)
            P.op("pool", lambda e, QF3=QF3: e.tensor_tensor(out=QF3, in0=QF3, in1=bc_last(rs.t[:], HD), op=ALU.mult), reads=RS(QF, rs), writes=RS(QF))
            for k, (g0, ng) in enumerate(((0, 16), (16, 16), (32, 16), (48, 4))):
                P.op("pool", lambda e, QF3=QF3, k=k, g0=g0, ng=ng: e.tensor_tensor(out=QF3[:, g0:g0 + ng, :], in0=QF3[:, g0:g0 + ng, :], in1=bc_mid(gain.t[:, l, k, :], ng), op=ALU.mult),
                     reads=RS(QF, gain), writes=RS(QF))
            if seg == 0:
                QF5 = QF.t[:].rearrange("p (g a r i) -> p g a r i", a=2, r=2, i=16)
                J5 = junk.t[:].rearrange("p (g a r i) -> p g a r i", a=2, r=2, i=16)
                QB5 = QB.t[:].rearrange("p (g a r i) -> p g a r i", a=2, r=2, i=16)
                SN = rp.t[:, 64:96].rearrange("p (o a i) -> p o a i", o=1, a=2).to_broadcast([128, 52, 2, 16])
                P.op("pool", lambda e, QF5=QF5, J5=J5, SN=SN: e.tensor_tensor(out=J5[:, :, :, 0, :], in0=QF5[:, :, :, 1, :], in1=SN, op=ALU.mult),
                     reads=RS(QF, rp), writes=RS(junk))
                P.op("pool", lambda e, QF5=QF5, J5=J5, SN=SN: e.tensor_tensor(out=J5[:, :, :, 1, :], in0=QF5[:, :, :, 0, :], in1=SN, op=ALU.mult),
                     reads=RS(QF, rp), writes=RS(junk))
                P.op("dve", lambda e, QF3=QF3, rp=rp: e.tensor_tensor(out=QF3, in0=QF3, in1=bc_mid(rp.t[:, 0:64], 52), op=ALU.mult), reads=RS(QF, rp), writes=RS(QF))
                P.op("dve", lambda e, QF5=QF5, J5=J5, QB5=QB5: e.tensor_tensor(out=QB5[:, :, :, 0, :], in0=QF5[:, :, :, 0, :], in1=J5[:, :, :, 0, :], op=ALU.subtract),
                     reads=RS(QF, junk), writes=RS(QB))
                P.op("dve", lambda e, QF5=QF5, J5=J5, QB5=QB5: e.tensor_tensor(out=QB5[:, :, :, 1, :], in0=QF5[:, :, :, 1, :], in1=J5[:, :, :, 1, :], op=ALU.add),
                     reads=RS(QF, junk), writes=RS(QB))
            else:
                P.op("dve", lambda e: e.tensor_copy(QB.t[:], QF.t[:]), reads=RS(QF), writes=RS(QB))

        def post_pe(i):
            b, seg, t = tl_all[i]
            for jb in range(4):
                pq = pQT[jb % 2]
                nj = 8 if jb < 3 else 2
                for jj in range(nj):
                    j = jb * 8 + jj
                    P.op("pe", lambda e, pq=pq, jj=jj, j=j: e.transpose(pq.t[:, jj, :], QB.t[:, j * 128:(j + 1) * 128], C.ident_b.t[:]), reads=RS(QB, C.ident_b), writes=RS(pq))
                if jb % 2 == 0:
                    P.op("act", lambda e, pq=pq, jb=jb, nj=nj: e.copy(QT.t[:, jb * 8:jb * 8 + nj, :], pq.t[:, 0:nj, :]), reads=RS(pq), writes=RS(QT))
                else:
                    P.op("dve", lambda e, pq=pq, jb=jb, nj=nj: e.tensor_copy(QT.t[:, jb * 8:jb * 8 + nj, :], pq.t[:, 0:nj, :]), reads=RS(pq), writes=RS(QT))
            tok0 = t * 128
            for jq in range(0, 26, 2):
                P.op("sp", lambda e, b=b, tok0=tok0, jq=jq: e.dma_start(out=C.QKT[b, jq * 128:(jq + 2) * 128, tok0:tok0 + 128].rearrange("(j p) n -> p j n", p=128), in_=QT.t[:, jq:jq + 2, :]), reads=RS(QT), dma=True)

        CH_A = (4, 5, 9, 10, 11, 12)
        CH_B = (0, 1, 2, 3, 6, 7, 8)
        n_t = len(tl_all)
        pre(0)
        mm(0, CH_A)
        mm(0, CH_B)
        store_vg(0)
        for i in range(n_t):
            post_elem(i)
            if i + 1 < n_t:
                pre(i + 1)
                mm(i + 1, CH_A)
            post_pe(i)
            if i + 1 < n_t:
                mm(i + 1, CH_B)
                store_vg(i + 1)


def stage_diff(C, l):
    nc, P = C.nc, C.P
    last = (l == DEPTH - 1)
    with Stage(C, f"s2_{l}") as S:
        KT = [S.sb([128, NT], BF16, f"KT{i}") for i in range(2)]
        QT = [[S.sb([128, NT], BF16, f"QZ{i}_{c}") for c in range(2)] for i in range(2)]
        V1 = [S.sb([128, TT, 129], BF16, f"V1{i}") for i in range(2)]
        PT = [S.sb([128, 512], BF16, f"PT{i}") for i in range(3)]
        o0 = S.sb([128, 128], F32, "o0")
        av = S.sb([128, 128], F32, "av")
        junk = S.sb([128, 128], F32, "junk")
        rec = S.sb([128, 2], F32, "rec")
        s1 = S.sb([128, 1], F32, "s1")
        ssq = S.sb([128, 1], F32, "ssq")
        rstd = S.sb([128, 1], F32, "rstd")
        yo = [S.sb([128, 4, 128], BF16, f"yo{i}") for i in range(2)]
        NPS = 4
        LOOK = 3
        ps = [S.ps([128, 512], F32, f"ps{i}") for i in range(NPS)]
        accb = [S.ps([128, 3, 129], F32, f"acc_{i}") for i in range(3)]
        accS = [[S.sb([128, 3, 129], F32, f"accS{k}_{i}") for i in range(3)] for k in range(2)]
        for v in V1:
            P.op("pool", lambda e, v=v: e.memset(v.t[:, :, 128:129], 1.0), writes=RS(v))
        for qz in QT:
            P.op("pool", lambda e, qz=qz: e.memset(qz[0].t[64:128, :], 0.0), writes=RS(qz[0]))
            P.op("dve", lambda e, qz=qz: e.memset(qz[1].t[0:64, :], 0.0), writes=RS(qz[1]))

        def acc_ap(accb, c, qs):
            a = c * 4 + qs
            return accb[a // 3], accb[a // 3].t[:, a % 3, :]

        ih = 0
        iyo = 0
        ich = 0
        for b in range(NB):
            for h in range(8):
                kt_, qt_, v1_ = KT[ih % 2], QT[ih % 2], V1[ih % 2]
                ih += 1
                P.op("sp", lambda e, kt_=kt_, b=b, h=h: e.dma_start(out=kt_.t[:], in_=C.QKT[b, 1024 + h * 128:1024 + (h + 1) * 128, :]), writes=RS(kt_), dma=True)
                P.op("sp", lambda e, qt_=qt_, b=b, h=h: e.dma_start(out=qt_[0].t[0:64, :], in_=C.QKT[b, h * 128:h * 128 + 64, :]), writes=RS(qt_[0]), dma=True)
                P.op("sp", lambda e, qt_=qt_, b=b, h=h: e.dma_start(out=qt_[1].t[64:128, :], in_=C.QKT[b, h * 128 + 64:(h + 1) * 128, :]), writes=RS(qt_[1]), dma=True)
                vsrc = C.VA[b].rearrange("(kt p) c -> p kt c", p=128)
                for k0 in range(0, TT, 6):
                    k1 = min(TT, k0 + 6)
                    P.op("sp", lambda e, v1_=v1_, k0=k0, k1=k1, h=h, vsrc=vsrc: e.dma_start(out=v1_.t[:, k0:k1, 0:128], in_=vsrc[:, k0:k1, h * 128:(h + 1) * 128]),
                         writes=RS(v1_), dma=True)
                chunks = [(qc * 512, 4, list(range(TT))) for qc in range(8)]
                if not last:
                    chunks.append((NL, 2, [TL, TL + 1]))
                if "s2_chunks" in DBG:
                    chunks = chunks[:DBG["s2_chunks"]] + chunks[8:]
                for (q0, nqs, kts) in chunks:
                    nq = nqs * 128
                    accs_ = accS[ich % 2]
                    ich += 1
                    started = set()
                    units = [(c, ki, kt) for c in range(2) for ki, kt in enumerate(kts)]

                    def qk(i, units=units, kt_=kt_, qt_=qt_, q0=q0, nq=nq):
                        c, ki, kt = units[i]
                        p_ = ps[i % NPS]
                        P.op("pe", lambda e, p_=p_, c=c, kt=kt: e.matmul(
                            p_.t[:, 0:nq], kt_.t[:, kt * 128:(kt + 1) * 128], qt_[c].t[:, q0:q0 + nq], start=True, stop=True),
                            reads=RS(kt_, qt_[c]), writes=RS(p_))

                    for i0 in range(min(LOOK, len(units))):
                        qk(i0)
                    for i, (c, ki, kt) in enumerate(units):
                        p_ = ps[i % NPS]
                        pt_ = PT[i % 3]
                        if i + LOOK < len(units):
                            qk(i + LOOK)
                        P.op("act", lambda e, p_=p_, pt_=pt_, nq=nq: e.activation(out=pt_.t[:, 0:nq], in_=p_.t[:, 0:nq], func=ACT.Exp, scale=0.125),
                             reads=RS(p_), writes=RS(pt_))
                        for qs in range(nqs):
                            at, aap = acc_ap(accb, c, qs)
                            st = (ki == 0) and (id(at) not in started)
                            started.add(id(at))
                            P.op("pe", lambda e, aap=aap, pt_=pt_, v1_=v1_, qs=qs, kt=kt, st=st: e.matmul(
                                aap, pt_.t[:, qs * 128:(qs + 1) * 128], v1_.t[:, kt, :], start=st, stop=False, skip_group_check=True),
                                reads=RS(pt_, v1_), writes=RS(at))
                    for k3 in range(3):
                        P.op("dve", lambda e, k3=k3, accs_=accs_: e.tensor_copy(accs_[k3].t[:], accb[k3].t[:]), reads=RS(accb[k3]), writes=RS(accs_[k3]))
                    y_ = yo[iyo % 2]
                    iyo += 1
                    for qs in range(nqs):
                        a0t, a0 = acc_ap(accs_, 0, qs)
                        a1t, a1 = acc_ap(accs_, 1, qs)
                        P.op("dve", lambda e, a0=a0: e.reciprocal(rec.t[:, 0:1], a0[:, 128:129]), reads=RS(a0t), writes=RS(rec))
                        P.op("dve", lambda e, a1=a1: e.reciprocal(rec.t[:, 1:2], a1[:, 128:129]), reads=RS(a1t), writes=RS(rec))
                        P.op("dve", lambda e: e.tensor_tensor(out=s1.t[:], in0=rec.t[:, 1:2], in1=C.nlam.t[:, l:l + 1], op=ALU.mult), reads=RS(rec, C.nlam), writes=RS(s1))
                        P.op("dve", lambda e, a0=a0: e.tensor_scalar(o0.t[:], a0[:, 0:128], rec.t[:, 0:1], None, op0=ALU.mult), reads=RS(a0t, rec), writes=RS(o0))
                        P.op("dve", lambda e, a1=a1: e.scalar_tensor_tensor(out=av.t[:], in0=a1[:, 0:128], scalar=s1.t[:, 0:1], in1=o0.t[:], op0=ALU.mult, op1=ALU.add),
                             reads=RS(a1t, s1, o0), writes=RS(av))
                        P.op("pool", lambda e: e.tensor_tensor(out=junk.t[:], in0=av.t[:], in1=av.t[:], op=ALU.mult), reads=RS(av), writes=RS(junk))
                        P.op("dve", lambda e: e.tensor_reduce(out=ssq.t[:], in_=junk.t[:], axis=AX.X, op=ALU.add), reads=RS(junk), writes=RS(ssq))
                        P.op("act", lambda e: e.activation(out=rstd.t[:], in_=ssq.t[:], func=ACT.Ln, scale=1.0 / 128, bias=EPS), reads=RS(ssq), writes=RS(rstd))
                        P.op("act", lambda e: e.activation(out=rstd.t[:], in_=rstd.t[:], func=ACT.Exp, scale=-0.5), reads=RS(rstd), writes=RS(rstd))
                        P.op("dve", lambda e, y_=y_, qs=qs: e.scalar_tensor_tensor(out=y_.t[:, qs, :], in0=av.t[:], scalar=rstd.t[:, 0:1], in1=C.subln.t[:, l, :], op0=ALU.mult, op1=ALU.mult),
                             reads=RS(av, rstd, C.subln), writes=RS(y_))
                    P.op("sp", lambda e, y_=y_, b=b, q0=q0, nqs=nqs, nq=nq, h=h: e.dma_start(
                        out=C.YA[b, q0:q0 + nq, h * 128:(h + 1) * 128].rearrange("(s p) c -> p s c", p=128), in_=y_.t[:, 0:nqs, :]), reads=RS(y_), dma=True)


def stage_swa(C, l):
    nc, P = C.nc, C.P
    last = (l == DEPTH - 1)
    with Stage(C, f"s3_{l}") as S:
        KT = [S.sb([64, NT], BF16, f"KT{i}") for i in range(2)]
        QT = [S.sb([64, 4, NT], BF16, f"QT{i}") for i in range(2)]
        V1 = [S.sb([128, TT, 65], BF16, f"V1{i}") for i in range(2)]
        PT = [S.sb([128, 4, 128], BF16, f"PT{i}") for i in range(3)]
        den = S.sb([128, 4], F32, "den")
        yo = [S.sb([128, 4, 64], BF16, f"yo{i}") for i in range(2)]
        ps = [S.ps([128, 512], F32, f"ps{i}") for i in range(3)]
        acc = [S.ps([128, 4, 65], F32, f"acc{i}") for i in range(2)]
        for v in V1:
            P.op("pool", lambda e, v=v: e.memset(v.t[:, :, 64:65], 1.0), writes=RS(v))
        ig = 0
        ips = 0
        ipt = 0
        iacc = 0
        iyo = 0
        for b in range(NB):
            for g in range(4):
                kt_, qt_, v1_ = KT[ig % 2], QT[ig % 2], V1[ig % 2]
                ig += 1
                P.op("sp", lambda e, kt_=kt_, b=b, g=g: e.dma_start(out=kt_.t[:], in_=C.QKT[b, 3072 + g * 64:3072 + (g + 1) * 64, :]), writes=RS(kt_), dma=True)
                P.op("sp", lambda e, qt_=qt_, b=b, g=g: e.dma_start(out=qt_.t[:], in_=C.QKT[b, 2048 + g * 256:2048 + (g + 1) * 256, :].rearrange("(i d) n -> d i n", d=64)),
                     writes=RS(qt_), dma=True)
                vsrc = C.VB[b].rearrange("(kt p) c -> p kt c", p=128)
                for k0 in range(0, TT, 6):
                    k1 = min(TT, k0 + 6)
                    P.op("sp", lambda e, v1_=v1_, k0=k0, k1=k1, g=g, vsrc=vsrc: e.dma_start(out=v1_.t[:, k0:k1, 0:64], in_=vsrc[:, k0:k1, g * 64:(g + 1) * 64]),
                         writes=RS(v1_), dma=True)
                blocks = list(range(TL)) + ([] if last else [TL, TL + 1])
                if "s3_blocks" in DBG:
                    blocks = blocks[:DBG["s3_blocks"]] + blocks[TL:]
                for n in blocks:
                    if n < TL:
                        kts = ([(n - 1, C.mprev)] if n > 0 else []) + [(n, None)] + ([(n + 1, C.mnext)] if n < TL - 1 else []) + [(TL, None), (TL + 1, None)]
                    else:
                        kts = [(TL, None), (TL + 1, None)]
                    a_ = acc[iacc % 2]
                    iacc += 1

                    def qk(i, kts=kts, kt_=kt_, qt_=qt_, n=n, base=ips):
                        kt = kts[i][0]
                        p_ = ps[(base + i) % 3]
                        P.op("pe", lambda e, p_=p_, kt=kt: e.matmul(
                            p_.t[:].rearrange("p (i q) -> p i q", i=4), kt_.t[:, kt * 128:(kt + 1) * 128], qt_.t[:, :, n * 128:(n + 1) * 128], start=True, stop=True),
                            reads=RS(kt_, qt_), writes=RS(p_))

                    qk(0)
                    qk(1)
                    for ki, (kt, mask) in enumerate(kts):
                        p_ = ps[ips % 3]
                        ips += 1
                        pt_ = PT[ipt % 3]
                        ipt += 1
                        if ki + 2 < len(kts):
                            qk(ki + 2)
                        P.op("act", lambda e, p_=p_, pt_=pt_: e.activation(out=pt_.t[:].rearrange("p i q -> p (i q)"), in_=p_.t[:], func=ACT.Exp, scale=0.125),
                             reads=RS(p_), writes=RS(pt_))
                        if mask is not None:
                            P.op("pool", lambda e, pt_=pt_, mask=mask: e.tensor_tensor(out=pt_.t[:], in0=pt_.t[:], in1=mask.t[:], op=ALU.mult), reads=RS(pt_, mask), writes=RS(pt_))
                        for i in range(4):
                            P.op("pe", lambda e, a_=a_, pt_=pt_, v1_=v1_, i=i, kt=kt, ki=ki: e.matmul(
                                a_.t[:, i, :], pt_.t[:, i, :], v1_.t[:, kt, :], start=(ki == 0 and i == 0), stop=False, skip_group_check=True),
                                reads=RS(pt_, v1_), writes=RS(a_))
                    y_ = yo[iyo % 2]
                    iyo += 1
                    P.op("dve", lambda e, a_=a_, g=g: e.tensor_tensor(out=den.t[:], in0=a_.t[:, :, 64], in1=C.esink.t[:, l, 4 * g:4 * g + 4], op=ALU.add), reads=RS(a_, C.esink), writes=RS(den))
                    P.op("dve", lambda e: e.reciprocal(den.t[:], den.t[:]), reads=RS(den), writes=RS(den))
                    P.op("dve", lambda e, a_=a_, y_=y_: e.tensor_tensor(out=y_.t[:], in0=a_.t[:, :, 0:64], in1=bc_last(den.t[:], 64), op=ALU.mult), reads=RS(a_, den), writes=RS(y_))
                    P.op("sp", lambda e, y_=y_, b=b, n=n, g=g: e.dma_start(out=C.YB[b, n * 128:(n + 1) * 128, g * 256:(g + 1) * 256], in_=y_.t[:].rearrange("p i d -> p (i d)")),
                         reads=RS(y_), dma=True)


def stage_merge(C, l):
    nc, P, W = C.nc, C.P, C.W
    last = (l == DEPTH - 1)
    with Stage(C, f"s4_{l}") as S:
        wts = {}
        for nm in ("w_branch_a", "w_branch_b", "w_out"):
            wt = S.sb([128, 8, D], BF16, nm)
            for j0 in (0, 4):
                P.op("pool", lambda e, wt=wt, nm=nm, j0=j0: e.dma_start(out=wt.t[:, j0:j0 + 4, :], in_=W[nm][l, j0 * 128:(j0 + 4) * 128, :].rearrange("(j p) n -> p j n", p=128)),
                     writes=RS(wt), dma=True)
            wts[nm] = wt
        wa, wb, wo = wts["w_branch_a"], wts["w_branch_b"], wts["w_out"]
        wr = S.sb([128, 8, NE], F32, "wr")
        P.op("sp", lambda e: e.dma_start(out=wr.t[:], in_=W["w_router"][l].rearrange("(j p) n -> p j n", p=128)), writes=RS(wr), dma=True)
        GATE1 = S.sb([128, D], F32, "GATE1")
        G2 = S.sb([128, D], F32, "G2")
        SH2 = S.sb([128, D], F32, "SH2")
        ya = [S.sb([128, D], BF16, f"ya{i}") for i in range(2)]
        yb = [S.sb([128, D], BF16, f"yb{i}") for i in range(2)]
        gt = [S.sb([128, 2 * D], BF16, f"gt{i}") for i in range(2)]
        xt = [S.sb([128, D], F32, f"xt{i}") for i in range(2)]
        yaT = S.sb([128, 8, 128], BF16, "yaT")
        ybT = S.sb([128, 8, 128], BF16, "ybT")
        msum = S.sb([128, D], F32, "msum")
        tmp = [S.sb([128, 512], F32, f"tmp{i}") for i in range(2)]
        mb = S.sb([128, D], BF16, "mb")
        mT = S.sb([128, 8, 128], BF16, "mT")
        xn = S.sb([128, D], F32, "xn")
        junk = S.sb([128, D], F32, "junk")
        h2f = S.sb([128, D], F32, "h2f")
        h2b = S.sb([128, D], BF16, "h2b")
        h2T = S.sb([128, 8, 128], F32, "h2T")
        ssq = S.sb([128, 1], F32, "ssq")
        rstd = S.sb([128, 1], F32, "rstd")
        mx = S.sb([128, 1], F32, "mx")
        se = S.sb([128, 1], F32, "se")
        ex = S.sb([128, NE], F32, "ex")
        aff = S.sb([128, NE], F32, "aff")
        affT = S.sb([NE, 128], F32, "affT")
        pT = [S.ps([128, 8, 128], BF16, f"pT{i}") for i in range(2)]
        pacc = [S.ps([128, 512], F32, f"pacc{i}") for i in range(2)]
        pTf = [S.ps([128, 4, 128], F32, f"pTf{i}") for i in range(2)]
        plog = S.ps([128, NE], F32, "plog")
        paT = S.ps([NE, 128], F32, "paT")
        it = 0
        for b in range(NB):
            for seg in range(2):
                if seg == 1 and last:
                    continue
                r = b if seg == 0 else 2
                for tl, k in ((GATE1, 2), (G2, 4), (SH2, 3)):
                    P.op("sp", lambda e, tl=tl, r=r, k=k: e.dma_start(out=tl.t[:], in_=modrow(C, l, r, k).to_broadcast([128, D])), writes=RS(tl), dma=True)
                tiles = list(range(TL) if seg == 0 else range(TL, TT))
                if "s4_tiles" in DBG:
                    tiles = tiles[:DBG["s4_tiles"]]
                for t in tiles:
                    tok0 = t * 128
                    ya_, yb_, gt_, x_ = ya[it % 2], yb[it % 2], gt[it % 2], xt[it % 2]
                    it += 1
                    P.op("sp", lambda e, ya_=ya_, b=b, tok0=tok0: e.dma_start(out=ya_.t[:], in_=C.YA[b, tok0:tok0 + 128, :]), writes=RS(ya_), dma=True)
                    P.op("sp", lambda e, yb_=yb_, b=b, tok0=tok0: e.dma_start(out=yb_.t[:], in_=C.YB[b, tok0:tok0 + 128, :]), writes=RS(yb_), dma=True)
                    P.op("sp", lambda e, gt_=gt_, b=b, tok0=tok0: e.dma_start(out=gt_.t[:], in_=C.GT[b, tok0:tok0 + 128, :]), writes=RS(gt_), dma=True)
                    P.op("sp", lambda e, x_=x_, b=b, t=t: e.dma_start(out=x_.t[:], in_=xres(C, b, t)), writes=RS(x_), dma=True)
                    for j in range(8):
                        P.op("pe", lambda e, j=j, ya_=ya_: e.transpose(pT[0].t[:, j, :], ya_.t[:, j * 128:(j + 1) * 128], C.ident_b.t[:]), reads=RS(ya_, C.ident_b), writes=RS(pT[0]))
                    P.op("act", lambda e: e.copy(yaT.t[:], pT[0].t[:]), reads=RS(pT[0]), writes=RS(yaT))
                    for j in range(8):
                        P.op("pe", lambda e, j=j, yb_=yb_: e.transpose(pT[1].t[:, j, :], yb_.t[:, j * 128:(j + 1) * 128], C.ident_b.t[:]), reads=RS(yb_, C.ident_b), writes=RS(pT[1]))
                    P.op("dve", lambda e: e.tensor_copy(ybT.t[:], pT[1].t[:]), reads=RS(pT[1]), writes=RS(ybT))
                    for n in range(2):
                        cs = slice(n * 512, (n + 1) * 512)
                        for j in range(8):
                            P.op("pe", lambda e, j=j, cs=cs: e.matmul(pacc[0].t[:], yaT.t[:, j, :], wa.t[:, j, cs], start=(j == 0), stop=(j == 7)), reads=RS(yaT, wa), writes=RS(pacc[0]))
                        P.op("dve", lambda e, cs=cs, gt_=gt_: e.tensor_tensor(out=msum.t[:, cs], in0=pacc[0].t[:], in1=gt_.t[:, cs], op=ALU.mult), reads=RS(pacc[0], gt_), writes=RS(msum))
                        for j in range(8):
                            P.op("pe", lambda e, j=j, cs=cs: e.matmul(pacc[1].t[:], ybT.t[:, j, :], wb.t[:, j, cs], start=(j == 0), stop=(j == 7)), reads=RS(ybT, wb), writes=RS(pacc[1]))
                        tm = tmp[n]
                        P.op("dve", lambda e, n=n, tm=tm, gt_=gt_: e.tensor_tensor(out=tm.t[:], in0=pacc[1].t[:], in1=gt_.t[:, D + n * 512:D + (n + 1) * 512], op=ALU.mult), reads=RS(pacc[1], gt_), writes=RS(tm))
                        P.op("pool", lambda e, cs=cs, tm=tm: e.tensor_tensor(out=mb.t[:, cs], in0=msum.t[:, cs], in1=tm.t[:], op=ALU.add), reads=RS(msum, tm), writes=RS(mb))
                    for j in range(8):
                        P.op("pe", lambda e, j=j: e.transpose(pT[0].t[:, j, :], mb.t[:, j * 128:(j + 1) * 128], C.ident_b.t[:]), reads=RS(mb, C.ident_b), writes=RS(pT[0]))
                    P.op("act", lambda e: e.copy(mT.t[:], pT[0].t[:]), reads=RS(pT[0]), writes=RS(mT))
                    for n in range(2):
                        cs = slice(n * 512, (n + 1) * 512)
                        pa = pacc[n]
                        tm = tmp[n]
                        for j in range(8):
                            P.op("pe", lambda e, j=j, cs=cs, pa=pa: e.matmul(pa.t[:], mT.t[:, j, :], wo.t[:, j, cs], start=(j == 0), stop=(j == 7)), reads=RS(mT, wo), writes=RS(pa))
                        P.op("dve", lambda e, cs=cs, pa=pa, tm=tm: e.tensor_tensor(out=tm.t[:], in0=pa.t[:], in1=GATE1.t[:, cs], op=ALU.mult), reads=RS(pa, GATE1), writes=RS(tm))
                        P.op("pool", lambda e, cs=cs, tm=tm, x_=x_: e.tensor_tensor(out=xn.t[:, cs], in0=tm.t[:], in1=x_.t[:, cs], op=ALU.add), reads=RS(tm, x_), writes=RS(xn))
                    P.op("sp", lambda e, b=b, t=t: e.dma_start(out=xres(C, b, t), in_=xn.t[:]), reads=RS(xn), dma=True)
                    P.op("act", lambda e: e.activation(out=junk.t[:], in_=xn.t[:], func=ACT.Square, accum_out=ssq.t[:]), reads=RS(xn), writes=RS(junk, ssq))
                    P.op("act", lambda e: e.activation(out=rstd.t[:], in_=ssq.t[:], func=ACT.Sqrt, scale=1.0 / D, bias=EPS), reads=RS(ssq), writes=RS(rstd))
                    P.op("dve", lambda e: e.reciprocal(rstd.t[:], rstd.t[:]), reads=RS(rstd), writes=RS(rstd))
                    P.op("dve", lambda e: e.scalar_tensor_tensor(out=h2f.t[:], in0=xn.t[:], scalar=rstd.t[:, 0:1], in1=G2.t[:], op0=ALU.mult, op1=ALU.mult), reads=RS(xn, rstd, G2), writes=RS(h2f))
                    P.op("pool", lambda e: e.tensor_tensor(out=h2f.t[:], in0=h2f.t[:], in1=SH2.t[:], op=ALU.add), reads=RS(h2f, SH2), writes=RS(h2f))
                    P.op("act", lambda e: e.copy(h2b.t[:], h2f.t[:]), reads=RS(h2f), writes=RS(h2b))
                    if seg == 0:
                        P.op("sp", lambda e, b=b, tok0=tok0: e.dma_start(out=C.H2[b][tok0:tok0 + 128, :], in_=h2b.t[:]), reads=RS(h2b), dma=True)
                    else:
                        P.op("sp", lambda e, b=b, tok0=tok0: e.dma_start(out=C.H2C[b][tok0 - NL:tok0 - NL + 128, :], in_=h2b.t[:]), reads=RS(h2b), dma=True)
                    for half in range(2):
                        for jj in range(4):
                            j = half * 4 + jj
                            P.op("pe", lambda e, half=half, jj=jj, j=j: e.transpose(pTf[half].t[:, jj, :], h2f.t[:, j * 128:(j + 1) * 128], C.ident_f.t[:]), reads=RS(h2f, C.ident_f), writes=RS(pTf[half]))
                    P.op("act", lambda e: e.copy(h2T.t[:, 0:4, :], pTf[0].t[:]), reads=RS(pTf[0]), writes=RS(h2T))
                    P.op("dve", lambda e: e.tensor_copy(h2T.t[:, 4:8, :], pTf[1].t[:]), reads=RS(pTf[1]), writes=RS(h2T))
                    for j in range(8):
                        P.op("pe", lambda e, j=j: e.matmul(plog.t[:], h2T.t[:, j, :], wr.t[:, j, :], start=(j == 0), stop=(j == 7)), reads=RS(h2T, wr), writes=RS(plog))
                    P.op("dve", lambda e: e.tensor_reduce(out=mx.t[:], in_=plog.t[:], axis=AX.X, op=ALU.max), reads=RS(plog), writes=RS(mx))
                    P.op("dve", lambda e: e.tensor_scalar(mx.t[:], mx.t[:], -1.0, None, op0=ALU.mult), reads=RS(mx), writes=RS(mx))
                    P.op("act", lambda e: e.activation(out=ex.t[:], in_=plog.t[:], func=ACT.Exp, bias=mx.t[:, 0:1], accum_out=se.t[:]), reads=RS(plog, mx), writes=RS(ex, se))
                    P.op("dve", lambda e: e.reciprocal(se.t[:], se.t[:]), reads=RS(se), writes=RS(se))
                    P.op("dve", lambda e: e.tensor_scalar(aff.t[:], ex.t[:], se.t[:, 0:1], None, op0=ALU.mult), reads=RS(ex, se), writes=RS(aff))
                    P.op("pe", lambda e: e.transpose(paT.t[:], aff.t[:], C.ident_f.t[:]), reads=RS(aff, C.ident_f), writes=RS(paT))
                    P.op("act", lambda e: e.copy(affT.t[:], paT.t[:]), reads=RS(paT), writes=RS(affT))
                    P.op("sp", lambda e, b=b, tok0=tok0: e.dma_start(out=C.AFF[b, :, tok0:tok0 + 128], in_=affT.t[:]), reads=RS(affT), dma=True)


def alloc_route_tiles(C):
    g = C.es
    nc = C.nc
    pers = lambda shape, dt, nm: T(g.enter_context(nc.sbuf_tensor("c_" + nm, list(shape), dt)), nm)
    C.idxT = pers([128, 4, 32], U32, "idxT")
    C.valsT = pers([128, 4, 32], F32, "valsT")
    C.idxcT = pers([32, 32], U32, "idxcT")
    C.valscT = pers([32, 32], F32, "valscT")


def stage_topk(C, l):
    nc, P = C.nc, C.P
    last = (l == DEPTH - 1)
    with Stage(C, f"s5_{l}") as S:
        work = S.sb([32, NL], F32, "work")
        vals = S.sb([32, CAP_L], F32, "vals")
        idx = S.sb([32, CAP_L], U32, "idx")
        idxf = S.sb([32, CAP_L], F32, "idxf")
        pt = [S.ps([128, 32], F32, f"pt{i}") for i in range(2)]
        for b in range(NB):
            P.op("sp", lambda e, b=b: e.dma_start(out=work.t[b * 16:(b + 1) * 16, :], in_=C.AFF[b, :, 0:NL]), writes=RS(work), dma=True)
        for r in range(CAP_L // 8):
            rs_ = slice(r * 8, (r + 1) * 8)
            P.op("dve", lambda e, rs_=rs_: e.max(out=vals.t[:, rs_], in_=work.t[:]), reads=RS(work), writes=RS(vals))
            P.op("dve", lambda e, rs_=rs_: e.max_index(out=idx.t[:, rs_], in_max=vals.t[:, rs_], in_values=work.t[:]), reads=RS(work, vals), writes=RS(idx))
            P.op("dve", lambda e, rs_=rs_: e.match_replace(out=work.t[:], in_to_replace=vals.t[:, rs_], in_values=work.t[:], imm_value=-1.0), reads=RS(work, vals), writes=RS(work))
        P.op("dve", lambda e: e.tensor_copy(idxf.t[:], idx.t[:]), reads=RS(idx), writes=RS(idxf))
        k = 0
        for s in range(4):
            for src, dst in ((idxf, C.idxT), (vals, C.valsT)):
                p_ = pt[k % 2]
                k += 1
                P.op("pe", lambda e, p_=p_, src=src, s=s: e.transpose(p_.t[:], src.t[:, s * 128:(s + 1) * 128], C.ident_f.t[0:32, 0:32]), reads=RS(src, C.ident_f), writes=RS(p_))
                P.op("dve", lambda e, p_=p_, dst=dst, s=s: e.tensor_copy(dst.t[:, s, :], p_.t[:]), reads=RS(p_), writes=RS(dst))
        if not last:
            workc = S.sb([32, NCX], F32, "workc")
            valsc = S.sb([32, CAP_C], F32, "valsc")
            idxc = S.sb([32, CAP_C], U32, "idxc")
            idxcf = S.sb([32, CAP_C], F32, "idxcf")
            for b in range(NB):
                P.op("sp", lambda e, b=b: e.dma_start(out=workc.t[b * 16:(b + 1) * 16, :], in_=C.AFF[b, :, NL:NT]), writes=RS(workc), dma=True)
            for r in range(CAP_C // 8):
                rs_ = slice(r * 8, (r + 1) * 8)
                P.op("dve", lambda e, rs_=rs_: e.max(out=valsc.t[:, rs_], in_=workc.t[:]), reads=RS(workc), writes=RS(valsc))
                P.op("dve", lambda e, rs_=rs_: e.max_index(out=idxc.t[:, rs_], in_max=valsc.t[:, rs_], in_values=workc.t[:]), reads=RS(workc, valsc), writes=RS(idxc))
                P.op("dve", lambda e, rs_=rs_: e.match_replace(out=workc.t[:], in_to_replace=valsc.t[:, rs_], in_values=workc.t[:], imm_value=-1.0), reads=RS(workc, valsc), writes=RS(workc))
            P.op("dve", lambda e: e.tensor_copy(idxcf.t[:], idxc.t[:]), reads=RS(idxc), writes=RS(idxcf))
            for src, dst in ((idxcf, C.idxcT), (valsc, C.valscT)):
                p_ = pt[k % 2]
                k += 1
                P.op("pe", lambda e, p_=p_, src=src: e.transpose(p_.t[0:32, :], src.t[:], C.ident_f.t[0:32, 0:32]), reads=RS(src, C.ident_f), writes=RS(p_))
                P.op("dve", lambda e, p_=p_, dst=dst: e.tensor_copy(dst.t[:], p_.t[0:32, :]), reads=RS(p_), writes=RS(dst))
        if "dump_route" in DBG:
            P.op("sp", lambda e: e.dma_start(out=C.DIDX[l], in_=C.idxT.t[:].rearrange("p s c -> p (s c)")), reads=RS(C.idxT), dma=True)
            P.op("sp", lambda e: e.dma_start(out=C.DVAL[l], in_=C.valsT.t[:].rearrange("p s c -> p (s c)")), reads=RS(C.valsT), dma=True)


def stage_experts(C, l):
    nc, P, W = C.nc, C.P, C.W
    last = (l == DEPTH - 1)
    ncx = 0 if last else 2 * CAP_C
    NTOK = NB * CAP_L + ncx
    with Stage(C, f"s6_{l}") as S:
        xgT = S.sb([128, 8, NB * CAP_L + 2 * CAP_C], BF16, "xgT")
        hT = S.sb([128, FFC, NB * CAP_L + 2 * CAP_C], BF16, "hT")
        w2 = S.sb([128, FFC, D], BF16, "w2")
        wblk = [S.sb([128, 2, 8, 256], BF16, f"wblk{i}") for i in range(4)]
        xgs = [S.sb([128, D], BF16, f"xg{i}") for i in range(NB * 4 + NB)]
        ye = [S.sb([128, D], F32, f"ye{i}") for i in range(2)]
        stmp = [S.sb([128, 512], F32, f"stmp{i}") for i in range(2)]
        ytmp = [S.sb([128, 512], F32, f"ytmp{i}") for i in range(2)]
        G2g = [S.sb([128, D], F32, f"G2g{i}") for i in range(3)]
        pT = [S.ps([128, 8, 128], BF16, f"pT{i}") for i in range(2)]
        ps1 = [S.ps([128, 512], F32, f"ps1{i}") for i in range(2)]
        ps3 = [S.ps([128, 512], F32, f"ps3{i}") for i in range(2)]
        py = [S.ps([128, 512], F32, f"py{i}") for i in range(2)]
        for r in range(3):
            P.op("sp", lambda e, r=r: e.dma_start(out=G2g[r].t[:], in_=modrow(C, l, r, 5).to_broadcast([128, D])), writes=RS(G2g[r]), dma=True)
        xres_r = [Res(f"xres{b}") for b in range(NB)]
        xcres_r = [Res(f"xcres{b}") for b in range(NB)]
        ipT = iw = ips = iy = iye = 0
        experts = list(range(NE) if "s6_experts" not in DBG else range(DBG["s6_experts"]))
        n_g = NB * 4 + (0 if last else NB)

        def gather(ex):
            k = 0
            for b in range(NB):
                be = b * 16 + ex
                for s in range(4):
                    g_ = xgs[k]
                    k += 1
                    P.op("pool", lambda e, g_=g_, b=b, s=s, be=be: e.indirect_dma_start(
                        out=g_.t[:], out_offset=None, in_=C.H2[b], in_offset=bass.IndirectOffsetOnAxis(ap=C.idxT.t[:, s, be:be + 1], axis=0)),
                        reads=RS(C.idxT), writes=RS(g_), dma=True)
            if not last:
                for b in range(NB):
                    be = b * 16 + ex
                    g_ = xgs[k]
                    k += 1
                    P.op("pool", lambda e, g_=g_, b=b, be=be: e.indirect_dma_start(
                        out=g_.t[0:CAP_C, :], out_offset=None, in_=C.H2C[b], in_offset=bass.IndirectOffsetOnAxis(ap=C.idxcT.t[0:CAP_C, be:be + 1], axis=0)),
                        reads=RS(C.idxcT), writes=RS(g_), dma=True)

        def to_feature_major(ex):
            nonlocal ipT
            k = 0
            for b in range(NB):
                for s in range(4):
                    g_ = xgs[k]
                    k += 1
                    p_ = pT[ipT % 2]
                    ipT += 1
                    for j in range(8):
                        P.op("pe", lambda e, p_=p_, g_=g_, j=j: e.transpose(p_.t[:, j, :], g_.t[:, j * 128:(j + 1) * 128], C.ident_b.t[:]), reads=RS(g_, C.ident_b), writes=RS(p_))
                    c0 = b * CAP_L + s * 128
                    P.op("act", lambda e, p_=p_, c0=c0: e.copy(xgT.t[:, :, c0:c0 + 128], p_.t[:]), reads=RS(p_), writes=RS(xgT))
            if not last:
                for b in range(NB):
                    g_ = xgs[k]
                    k += 1
                    p_ = pT[ipT % 2]
                    ipT += 1
                    for j in range(8):
                        P.op("pe", lambda e, p_=p_, g_=g_, j=j: e.transpose(p_.t[:, j, 0:CAP_C], g_.t[0:CAP_C, j * 128:(j + 1) * 128], C.ident_b.t[0:CAP_C, 0:CAP_C]), reads=RS(g_, C.ident_b), writes=RS(p_))
                    c0 = NB * CAP_L + b * CAP_C
                    P.op("act", lambda e, p_=p_, c0=c0: e.copy(xgT.t[:, :, c0:c0 + CAP_C], p_.t[:, :, 0:CAP_C]), reads=RS(p_), writes=RS(xgT))

        pre_w = {}

        def load_w13(ex, fg):
            nonlocal iw
            wk = wblk[iw % 4]
            iw += 1
            for k, nm in enumerate(("w_e1", "w_e3")):
                P.op("pool", lambda e, wk=wk, k=k, nm=nm, fg=fg, ex=ex: e.dma_start(out=wk.t[:, k, :, :], in_=W[nm][l, ex, :, fg * 256:(fg + 1) * 256].rearrange("(j p) f -> p j f", p=128)),
                     writes=RS(wk), dma=True)
            return wk

        gather(experts[0])
        to_feature_major(experts[0])
        for iex, ex in enumerate(experts):
            nxt = experts[iex + 1] if iex + 1 < len(experts) else None
            for f0, f1 in ((0, 6), (6, 12), (12, 17), (17, 22)):
                P.op("pool", lambda e, f0=f0, f1=f1, ex=ex: e.dma_start(out=w2.t[:, f0:f1, :], in_=W["w_e2"][l, ex, f0 * 128:f1 * 128, :].rearrange("(f p) d -> p f d", p=128)),
                     writes=RS(w2), dma=True)
            groups = [(0, 384), (384, 384), (768, NTOK - 768)] if ncx else [(0, 512), (512, 512)]
            for fg in range(FFC // 2):
                wk = pre_w.pop((ex, fg)) if (ex, fg) in pre_w else load_w13(ex, fg)
                if fg == 4 and nxt is not None:
                    gather(nxt)
                for fc in range(2):
                    f = fg * 2 + fc
                    for (c0, n) in groups:
                        p1, p3 = ps1[ips % 2], ps3[ips % 2]
                        st_ = stmp[ips % 2]
                        ips += 1
                        for j in range(8):
                            P.op("pe", lambda e, p1=p1, wk=wk, j=j, fc=fc, c0=c0, n=n: e.matmul(p1.t[:, 0:n], wk.t[:, 0, j, fc * 128:(fc + 1) * 128], xgT.t[:, j, c0:c0 + n], start=(j == 0), stop=(j == 7)),
                                 reads=RS(wk, xgT), writes=RS(p1))
                        for j in range(8):
                            P.op("pe", lambda e, p3=p3, wk=wk, j=j, fc=fc, c0=c0, n=n: e.matmul(p3.t[:, 0:n], wk.t[:, 1, j, fc * 128:(fc + 1) * 128], xgT.t[:, j, c0:c0 + n], start=(j == 0), stop=(j == 7)),
                                 reads=RS(wk, xgT), writes=RS(p3))
                        P.op("act", lambda e, p1=p1, st_=st_, n=n: e.activation(out=st_.t[:, 0:n], in_=p1.t[:, 0:n], func=ACT.Silu), reads=RS(p1), writes=RS(st_))
                        P.op("dve", lambda e, p3=p3, st_=st_, f=f, c0=c0, n=n: e.tensor_tensor(out=hT.t[:, f, c0:c0 + n], in0=p3.t[:, 0:n], in1=st_.t[:, 0:n], op=ALU.mult), reads=RS(p3, st_), writes=RS(hT))
            if nxt is not None:
                for fg in range(4):
                    pre_w[(nxt, fg)] = load_w13(nxt, fg)
                to_feature_major(nxt)
            subt = [(b, s, b * CAP_L + s * 128, 128) for b in range(NB) for s in range(4)]
            if not last:
                subt += [(b, None, NB * CAP_L + b * CAP_C, CAP_C) for b in range(NB)]
            for (b, s, c0, m) in subt:
                be = b * 16 + ex
                ye_ = ye[iye % 2]
                iye += 1
                if s is not None:
                    gate_ap = C.valsT.t[:, s, be:be + 1]
                    gres = C.valsT
                    g2_ = G2g[b]
                else:
                    gate_ap = C.valscT.t[0:CAP_C, be:be + 1]
                    gres = C.valscT
                    g2_ = G2g[2]
                for n in range(2):
                    p_ = py[iy % 2]
                    yt_ = ytmp[iy % 2]
                    iy += 1
                    for f in range(FFC):
                        P.op("pe", lambda e, p_=p_, f=f, c0=c0, m=m, n=n: e.matmul(p_.t[0:m, :], hT.t[:, f, c0:c0 + m], w2.t[:, f, n * 512:(n + 1) * 512], start=(f == 0), stop=(f == FFC - 1)),
                             reads=RS(hT, w2), writes=RS(p_))
                    P.op("act", lambda e, p_=p_, yt_=yt_, m=m, gate_ap=gate_ap: e.activation(out=yt_.t[0:m, :], in_=p_.t[0:m, :], func=ACT.Copy, scale=gate_ap), reads=RS(p_, gres), writes=RS(yt_))
                    P.op("dve", lambda e, yt_=yt_, ye_=ye_, g2_=g2_, m=m, n=n: e.tensor_tensor(out=ye_.t[0:m, n * 512:(n + 1) * 512], in0=yt_.t[0:m, :], in1=g2_.t[0:m, n * 512:(n + 1) * 512], op=ALU.mult),
                         reads=RS(yt_, g2_), writes=RS(ye_))
                if s is not None:
                    P.op("pool", lambda e, ye_=ye_, b=b, s=s, be=be: e.indirect_dma_start(
                        out=C.out[b], out_offset=bass.IndirectOffsetOnAxis(ap=C.idxT.t[:, s, be:be + 1], axis=0), in_=ye_.t[:], in_offset=None, compute_op=ALU.add),
                        reads=RS(ye_, C.idxT), writes=[xres_r[b]], dma=True)
                else:
                    P.op("pool", lambda e, ye_=ye_, b=b, be=be: e.indirect_dma_start(
                        out=C.XC[b], out_offset=bass.IndirectOffsetOnAxis(ap=C.idxcT.t[0:CAP_C, be:be + 1], axis=0), in_=ye_.t[0:CAP_C, :], in_offset=None, compute_op=ALU.add),
                        reads=RS(ye_, C.idxcT), writes=[xcres_r[b]], dma=True)


def build_program(nc, n_layers=DEPTH, dbg_outs=(), no_experts=False, stages=None):
    C = setup(nc, dbg_outs=dbg_outs, n_layers=n_layers, no_experts=no_experts)
    if "dump_route" in DBG:
        C.DIDX = nc.dram_tensor("DIDX", [DEPTH, 128, 128], U32, kind="ExternalOutput").ap()
        C.DVAL = nc.dram_tensor("DVAL", [DEPTH, 128, 128], F32, kind="ExternalOutput").ap()
    stage_consts(C)
    alloc_route_tiles(C)
    fns = {"inproj": stage_inproj, "diff": stage_diff, "swa": stage_swa, "merge": stage_merge, "topk": stage_topk, "experts": stage_experts}
    order = ["inproj", "diff", "swa", "merge", "topk", "experts"]
    for l in range(n_layers):
        for nm in order:
            if stages is None or nm in stages:
                fns[nm](C, l)
    C.es.close()
    return C


def _rope_table():
    rows = NL // 64
    row = np.repeat(np.arange(rows), 64).astype(np.float32)
    col = np.tile(np.arange(64), rows).astype(np.float32)
    inv = (np.float32(10000.0) ** (-np.arange(0, 32, 2, dtype=np.float32) / np.float32(32))).astype(np.float32)
    ang = np.stack([row[:, None] * inv, col[:, None] * inv], axis=1).astype(np.float32)
    cs, sn = np.cos(ang).astype(np.float32), np.sin(ang).astype(np.float32)
    csx = np.repeat(cs[:, :, None, :], 2, axis=2).reshape(NL, 64)
    return np.ascontiguousarray(np.concatenate([csx, sn.reshape(NL, 32)], axis=1).astype(np.float32))


_NC_CACHE = {}


def kernel(**inputs):
    n_cores = 8
    if "nc" not in _NC_CACHE:
        nc = bass.Bass("TRN2", target_bir_lowering=False)
        build_program(nc)
        _NC_CACHE["nc"] = nc
    nc = _NC_CACHE["nc"]
    f32 = lambda a: np.ascontiguousarray(np.asarray(a, dtype=np.float32))
    x, c, ctx, c_ctx = f32(inputs["x"]), f32(inputs["c"]), f32(inputs["ctx"]), f32(inputs["c_ctx"])
    rope = _rope_table()
    shared = {}
    for n, s in WEIGHT_SPECS:
        shared[n] = f32(inputs[n]).reshape(s)
    in_maps = []
    for core in range(n_cores):
        b0 = core * NB
        cc = np.concatenate([c[b0:b0 + NB], c_ctx[None, :]], axis=0)
        m = {"x": x[b0:b0 + NB], "ctxin": ctx[b0:b0 + NB],
             "cT": np.ascontiguousarray(cc.reshape(3, 8, 128).transpose(2, 1, 0)), "rope": rope}
        m.update(shared)
        in_maps.append(m)
    res = run_bass_kernel_spmd(nc, in_maps, core_ids=list(range(n_cores)))
    out = np.empty((n_cores * NB, NL, D), np.float32)
    for core in range(n_cores):
        for b in range(NB):
            out[core * NB + b] = res.results[core][f"out{b}"]
    return out
```

```python
import numpy as np
import concourse.bass as bass
import concourse.mybir as mybir
from concourse.bass_utils import run_bass_kernel_spmd
from contextlib import ExitStack

F32 = mybir.dt.float32
BF16 = mybir.dt.bfloat16
U32 = mybir.dt.uint32
I32 = mybir.dt.int32
ALU = mybir.AluOpType
ACT = mybir.ActivationFunctionType
AX = mybir.AxisListType

D = 1024
NL = 4096
NCX = 256
NT = NL + NCX
TL = NL // 128
TC = NCX // 128
TT = TL + TC
DEPTH = 4
HD = 64
IN_W = 6656
NE = 16
FF = 2816
FFC = FF // 128
CAP_L = 512
CAP_C = 32
EPS = 1e-6
NB = 2
QKW = 3328

ENGS = ("pe", "act", "dve", "pool", "sp")
SAME_ENG_SYNC = True
DBG = {}


class Res:
    __slots__ = ("name", "w", "r_eng", "r_dma", "excl")

    def __init__(self, name="", excl=False):
        self.name = name
        self.excl = excl
        self.w = None
        self.r_eng = {}
        self.r_dma = []


class Prog:
    def __init__(self, nc, n_dma_slots=None):
        self.nc = nc
        self.eng_obj = {"pe": nc.tensor, "act": nc.scalar, "dve": nc.vector,
                        "pool": nc.gpsimd, "sp": nc.sync}
        self.sem = {e: nc.alloc_semaphore(name=f"sem_{e}") for e in ENGS}
        self.cnt = {e: 0 for e in ENGS}
        n_dma_slots = n_dma_slots or {"sp": 40, "pool": 32, "act": 16}
        self.slots = {q: [[nc.alloc_semaphore(name=f"dq_{q}{i}"), 0] for i in range(n)]
                      for q, n in n_dma_slots.items()}
        self.slot_i = {q: 0 for q in self.slots}
        self.seen = {e: {} for e in ENGS}
        self.ops = {e: [] for e in ENGS}
        self.semobj = {}
        for e in ENGS:
            self.semobj[("E", e)] = self.sem[e]
        self.n_ops = 0

    def _need(self, eng, tok, waits):
        if tok is None:
            return
        if tok[0] == "E":
            if tok[1] == eng and (eng == "pe" or not SAME_ENG_SYNC):
                return
            key = ("E", tok[1])
        else:
            key = ("D",) + tok[1]
        if self.seen[eng].get(key, 0) >= tok[2]:
            return
        self.seen[eng][key] = tok[2]
        waits[key] = max(waits.get(key, 0), tok[2])

    def op(self, eng, fn, reads=(), writes=(), dma=False):
        if DBG.get("max_ops") is not None and self.n_ops >= DBG["max_ops"]:
            return None
        waits = {}
        if any(r.excl for r in reads):
            writes = list(writes) + [r for r in reads if r.excl]
            reads = [r for r in reads if not r.excl]
        for r in reads:
            self._need(eng, r.w, waits)
        for w in writes:
            self._need(eng, w.w, waits)
            for e2, c in w.r_eng.items():
                self._need(eng, ("E", e2, c), waits)
            for t in w.r_dma:
                self._need(eng, t, waits)
        if dma:
            q = eng
            i = self.slot_i[q]
            self.slot_i[q] = (i + 1) % len(self.slots[q])
            slot = self.slots[q][i]
            if slot[1] > 0:
                self._need(eng, ("D", (q, i), slot[1]), waits)
            slot[1] += 16
            tok = ("D", (q, i), slot[1])
            inc = (slot[0], 16)
        else:
            self.cnt[eng] += 1
            tok = ("E", eng, self.cnt[eng])
            inc = (self.sem[eng], 1)
        wl = []
        for key, val in waits.items():
            s = self.sem[key[1]] if key[0] == "E" else self.slots[key[1]][key[2]][0]
            wl.append((s, val))
        self.ops[eng].append((wl, fn, inc))
        self.n_ops += 1
        for r in reads:
            if dma:
                r.r_dma.append(tok)
            else:
                r.r_eng[eng] = tok[2]
        for w in writes:
            w.w = tok
            w.r_eng = {}
            w.r_dma = []
        return tok

    def barrier(self):
        waits = {}
        for e in ENGS:
            if e != "sp" and self.cnt[e] > 0:
                self._need("sp", ("E", e, self.cnt[e]), waits)
        for q, sl in self.slots.items():
            for i, (s, v) in enumerate(sl):
                if v > 0:
                    self._need("sp", ("D", (q, i), v), waits)
        wl = []
        for key, val in waits.items():
            s = self.sem[key[1]] if key[0] == "E" else self.slots[key[1]][key[2]][0]
            wl.append((s, val))
        self.cnt["sp"] += 1
        tok = ("E", "sp", self.cnt["sp"])
        self.ops["sp"].append((wl, lambda e: e.nop(), (self.sem["sp"], 1)))
        for e in ENGS:
            if e == "sp":
                continue
            self.seen[e][("E", "sp")] = tok[2]
            self.cnt[e] += 1
            self.ops[e].append(([(self.sem["sp"], tok[2])], lambda e_: e_.nop(), (self.sem[e], 1)))
            for key in list(self.seen["sp"].keys()):
                self.seen[e][key] = max(self.seen[e].get(key, 0), self.seen["sp"][key])

    def emit(self):
        nc = self.nc
        ops = self.ops
        self.ops = {e: [] for e in ENGS}

        def run(e, lst):
            for wl, fn, inc in lst:
                for s, v in wl:
                    e.wait_ge(s, v)
                ins = fn(e)
                ins.then_inc(inc[0], inc[1])

        with nc.Block() as block:
            @block.tensor
            def _(e):
                run(e, ops["pe"])

            @block.scalar
            def _(e):
                run(e, ops["act"])

            @block.vector
            def _(e):
                run(e, ops["dve"])

            @block.gpsimd
            def _(e):
                run(e, ops["pool"])

            @block.sync
            def _(e):
                run(e, ops["sp"])


class T:
    __slots__ = ("t", "r")

    def __init__(self, t, name=""):
        self.t = t
        self.r = Res(name)


def RS(*tiles):
    return [x.r for x in tiles]


class Ctx:
    pass


class Stage:
    def __init__(self, C, name):
        self.C = C
        self.name = name
        self.es = ExitStack()
        self.n = 0

    def __enter__(self):
        self.es.__enter__()
        return self

    def __exit__(self, *a):
        if a[0] is None:
            self.C.P.barrier()
            self.C.P.emit()
        return self.es.__exit__(*a)

    def sb(self, shape, dt, name=None):
        self.n += 1
        nm = f"{self.name}_{name or 's'}{self.n}"
        return T(self.es.enter_context(self.C.nc.sbuf_tensor(nm, list(shape), dt)), nm)

    def ps(self, shape, dt, name=None):
        self.n += 1
        nm = f"{self.name}_{name or 'p'}{self.n}"
        t = T(self.es.enter_context(self.C.nc.psum_tensor(nm, list(shape), dt)), nm)
        t.r.excl = True
        return t


def bc_mid(ap2d, n):
    p, w = ap2d.shape
    return ap2d.rearrange("p (o w) -> p o w", o=1).to_broadcast([p, n, w])


def bc_last(ap2d, w):
    p, n = ap2d.shape
    return ap2d.rearrange("p (n o) -> p n o", o=1).to_broadcast([p, n, w])


WEIGHT_SPECS = [
    ("w_ada", [DEPTH, D, 6 * D]), ("b_ada", [DEPTH, 6 * D]), ("norm1_g", [DEPTH, D]),
    ("w_in", [DEPTH, D, IN_W]), ("b_gate", [DEPTH, 2 * D]), ("diff_q_g", [DEPTH, HD]),
    ("diff_k_g", [DEPTH, HD]), ("diff_lambda", [DEPTH, 4 * HD]), ("diff_subln_g", [DEPTH, 128]),
    ("swa_q_g", [DEPTH, HD]), ("swa_k_g", [DEPTH, HD]), ("swa_sink", [DEPTH, 16]),
    ("w_branch_a", [DEPTH, D, D]), ("w_branch_b", [DEPTH, D, D]), ("w_out", [DEPTH, D, D]),
    ("norm2_g", [DEPTH, D]), ("w_router", [DEPTH, D, NE]),
    ("w_e1", [DEPTH, NE, D, FF]), ("w_e3", [DEPTH, NE, D, FF]), ("w_e2", [DEPTH, NE, FF, D]),
]


def setup(nc, dbg_outs=(), n_layers=DEPTH, no_experts=False):
    C = Ctx()
    C.n_layers = n_layers
    C.nc = nc
    C.P = Prog(nc)
    di = lambda n, s, dt=F32: nc.dram_tensor(n, list(s), dt, kind="ExternalInput").ap()
    dn = lambda n, s, dt: nc.dram_tensor(n, list(s), dt, kind=("ExternalOutput" if n in dbg_outs else "Internal")).ap()
    C.x = di("x", [NB, NL, D])
    C.ctxin = di("ctxin", [NB, NCX, D])
    C.cT = di("cT", [128, 8, 3])
    C.rope = di("rope", [NL, 96])
    C.W = {}
    for n, s in WEIGHT_SPECS:
        s = [n_layers] + list(s[1:])
        if no_experts and n.startswith("w_e"):
            s = [1, 1, 2, 2]
        C.W[n] = di(n, s)
    C.out = [nc.dram_tensor(f"out{b}", [NL, D], F32, kind="ExternalOutput").ap() for b in range(NB)]
    C.XC = [dn(f"XC{b}", [NCX, D], F32) for b in range(NB)]
    C.QKT = dn("QKT", [NB, QKW, NT], BF16)
    C.VA = dn("VA", [NB, NT, D], BF16)
    C.VB = dn("VB", [NB, NT, 256], BF16)
    C.GT = dn("GT", [NB, NT, 2 * D], BF16)
    C.YA = dn("YA", [NB, NT, D], BF16)
    C.YB = dn("YB", [NB, NT, D], BF16)
    C.H2 = [dn(f"H2_{b}", [NL, D], BF16) for b in range(NB)]
    C.MODV = dn("MODV", [DEPTH, 3, 6 * D], F32)
    C.AFF = dn("AFF", [NB, NE, NT], F32)
    C.H2C = [dn(f"H2C_{b}", [NCX, D], BF16) for b in range(NB)]
    C.es = ExitStack()
    return C


def xres(C, b, t):
    if t < TL:
        return C.out[b][t * 128:(t + 1) * 128, :]
    return C.XC[b][(t - TL) * 128:(t - TL + 1) * 128, :]


def lam_init(l):
    import math
    return 0.8 - 0.6 * math.exp(-0.3 * l)


def stage_consts(C):
    nc, P = C.nc, C.P
    g = C.es
    pers = lambda shape, dt, nm: T(g.enter_context(nc.sbuf_tensor("c_" + nm, list(shape), dt)), nm)
    C.ident_b = pers([128, 128], BF16, "identb")
    C.ident_f = pers([128, 128], F32, "identf")
    C.gain = pers([128, DEPTH, 4, HD], F32, "gain")
    C.subln = pers([128, DEPTH, 128], F32, "subln")
    C.esink = pers([128, DEPTH, 16], F32, "esink")
    C.nlam = pers([128, DEPTH], F32, "nlam")
    C.mprev = pers([128, 4, 128], BF16, "mprev")
    C.mnext = pers([128, 4, 128], BF16, "mnext")
    W = C.W
    with Stage(C, "cst") as S:
        for b in range(NB):
            for k in range(8):
                P.op("sp", lambda e, b=b, k=k: e.dma_start(out=C.out[b][k * 512:(k + 1) * 512, :], in_=C.x[b, k * 512:(k + 1) * 512, :]), dma=True)
            P.op("sp", lambda e, b=b: e.dma_start(out=C.XC[b], in_=C.ctxin[b]), dma=True)
        P.op("pool", lambda e: e.memset(C.ident_f.t[:], 0.0), writes=RS(C.ident_f))
        P.op("pool", lambda e: e.affine_select(out=C.ident_f.t[:], in_=C.ident_f.t[:], pattern=[[-1, 128]],
                                               compare_op=ALU.not_equal, fill=1.0, base=0, channel_multiplier=1),
             reads=RS(C.ident_f), writes=RS(C.ident_f))
        P.op("dve", lambda e: e.tensor_copy(C.ident_b.t[:], C.ident_f.t[:]), reads=RS(C.ident_f), writes=RS(C.ident_b))
        mf = S.sb([128, 4, 128], F32, "mf")
        P.op("pool", lambda e: e.memset(mf.t[:], 1.0), writes=RS(mf))
        P.op("pool", lambda e: e.affine_select(out=mf.t[:], in_=mf.t[:], pattern=[[0, 4], [-1, 128]],
                                               compare_op=ALU.is_ge, fill=0.0, base=0, channel_multiplier=1),
             reads=RS(mf), writes=RS(mf))
        P.op("dve", lambda e: e.tensor_copy(C.mprev.t[:], mf.t[:]), reads=RS(mf), writes=RS(C.mprev))
        mf2 = S.sb([128, 4, 128], F32, "mf2")
        P.op("pool", lambda e: e.memset(mf2.t[:], 1.0), writes=RS(mf2))
        P.op("pool", lambda e: e.affine_select(out=mf2.t[:], in_=mf2.t[:], pattern=[[0, 4], [1, 128]],
                                               compare_op=ALU.is_ge, fill=0.0, base=0, channel_multiplier=-1),
             reads=RS(mf2), writes=RS(mf2))
        P.op("dve", lambda e: e.tensor_copy(C.mnext.t[:], mf2.t[:]), reads=RS(mf2), writes=RS(C.mnext))
        for k, nm in enumerate(["diff_q_g", "diff_k_g", "swa_q_g", "swa_k_g"]):
            for l in range(C.n_layers):
                P.op("sp", lambda e, k=k, nm=nm, l=l: e.dma_start(out=C.gain.t[:, l, k, :], in_=W[nm][l:l + 1, :].to_broadcast([128, HD])),
                     writes=RS(C.gain), dma=True)
        for l in range(C.n_layers):
            P.op("sp", lambda e, l=l: e.dma_start(out=C.subln.t[:, l, :], in_=W["diff_subln_g"][l:l + 1, :].to_broadcast([128, 128])),
                 writes=RS(C.subln), dma=True)
        for l in range(C.n_layers):
            P.op("dve", lambda e, l=l: e.tensor_scalar(C.subln.t[:, l, :], C.subln.t[:, l, :], 1.0 - lam_init(l), None, op0=ALU.mult),
                 reads=RS(C.subln), writes=RS(C.subln))
        for l in range(C.n_layers):
            P.op("sp", lambda e, l=l: e.dma_start(out=C.esink.t[:, l, :], in_=W["swa_sink"][l:l + 1, :].to_broadcast([128, 16])),
                 writes=RS(C.esink), dma=True)
        P.op("act", lambda e: e.activation(out=C.esink.t[:], in_=C.esink.t[:], func=ACT.Exp), reads=RS(C.esink), writes=RS(C.esink))
        dl = S.sb([128, DEPTH, 4, HD], F32, "dl")
        for l in range(C.n_layers):
            P.op("sp", lambda e, l=l: e.dma_start(out=dl.t[:, l].rearrange("p a d -> p (a d)"), in_=W["diff_lambda"][l:l + 1, :].to_broadcast([128, 4 * HD])),
                 writes=RS(dl), dma=True)
        pr = S.sb([128, DEPTH, 2, HD], F32, "pr")
        P.op("dve", lambda e: e.tensor_tensor(out=pr.t[:], in0=dl.t[:, :, 0:4:2, :], in1=dl.t[:, :, 1:4:2, :], op=ALU.mult), reads=RS(dl), writes=RS(pr))
        sm = S.sb([128, DEPTH, 2], F32, "sm")
        P.op("dve", lambda e: e.tensor_reduce(out=sm.t[:], in_=pr.t[:], axis=AX.X, op=ALU.add), reads=RS(pr), writes=RS(sm))
        P.op("act", lambda e: e.activation(out=sm.t[:], in_=sm.t[:], func=ACT.Exp), reads=RS(sm), writes=RS(sm))
        P.op("dve", lambda e: e.tensor_tensor(out=C.nlam.t[:], in0=sm.t[:, :, 1], in1=sm.t[:, :, 0], op=ALU.subtract), reads=RS(sm), writes=RS(C.nlam))
        for l in range(C.n_layers):
            P.op("dve", lambda e, l=l: e.tensor_scalar(C.nlam.t[:, l:l + 1], C.nlam.t[:, l:l + 1], -lam_init(l), None, op0=ALU.add),
                 reads=RS(C.nlam), writes=RS(C.nlam))
        sc = S.sb([128, 8, 3], F32, "sc")
        P.op("sp", lambda e: e.dma_start(out=sc.t[:], in_=C.cT), writes=RS(sc), dma=True)
        P.op("act", lambda e: e.activation(out=sc.t[:], in_=sc.t[:], func=ACT.Silu), reads=RS(sc), writes=RS(sc))
        wa = [S.sb([128, 3072], F32, f"wa{i}") for i in range(2)]
        pm = [S.ps([3, 512], F32, f"pm{i}") for i in range(6)]
        msb = S.sb([3, 6 * D], F32, "msb")
        bsb = S.sb([3, 6 * D], F32, "bsb")
        gsb = S.sb([3, 2, D], F32, "gsb")
        it = 0
        for l in range(C.n_layers):
            P.op("sp", lambda e, l=l: e.dma_start(out=bsb.t[:], in_=W["b_ada"][l:l + 1, :].to_broadcast([3, 6 * D])), writes=RS(bsb), dma=True)
            P.op("sp", lambda e, l=l: e.dma_start(out=gsb.t[:, 0, :], in_=W["norm1_g"][l:l + 1, :].to_broadcast([3, D])), writes=RS(gsb), dma=True)
            P.op("sp", lambda e, l=l: e.dma_start(out=gsb.t[:, 1, :], in_=W["norm2_g"][l:l + 1, :].to_broadcast([3, D])), writes=RS(gsb), dma=True)
            for half in range(2):
                for j in range(8):
                    w = wa[it % 2]
                    it += 1
                    P.op("sp", lambda e, w=w, l=l, j=j, half=half: e.dma_start(out=w.t[:], in_=W["w_ada"][l, j * 128:(j + 1) * 128, half * 3072:(half + 1) * 3072]),
                         writes=RS(w), dma=True)
                    for n in range(6):
                        P.op("pe", lambda e, w=w, j=j, n=n: e.matmul(pm[n].t[:], sc.t[:, j, :], w.t[:, n * 512:(n + 1) * 512], start=(j == 0), stop=(j == 7)),
                             reads=RS(sc, w), writes=RS(pm[n]))
                for n in range(6):
                    c0 = half * 3072 + n * 512
                    P.op("dve", lambda e, n=n, c0=c0: e.tensor_tensor(out=msb.t[:, c0:c0 + 512], in0=pm[n].t[:], in1=bsb.t[:, c0:c0 + 512], op=ALU.add),
                         reads=RS(pm[n], bsb), writes=RS(msb))
            for k, c0 in enumerate((D, 4 * D)):
                P.op("dve", lambda e, k=k, c0=c0: e.scalar_tensor_tensor(out=msb.t[:, c0:c0 + D], in0=msb.t[:, c0:c0 + D], scalar=1.0, in1=gsb.t[:, k, :], op0=ALU.add, op1=ALU.mult),
                     reads=RS(msb, gsb), writes=RS(msb))
            P.op("sp", lambda e, l=l: e.dma_start(out=C.MODV[l], in_=msb.t[:]), reads=RS(msb), dma=True)


def modrow(C, l, r, k):
    return C.MODV[l, r:r + 1, k * D:(k + 1) * D]


def stage_inproj(C, l):
    nc, P, W = C.nc, C.P, C.W
    with Stage(C, f"s1_{l}") as S:
        win = S.sb([128, 8, IN_W], BF16, "win")
        winr = [Res(f"win{j}") for j in range(8)]
        for j in range(0 if not DBG.get("no_win") else 8, 8):
            for h in range(2):
                P.op("pool", lambda e, j=j, h=h: e.dma_start(out=win.t[:, j, h * 3328:(h + 1) * 3328], in_=W["w_in"][l, j * 128:(j + 1) * 128, h * 3328:(h + 1) * 3328]),
                     writes=[winr[j]], dma=True)
        BG = S.sb([128, 2 * D], BF16, "BG")
        P.op("pool", lambda e: e.dma_start(out=BG.t[:], in_=W["b_gate"][l:l + 1, :].to_broadcast([128, 2 * D])), writes=RS(BG), dma=True)
        G1 = S.sb([128, D], F32, "G1")
        SH1 = S.sb([128, D], F32, "SH1")
        xt = [S.sb([128, D], F32, f"xt{i}") for i in range(2)]
        junk = S.sb([128, QKW], F32, "junk")
        ssq = S.sb([128, 1], F32, "ssq")
        rstd = S.sb([128, 1], F32, "rstd")
        hbs = [S.sb([128, D], BF16, f"hb{i}") for i in range(2)]
        hTs = [S.sb([128, 8, 128], BF16, f"hT{i}") for i in range(2)]
        QF = S.sb([128, QKW], F32, "QF")
        QB = S.sb([128, QKW], BF16, "QB")
        QT = S.sb([128, 26, 128], BF16, "QT")
        vout = S.sb([128, 1280], BF16, "vout")
        gout = S.sb([128, 2 * D], BF16, "gout")
        gtmp = [S.sb([128, 512], F32, f"gtmp{i}") for i in range(2)]
        ss = S.sb([128, 52], F32, "ss")
        rs = S.sb([128, 52], F32, "rs")
        rp = S.sb([128, 96], F32, "rp")
        pT = S.ps([128, 8, 128], BF16, "pT")
        pacc = [S.ps([128, 512], F32, f"pacc{i}") for i in range(5)]
        pQT = [S.ps([128, 8, 128], BF16, f"pQT{i}") for i in range(2)]
        gain = C.gain
        sqj = S.sb([128, D], F32, "sqj")
        rps = [S.sb([128, 96], F32, f"rp{i}") for i in range(2)]
        state = {"ipa": 0, "seg": None}
        tl_all = []
        for b in range(NB):
            for seg in range(2):
                tiles = list(range(TL) if seg == 0 else range(TL, TT))
                if "s1_tiles" in DBG:
                    tiles = tiles[:DBG["s1_tiles"]]
                tl_all += [(b, seg, t) for t in tiles]

        def pre(i):
            b, seg, t = tl_all[i]
            x, hb, hT, rp = xt[i % 2], hbs[i % 2], hTs[i % 2], rps[i % 2]
            if state["seg"] != (b, seg):
                state["seg"] = (b, seg)
                r = b if seg == 0 else 2
                P.op("sp", lambda e, r=r: e.dma_start(out=G1.t[:], in_=modrow(C, l, r, 1).to_broadcast([128, D])), writes=RS(G1), dma=True)
                P.op("sp", lambda e, r=r: e.dma_start(out=SH1.t[:], in_=modrow(C, l, r, 0).to_broadcast([128, D])), writes=RS(SH1), dma=True)
            P.op("sp", lambda e, x=x, b=b, t=t: e.dma_start(out=x.t[:], in_=xres(C, b, t)), writes=RS(x), dma=True)
            if seg == 0:
                P.op("sp", lambda e, t=t, rp=rp: e.dma_start(out=rp.t[:], in_=C.rope[t * 128:(t + 1) * 128, :]), writes=RS(rp), dma=True)
            P.op("act", lambda e, x=x: e.activation(out=sqj.t[:], in_=x.t[:], func=ACT.Square, accum_out=ssq.t[:]), reads=RS(x), writes=RS(sqj, ssq))
            P.op("act", lambda e: e.activation(out=rstd.t[:], in_=ssq.t[:], func=ACT.Sqrt, scale=1.0 / D, bias=EPS), reads=RS(ssq), writes=RS(rstd))
            P.op("dve", lambda e: e.reciprocal(rstd.t[:], rstd.t[:]), reads=RS(rstd), writes=RS(rstd))
            P.op("dve", lambda e, x=x: e.scalar_tensor_tensor(out=x.t[:], in0=x.t[:], scalar=rstd.t[:, 0:1], in1=G1.t[:], op0=ALU.mult, op1=ALU.mult),
                 reads=RS(x, rstd, G1), writes=RS(x))
            P.op("dve", lambda e, x=x, hb=hb: e.tensor_tensor(out=hb.t[:], in0=x.t[:], in1=SH1.t[:], op=ALU.add), reads=RS(x, SH1), writes=RS(hb))
            for j in range(8):
                P.op("pe", lambda e, j=j, hb=hb: e.transpose(pT.t[:, j, :], hb.t[:, j * 128:(j + 1) * 128], C.ident_b.t[:]), reads=RS(hb, C.ident_b), writes=RS(pT))
            P.op("act", lambda e, hT=hT: e.copy(hT.t[:], pT.t[:]), reads=RS(pT), writes=RS(hT))

        def mm(i, chunks):
            hT = hTs[i % 2]
            for n in chunks:
                pa = pacc[state["ipa"] % 5]
                state["ipa"] += 1
                for j in range(8):
                    P.op("pe", lambda e, pa=pa, j=j, n=n, hT=hT: e.matmul(pa.t[:], hT.t[:, j, :], win.t[:, j, n * 512:(n + 1) * 512], start=(j == 0), stop=(j == 7)),
                         reads=[hT.r, winr[j]], writes=RS(pa))
                if n in (4, 5):
                    P.op("act", lambda e, pa=pa, n=n: e.copy(vout.t[:, (n - 4) * 512:(n - 3) * 512], pa.t[:]), reads=RS(pa), writes=RS(vout))
                elif n >= 9:
                    gt_ = gtmp[n % 2]
                    c0 = (n - 9) * 512
                    P.op("dve", lambda e, pa=pa, gt_=gt_, c0=c0: e.tensor_tensor(out=gt_.t[:], in0=pa.t[:], in1=BG.t[:, c0:c0 + 512], op=ALU.add),
                         reads=RS(pa, BG), writes=RS(gt_))
                    P.op("act", lambda e, gt_=gt_, c0=c0: e.activation(out=gout.t[:, c0:c0 + 512], in_=gt_.t[:], func=ACT.Sigmoid), reads=RS(gt_), writes=RS(gout))
                elif n <= 3:
                    P.op("act", lambda e, pa=pa, n=n: e.copy(QF.t[:, n * 512:(n + 1) * 512], pa.t[:]), reads=RS(pa), writes=RS(QF))
                elif n in (6, 7):
                    P.op("dve", lambda e, pa=pa, n=n: e.tensor_copy(QF.t[:, 2048 + (n - 6) * 512:2048 + (n - 5) * 512], pa.t[:]), reads=RS(pa), writes=RS(QF))
                else:
                    P.op("act", lambda e, pa=pa: e.copy(QF.t[:, 3072:3328], pa.t[:, 0:256]), reads=RS(pa), writes=RS(QF))
                    P.op("act", lambda e, pa=pa: e.copy(vout.t[:, 1024:1280], pa.t[:, 256:512]), reads=RS(pa), writes=RS(vout))

        def store_vg(i):
            b, seg, t = tl_all[i]
            tok0 = t * 128
            P.op("sp", lambda e, b=b, tok0=tok0: e.dma_start(out=C.VA[b, tok0:tok0 + 128, :], in_=vout.t[:, 0:D]), reads=RS(vout), dma=True)
            P.op("sp", lambda e, b=b, tok0=tok0: e.dma_start(out=C.VB[b, tok0:tok0 + 128, :], in_=vout.t[:, D:1280]), reads=RS(vout), dma=True)
            P.op("sp", lambda e, b=b, tok0=tok0: e.dma_start(out=C.GT[b, tok0:tok0 + 128, :], in_=gout.t[:]), reads=RS(gout), dma=True)

        def post_elem(i):
            b, seg, t = tl_all[i]
            rp = rps[i % 2]
            P.op("act", lambda e: e.activation(out=junk.t[:], in_=QF.t[:], func=ACT.Square), reads=RS(QF), writes=RS(junk))
            P.op("dve", lambda e: e.tensor_reduce(out=ss.t[:], in_=junk.t[:].rearrange("p (g d) -> p g d", d=HD), axis=AX.X, op=ALU.add),
                 reads=RS(junk), writes=RS(ss))
            P.op("act", lambda e: e.activation(out=rs.t[:], in_=ss.t[:], func=ACT.Sqrt, scale=1.0 / HD, bias=EPS), reads=RS(ss), writes=RS(rs))
            P.op("dve", lambda e: e.reciprocal(rs.t[:], rs.t[:]), reads=RS(rs), writes=RS(rs))
            QF3 = QF.t[:].rearrange("p (g d) -> p g d", d=HD)
            P.op("pool", lambda e, QF3=QF3: e.tensor_tensor(out=QF3, in0=QF3, in1=bc_last(rs.t[:], HD), op=ALU.mult), reads=RS(QF, rs), writes=RS(QF))
            for k, (g0, ng) in enumerate(((0, 16), (16, 16), (32, 16), (48, 4))):
                P.op("pool", lambda e, QF3=QF3, k=k, g0=g0, ng=ng: e.tensor_tensor(out=QF3[:, g0:g0 + ng, :], in0=QF3[:, g0:g0 + ng, :], in1=bc_mid(gain.t[:, l, k, :], ng), op=ALU.mult),
                     reads=RS(QF, gain), writes=RS(QF))
            if seg == 0:
                QF5 = QF.t[:].rearrange("p (g a r i) -> p g a r i", a=2, r=2, i=16)
                J5 = junk.t[:].rearrange("p (g a r i) -> p g a r i", a=2, r=2, i=16)
                QB5 = QB.t[:].rearrange("p (g a r i) -> p g a r i", a=2, r=2, i=16)
                SN = rp.t[:, 64:96].rearrange("p (o a i) -> p o a i", o=1, a=2).to_broadcast([128, 52, 2, 16])
                P.op("pool", lambda e, QF5=QF5, J5=J5, SN=SN: e.tensor_tensor(out=J5[:, :, :, 0, :], in0=QF5[:, :, :, 1, :], in1=SN, op=ALU.mult),
                     reads=RS(QF, rp), writes=RS(junk))
                P.op("pool", lambda e, QF5=QF5, J5=J5, SN=SN: e.tensor_tensor(out=J5[:, :, :, 1, :], in0=QF5[:, :, :, 0, :], in1=SN, op=ALU.mult),
                     reads=RS(QF, rp), writes=RS(junk))
                P.op("dve", lambda e, QF3=QF3, rp=rp: e.tensor_tensor(out=QF3, in0=QF3, in1=bc_mid(rp.t[:, 0:64], 52), op=ALU.mult), reads=RS(QF, rp), writes=RS(QF))
                P.op("dve", lambda e, QF5=QF5, J5=J5, QB5=QB5: e.tensor_tensor(out=QB5[:, :, :, 0, :], in0=QF5[:, :, :, 0, :], in1=J5[:, :, :, 0, :], op=ALU.subtract),
                     reads=RS(QF, junk), writes=RS(QB))
                P.op("dve", lambda e, QF5=QF5, J5=J5, QB5=QB5: e.tensor_tensor(out=QB5[:, :, :, 1, :], in0=QF5[:, :, :, 1, :], in1=J5[:, :, :, 1, :], op=ALU.add),
                     reads=RS(QF, junk), writes=RS(QB))
            else:
                P.op("dve", lambda e: e.tensor_copy(QB.t[:], QF.t[:]), reads=RS(QF), writes=RS(QB))

        def post_pe(i):
            b, seg, t = tl_all[i]
            for jb in range(4):
                pq = pQT[jb % 2]
                nj = 8 if jb < 3 else 2
                for jj in range(nj):
                    j = jb * 8 + jj
                    P.op("pe", lambda e, pq=pq, jj=jj, j=j: e.transpose(pq.t[:, jj, :], QB.t[:, j * 128:(j + 1) * 128], C.ident_b.t[:]), reads=RS(QB, C.ident_b), writes=RS(pq))
                if jb % 2 == 0:
                    P.op("act", lambda e, pq=pq, jb=jb, nj=nj: e.copy(QT.t[:, jb * 8:jb * 8 + nj, :], pq.t[:, 0:nj, :]), reads=RS(pq), writes=RS(QT))
                else:
                    P.op("dve", lambda e, pq=pq, jb=jb, nj=nj: e.tensor_copy(QT.t[:, jb * 8:jb * 8 + nj, :], pq.t[:, 0:nj, :]), reads=RS(pq), writes=RS(QT))
            tok0 = t * 128
            for jq in range(0, 26, 2):
                P.op("sp", lambda e, b=b, tok0=tok0, jq=jq: e.dma_start(out=C.QKT[b, jq * 128:(jq + 2) * 128, tok0:tok0 + 128].rearrange("(j p) n -> p j n", p=128), in_=QT.t[:, jq:jq + 2, :]), reads=RS(QT), dma=True)

        CH_A = (4, 5, 9, 10, 11, 12)
        CH_B = (0, 1, 2, 3, 6, 7, 8)
        n_t = len(tl_all)
        pre(0)
        mm(0, CH_A)
        mm(0, CH_B)
        store_vg(0)
        for i in range(n_t):
            post_elem(i)
            if i + 1 < n_t:
                pre(i + 1)
                mm(i + 1, CH_A)
            post_pe(i)
            if i + 1 < n_t:
                mm(i + 1, CH_B)
                store_vg(i + 1)


def stage_diff(C, l):
    nc, P = C.nc, C.P
    last = (l == DEPTH - 1)
    with Stage(C, f"s2_{l}") as S:
        KT = [S.sb([128, NT], BF16, f"KT{i}") for i in range(2)]
        QT = [[S.sb([128, NT], BF16, f"QZ{i}_{c}") for c in range(2)] for i in range(2)]
        V1 = [S.sb([128, TT, 129], BF16, f"V1{i}") for i in range(2)]
        PT = [S.sb([128, 512], BF16, f"PT{i}") for i in range(3)]
        o0 = S.sb([128, 128], F32, "o0")
        av = S.sb([128, 128], F32, "av")
        junk = S.sb([128, 128], F32, "junk")
        rec = S.sb([128, 2], F32, "rec")
        s1 = S.sb([128, 1], F32, "s1")
        ssq = S.sb([128, 1], F32, "ssq")
        rstd = S.sb([128, 1], F32, "rstd")
        yo = [S.sb([128, 4, 128], BF16, f"yo{i}") for i in range(2)]
        NPS = 4
        LOOK = 3
        ps = [S.ps([128, 512], F32, f"ps{i}") for i in range(NPS)]
        accb = [S.ps([128, 3, 129], F32, f"acc_{i}") for i in range(3)]
        accS = [[S.sb([128, 3, 129], F32, f"accS{k}_{i}") for i in range(3)] for k in range(2)]
        for v in V1:
            P.op("pool", lambda e, v=v: e.memset(v.t[:, :, 128:129], 1.0), writes=RS(v))
        for qz in QT:
            P.op("pool", lambda e, qz=qz: e.memset(qz[0].t[64:128, :], 0.0), writes=RS(qz[0]))
            P.op("dve", lambda e, qz=qz: e.memset(qz[1].t[0:64, :], 0.0), writes=RS(qz[1]))

        def acc_ap(accb, c, qs):
            a = c * 4 + qs
            return accb[a // 3], accb[a // 3].t[:, a % 3, :]

        ih = 0
        iyo = 0
        ich = 0
        for b in range(NB):
            for h in range(8):
                kt_, qt_, v1_ = KT[ih % 2], QT[ih % 2], V1[ih % 2]
                ih += 1
                P.op("sp", lambda e, kt_=kt_, b=b, h=h: e.dma_start(out=kt_.t[:], in_=C.QKT[b, 1024 + h * 128:1024 + (h + 1) * 128, :]), writes=RS(kt_), dma=True)
                P.op("sp", lambda e, qt_=qt_, b=b, h=h: e.dma_start(out=qt_[0].t[0:64, :], in_=C.QKT[b, h * 128:h * 128 + 64, :]), writes=RS(qt_[0]), dma=True)
                P.op("sp", lambda e, qt_=qt_, b=b, h=h: e.dma_start(out=qt_[1].t[64:128, :], in_=C.QKT[b, h * 128 + 64:(h + 1) * 128, :]), writes=RS(qt_[1]), dma=True)
                vsrc = C.VA[b].rearrange("(kt p) c -> p kt c", p=128)
                for k0 in range(0, TT, 6):
                    k1 = min(TT, k0 + 6)
                    P.op("sp", lambda e, v1_=v1_, k0=k0, k1=k1, h=h, vsrc=vsrc: e.dma_start(out=v1_.t[:, k0:k1, 0:128], in_=vsrc[:, k0:k1, h * 128:(h + 1) * 128]),
                         writes=RS(v1_), dma=True)
                chunks = [(qc * 512, 4, list(range(TT))) for qc in range(8)]
                if not last:
                    chunks.append((NL, 2, [TL, TL + 1]))
                if "s2_chunks" in DBG:
                    chunks = chunks[:DBG["s2_chunks"]] + chunks[8:]
                for (q0, nqs, kts) in chunks:
                    nq = nqs * 128
                    accs_ = accS[ich % 2]
                    ich += 1
                    started = set()
                    units = [(c, ki, kt) for c in range(2) for ki, kt in enumerate(kts)]

                    def qk(i, units=units, kt_=kt_, qt_=qt_, q0=q0, nq=nq):
                        c, ki, kt = units[i]
                        p_ = ps[i % NPS]
                        P.op("pe", lambda e, p_=p_, c=c, kt=kt: e.matmul(
                            p_.t[:, 0:nq], kt_.t[:, kt * 128:(kt + 1) * 128], qt_[c].t[:, q0:q0 + nq], start=True, stop=True),
                            reads=RS(kt_, qt_[c]), writes=RS(p_))

                    for i0 in range(min(LOOK, len(units))):
                        qk(i0)
                    for i, (c, ki, kt) in enumerate(units):
                        p_ = ps[i % NPS]
                        pt_ = PT[i % 3]
                        if i + LOOK < len(units):
                            qk(i + LOOK)
                        P.op("act", lambda e, p_=p_, pt_=pt_, nq=nq: e.activation(out=pt_.t[:, 0:nq], in_=p_.t[:, 0:nq], func=ACT.Exp, scale=0.125),
                             reads=RS(p_), writes=RS(pt_))
                        for qs in range(nqs):
                            at, aap = acc_ap(accb, c, qs)
                            st = (ki == 0) and (id(at) not in started)
                            started.add(id(at))
                            P.op("pe", lambda e, aap=aap, pt_=pt_, v1_=v1_, qs=qs, kt=kt, st=st: e.matmul(
                                aap, pt_.t[:, qs * 128:(qs + 1) * 128], v1_.t[:, kt, :], start=st, stop=False, skip_group_check=True),
                                reads=RS(pt_, v1_), writes=RS(at))
                    for k3 in range(3):
                        P.op("dve", lambda e, k3=k3, accs_=accs_: e.tensor_copy(accs_[k3].t[:], accb[k3].t[:]), reads=RS(accb[k3]), writes=RS(accs_[k3]))
                    y_ = yo[iyo % 2]
                    iyo += 1
                    for qs in range(nqs):
                        a0t, a0 = acc_ap(accs_, 0, qs)
                        a1t, a1 = acc_ap(accs_, 1, qs)
                        P.op("dve", lambda e, a0=a0: e.reciprocal(rec.t[:, 0:1], a0[:, 128:129]), reads=RS(a0t), writes=RS(rec))
                        P.op("dve", lambda e, a1=a1: e.reciprocal(rec.t[:, 1:2], a1[:, 128:129]), reads=RS(a1t), writes=RS(rec))
                        P.op("dve", lambda e: e.tensor_tensor(out=s1.t[:], in0=rec.t[:, 1:2], in1=C.nlam.t[:, l:l + 1], op=ALU.mult), reads=RS(rec, C.nlam), writes=RS(s1))
                        P.op("dve", lambda e, a0=a0: e.tensor_scalar(o0.t[:], a0[:, 0:128], rec.t[:, 0:1], None, op0=ALU.mult), reads=RS(a0t, rec), writes=RS(o0))
                        P.op("dve", lambda e, a1=a1: e.scalar_tensor_tensor(out=av.t[:], in0=a1[:, 0:128], scalar=s1.t[:, 0:1], in1=o0.t[:], op0=ALU.mult, op1=ALU.add),
                             reads=RS(a1t, s1, o0), writes=RS(av))
                        P.op("pool", lambda e: e.tensor_tensor(out=junk.t[:], in0=av.t[:], in1=av.t[:], op=ALU.mult), reads=RS(av), writes=RS(junk))
                        P.op("dve", lambda e: e.tensor_reduce(out=ssq.t[:], in_=junk.t[:], axis=AX.X, op=ALU.add), reads=RS(junk), writes=RS(ssq))
                        P.op("act", lambda e: e.activation(out=rstd.t[:], in_=ssq.t[:], func=ACT.Ln, scale=1.0 / 128, bias=EPS), reads=RS(ssq), writes=RS(rstd))
                        P.op("act", lambda e: e.activation(out=rstd.t[:], in_=rstd.t[:], func=ACT.Exp, scale=-0.5), reads=RS(rstd), writes=RS(rstd))
                        P.op("dve", lambda e, y_=y_, qs=qs: e.scalar_tensor_tensor(out=y_.t[:, qs, :], in0=av.t[:], scalar=rstd.t[:, 0:1], in1=C.subln.t[:, l, :], op0=ALU.mult, op1=ALU.mult),
                             reads=RS(av, rstd, C.subln), writes=RS(y_))
                    P.op("sp", lambda e, y_=y_, b=b, q0=q0, nqs=nqs, nq=nq, h=h: e.dma_start(
                        out=C.YA[b, q0:q0 + nq, h * 128:(h + 1) * 128].rearrange("(s p) c -> p s c", p=128), in_=y_.t[:, 0:nqs, :]), reads=RS(y_), dma=True)


def stage_swa(C, l):
    nc, P = C.nc, C.P
    last = (l == DEPTH - 1)
    with Stage(C, f"s3_{l}") as S:
        KT = [S.sb([64, NT], BF16, f"KT{i}") for i in range(2)]
        QT = [S.sb([64, 4, NT], BF16, f"QT{i}") for i in range(2)]
        V1 = [S.sb([128, TT, 65], BF16, f"V1{i}") for i in range(2)]
        PT = [S.sb([128, 4, 128], BF16, f"PT{i}") for i in range(3)]
        den = S.sb([128, 4], F32, "den")
        yo = [S.sb([128, 4, 64], BF16, f"yo{i}") for i in range(2)]
        ps = [S.ps([128, 512], F32, f"ps{i}") for i in range(3)]
        acc = [S.ps([128, 4, 65], F32, f"acc{i}") for i in range(2)]
        for v in V1:
            P.op("pool", lambda e, v=v: e.memset(v.t[:, :, 64:65], 1.0), writes=RS(v))
        ig = 0
        ips = 0
        ipt = 0
        iacc = 0
        iyo = 0
        for b in range(NB):
            for g in range(4):
                kt_, qt_, v1_ = KT[ig % 2], QT[ig % 2], V1[ig % 2]
                ig += 1
                P.op("sp", lambda e, kt_=kt_, b=b, g=g: e.dma_start(out=kt_.t[:], in_=C.QKT[b, 3072 + g * 64:3072 + (g + 1) * 64, :]), writes=RS(kt_), dma=True)
                P.op("sp", lambda e, qt_=qt_, b=b, g=g: e.dma_start(out=qt_.t[:], in_=C.QKT[b, 2048 + g * 256:2048 + (g + 1) * 256, :].rearrange("(i d) n -> d i n", d=64)),
                     writes=RS(qt_), dma=True)
                vsrc = C.VB[b].rearrange("(kt p) c -> p kt c", p=128)
                for k0 in range(0, TT, 6):
                    k1 = min(TT, k0 + 6)
                    P.op("sp", lambda e, v1_=v1_, k0=k0, k1=k1, g=g, vsrc=vsrc: e.dma_start(out=v1_.t[:, k0:k1, 0:64], in_=vsrc[:, k0:k1, g * 64:(g + 1) * 64]),
                         writes=RS(v1_), dma=True)
                blocks = list(range(TL)) + ([] if last else [TL, TL + 1])
                if "s3_blocks" in DBG:
                    blocks = blocks[:DBG["s3_blocks"]] + blocks[TL:]
                for n in blocks:
                    if n < TL:
                        kts = ([(n - 1, C.mprev)] if n > 0 else []) + [(n, None)] + ([(n + 1, C.mnext)] if n < TL - 1 else []) + [(TL, None), (TL + 1, None)]
                    else:
                        kts = [(TL, None), (TL + 1, None)]
                    a_ = acc[iacc % 2]
                    iacc += 1

                    def qk(i, kts=kts, kt_=kt_, qt_=qt_, n=n, base=ips):
                        kt = kts[i][0]
                        p_ = ps[(base + i) % 3]
                        P.op("pe", lambda e, p_=p_, kt=kt: e.matmul(
                            p_.t[:].rearrange("p (i q) -> p i q", i=4), kt_.t[:, kt * 128:(kt + 1) * 128], qt_.t[:, :, n * 128:(n + 1) * 128], start=True, stop=True),
                            reads=RS(kt_, qt_), writes=RS(p_))

                    qk(0)
                    qk(1)
                    for ki, (kt, mask) in enumerate(kts):
                        p_ = ps[ips % 3]
                        ips += 1
                        pt_ = PT[ipt % 3]
                        ipt += 1
                        if ki + 2 < len(kts):
                            qk(ki + 2)
                        P.op("act", lambda e, p_=p_, pt_=pt_: e.activation(out=pt_.t[:].rearrange("p i q -> p (i q)"), in_=p_.t[:], func=ACT.Exp, scale=0.125),
                             reads=RS(p_), writes=RS(pt_))
                        if mask is not None:
                            P.op("pool", lambda e, pt_=pt_, mask=mask: e.tensor_tensor(out=pt_.t[:], in0=pt_.t[:], in1=mask.t[:], op=ALU.mult), reads=RS(pt_, mask), writes=RS(pt_))
                        for i in range(4):
                            P.op("pe", lambda e, a_=a_, pt_=pt_, v1_=v1_, i=i, kt=kt, ki=ki: e.matmul(
                                a_.t[:, i, :], pt_.t[:, i, :], v1_.t[:, kt, :], start=(ki == 0 and i == 0), stop=False, skip_group_check=True),
                                reads=RS(pt_, v1_), writes=RS(a_))
                    y_ = yo[iyo % 2]
                    iyo += 1
                    P.op("dve", lambda e, a_=a_, g=g: e.tensor_tensor(out=den.t[:], in0=a_.t[:, :, 64], in1=C.esink.t[:, l, 4 * g:4 * g + 4], op=ALU.add), reads=RS(a_, C.esink), writes=RS(den))
                    P.op("dve", lambda e: e.reciprocal(den.t[:], den.t[:]), reads=RS(den), writes=RS(den))
                    P.op("dve", lambda e, a_=a_, y_=y_: e.tensor_tensor(out=y_.t[:], in0=a_.t[:, :, 0:64], in1=bc_last(den.t[:], 64), op=ALU.mult), reads=RS(a_, den), writes=RS(y_))
                    P.op("sp", lambda e, y_=y_, b=b, n=n, g=g: e.dma_start(out=C.YB[b, n * 128:(n + 1) * 128, g * 256:(g + 1) * 256], in_=y_.t[:].rearrange("p i d -> p (i d)")),
                         reads=RS(y_), dma=True)


def stage_merge(C, l):
    nc, P, W = C.nc, C.P, C.W
    last = (l == DEPTH - 1)
    with Stage(C, f"s4_{l}") as S:
        wts = {}
        for nm in ("w_branch_a", "w_branch_b", "w_out"):
            wt = S.sb([128, 8, D], BF16, nm)
            for j0 in (0, 4):
                P.op("pool", lambda e, wt=wt, nm=nm, j0=j0: e.dma_start(out=wt.t[:, j0:j0 + 4, :], in_=W[nm][l, j0 * 128:(j0 + 4) * 128, :].rearrange("(j p) n -> p j n", p=128)),
                     writes=RS(wt), dma=True)
            wts[nm] = wt
        wa, wb, wo = wts["w_branch_a"], wts["w_branch_b"], wts["w_out"]
        wr = S.sb([128, 8, NE], F32, "wr")
        P.op("sp", lambda e: e.dma_start(out=wr.t[:], in_=W["w_router"][l].rearrange("(j p) n -> p j n", p=128)), writes=RS(wr), dma=True)
        GATE1 = S.sb([128, D], F32, "GATE1")
        G2 = S.sb([128, D], F32, "G2")
        SH2 = S.sb([128, D], F32, "SH2")
        ya = [S.sb([128, D], BF16, f"ya{i}") for i in range(2)]
        yb = [S.sb([128, D], BF16, f"yb{i}") for i in range(2)]
        gt = [S.sb([128, 2 * D], BF16, f"gt{i}") for i in range(2)]
        xt = [S.sb([128, D], F32, f"xt{i}") for i in range(2)]
        yaT = S.sb([128, 8, 128], BF16, "yaT")
        ybT = S.sb([128, 8, 128], BF16, "ybT")
        msum = S.sb([128, D], F32, "msum")
        tmp = [S.sb([128, 512], F32, f"tmp{i}") for i in range(2)]
        mb = S.sb([128, D], BF16, "mb")
        mT = S.sb([128, 8, 128], BF16, "mT")
        xn = S.sb([128, D], F32, "xn")
        junk = S.sb([128, D], F32, "junk")
        h2f = S.sb([128, D], F32, "h2f")
        h2b = S.sb([128, D], BF16, "h2b")
        h2T = S.sb([128, 8, 128], F32, "h2T")
        ssq = S.sb([128, 1], F32, "ssq")
        rstd = S.sb([128, 1], F32, "rstd")
        mx = S.sb([128, 1], F32, "mx")
        se = S.sb([128, 1], F32, "se")
        ex = S.sb([128, NE], F32, "ex")
        aff = S.sb([128, NE], F32, "aff")
        affT = S.sb([NE, 128], F32, "affT")
        pT = [S.ps([128, 8, 128], BF16, f"pT{i}") for i in range(2)]
        pacc = [S.ps([128, 512], F32, f"pacc{i}") for i in range(2)]
        pTf = [S.ps([128, 4, 128], F32, f"pTf{i}") for i in range(2)]
        plog = S.ps([128, NE], F32, "plog")
        paT = S.ps([NE, 128], F32, "paT")
        it = 0
        for b in range(NB):
            for seg in range(2):
                if seg == 1 and last:
                    continue
                r = b if seg == 0 else 2
                for tl, k in ((GATE1, 2), (G2, 4), (SH2, 3)):
                    P.op("sp", lambda e, tl=tl, r=r, k=k: e.dma_start(out=tl.t[:], in_=modrow(C, l, r, k).to_broadcast([128, D])), writes=RS(tl), dma=True)
                tiles = list(range(TL) if seg == 0 else range(TL, TT))
                if "s4_tiles" in DBG:
                    tiles = tiles[:DBG["s4_tiles"]]
                for t in tiles:
                    tok0 = t * 128
                    ya_, yb_, gt_, x_ = ya[it % 2], yb[it % 2], gt[it % 2], xt[it % 2]
                    it += 1
                    P.op("sp", lambda e, ya_=ya_, b=b, tok0=tok0: e.dma_start(out=ya_.t[:], in_=C.YA[b, tok0:tok0 + 128, :]), writes=RS(ya_), dma=True)
                    P.op("sp", lambda e, yb_=yb_, b=b, tok0=tok0: e.dma_start(out=yb_.t[:], in_=C.YB[b, tok0:tok0 + 128, :]), writes=RS(yb_), dma=True)
                    P.op("sp", lambda e, gt_=gt_, b=b, tok0=tok0: e.dma_start(out=gt_.t[:], in_=C.GT[b, tok0:tok0 + 128, :]), writes=RS(gt_), dma=True)
                    P.op("sp", lambda e, x_=x_, b=b, t=t: e.dma_start(out=x_.t[:], in_=xres(C, b, t)), writes=RS(x_), dma=True)
                    for j in range(8):
                        P.op("pe", lambda e, j=j, ya_=ya_: e.transpose(pT[0].t[:, j, :], ya_.t[:, j * 128:(j + 1) * 128], C.ident_b.t[:]), reads=RS(ya_, C.ident_b), writes=RS(pT[0]))
                    P.op("act", lambda e: e.copy(yaT.t[:], pT[0].t[:]), reads=RS(pT[0]), writes=RS(yaT))
                    for j in range(8):
                        P.op("pe", lambda e, j=j, yb_=yb_: e.transpose(pT[1].t[:, j, :], yb_.t[:, j * 128:(j + 1) * 128], C.ident_b.t[:]), reads=RS(yb_, C.ident_b), writes=RS(pT[1]))
                    P.op("dve", lambda e: e.tensor_copy(ybT.t[:], pT[1].t[:]), reads=RS(pT[1]), writes=RS(ybT))
                    for n in range(2):
                        cs = slice(n * 512, (n + 1) * 512)
                        for j in range(8):
                            P.op("pe", lambda e, j=j, cs=cs: e.matmul(pacc[0].t[:], yaT.t[:, j, :], wa.t[:, j, cs], start=(j == 0), stop=(j == 7)), reads=RS(yaT, wa), writes=RS(pacc[0]))
                        P.op("dve", lambda e, cs=cs, gt_=gt_: e.tensor_tensor(out=msum.t[:, cs], in0=pacc[0].t[:], in1=gt_.t[:, cs], op=ALU.mult), reads=RS(pacc[0], gt_), writes=RS(msum))
                        for j in range(8):
                            P.op("pe", lambda e, j=j, cs=cs: e.matmul(pacc[1].t[:], ybT.t[:, j, :], wb.t[:, j, cs], start=(j == 0), stop=(j == 7)), reads=RS(ybT, wb), writes=RS(pacc[1]))
                        tm = tmp[n]
                        P.op("dve", lambda e, n=n, tm=tm, gt_=gt_: e.tensor_tensor(out=tm.t[:], in0=pacc[1].t[:], in1=gt_.t[:, D + n * 512:D + (n + 1) * 512], op=ALU.mult), reads=RS(pacc[1], gt_), writes=RS(tm))
                        P.op("pool", lambda e, cs=cs, tm=tm: e.tensor_tensor(out=mb.t[:, cs], in0=msum.t[:, cs], in1=tm.t[:], op=ALU.add), reads=RS(msum, tm), writes=RS(mb))
                    for j in range(8):
                        P.op("pe", lambda e, j=j: e.transpose(pT[0].t[:, j, :], mb.t[:, j * 128:(j + 1) * 128], C.ident_b.t[:]), reads=RS(mb, C.ident_b), writes=RS(pT[0]))
                    P.op("act", lambda e: e.copy(mT.t[:], pT[0].t[:]), reads=RS(pT[0]), writes=RS(mT))
                    for n in range(2):
                        cs = slice(n * 512, (n + 1) * 512)
                        pa = pacc[n]
                        tm = tmp[n]
                        for j in range(8):
                            P.op("pe", lambda e, j=j, cs=cs, pa=pa: e.matmul(pa.t[:], mT.t[:, j, :], wo.t[:, j, cs], start=(j == 0), stop=(j == 7)), reads=RS(mT, wo), writes=RS(pa))
                        P.op("dve", lambda e, cs=cs, pa=pa, tm=tm: e.tensor_tensor(out=tm.t[:], in0=pa.t[:], in1=GATE1.t[:, cs], op=ALU.mult), reads=RS(pa, GATE1), writes=RS(tm))
                        P.op("pool", lambda e, cs=cs, tm=tm, x_=x_: e.tensor_tensor(out=xn.t[:, cs], in0=tm.t[:], in1=x_.t[:, cs], op=ALU.add), reads=RS(tm, x_), writes=RS(xn))
                    P.op("sp", lambda e, b=b, t=t: e.dma_start(out=xres(C, b, t), in_=xn.t[:]), reads=RS(xn), dma=True)
                    P.op("act", lambda e: e.activation(out=junk.t[:], in_=xn.t[:], func=ACT.Square, accum_out=ssq.t[:]), reads=RS(xn), writes=RS(junk, ssq))
                    P.op("act", lambda e: e.activation(out=rstd.t[:], in_=ssq.t[:], func=ACT.Sqrt, scale=1.0 / D, bias=EPS), reads=RS(ssq), writes=RS(rstd))
                    P.op("dve", lambda e: e.reciprocal(rstd.t[:], rstd.t[:]), reads=RS(rstd), writes=RS(rstd))
                    P.op("dve", lambda e: e.scalar_tensor_tensor(out=h2f.t[:], in0=xn.t[:], scalar=rstd.t[:, 0:1], in1=G2.t[:], op0=ALU.mult, op1=ALU.mult), reads=RS(xn, rstd, G2), writes=RS(h2f))
                    P.op("pool", lambda e: e.tensor_tensor(out=h2f.t[:], in0=h2f.t[:], in1=SH2.t[:], op=ALU.add), reads=RS(h2f, SH2), writes=RS(h2f))
                    P.op("act", lambda e: e.copy(h2b.t[:], h2f.t[:]), reads=RS(h2f), writes=RS(h2b))
                    if seg == 0:
                        P.op("sp", lambda e, b=b, tok0=tok0: e.dma_start(out=C.H2[b][tok0:tok0 + 128, :], in_=h2b.t[:]), reads=RS(h2b), dma=True)
                    else:
                        P.op("sp", lambda e, b=b, tok0=tok0: e.dma_start(out=C.H2C[b][tok0 - NL:tok0 - NL + 128, :], in_=h2b.t[:]), reads=RS(h2b), dma=True)
                    for half in range(2):
                        for jj in range(4):
                            j = half * 4 + jj
                            P.op("pe", lambda e, half=half, jj=jj, j=j: e.transpose(pTf[half].t[:, jj, :], h2f.t[:, j * 128:(j + 1) * 128], C.ident_f.t[:]), reads=RS(h2f, C.ident_f), writes=RS(pTf[half]))
                    P.op("act", lambda e: e.copy(h2T.t[:, 0:4, :], pTf[0].t[:]), reads=RS(pTf[0]), writes=RS(h2T))
                    P.op("dve", lambda e: e.tensor_copy(h2T.t[:, 4:8, :], pTf[1].t[:]), reads=RS(pTf[1]), writes=RS(h2T))
                    for j in range(8):
                        P.op("pe", lambda e, j=j: e.matmul(plog.t[:], h2T.t[:, j, :], wr.t[:, j, :], start=(j == 0), stop=(j == 7)), reads=RS(h2T, wr), writes=RS(plog))
                    P.op("dve", lambda e: e.tensor_reduce(out=mx.t[:], in_=plog.t[:], axis=AX.X, op=ALU.max), reads=RS(plog), writes=RS(mx))
                    P.op("dve", lambda e: e.tensor_scalar(mx.t[:], mx.t[:], -1.0, None, op0=ALU.mult), reads=RS(mx), writes=RS(mx))
                    P.op("act", lambda e: e.activation(out=ex.t[:], in_=plog.t[:], func=ACT.Exp, bias=mx.t[:, 0:1], accum_out=se.t[:]), reads=RS(plog, mx), writes=RS(ex, se))
                    P.op("dve", lambda e: e.reciprocal(se.t[:], se.t[:]), reads=RS(se), writes=RS(se))
                    P.op("dve", lambda e: e.tensor_scalar(aff.t[:], ex.t[:], se.t[:, 0:1], None, op0=ALU.mult), reads=RS(ex, se), writes=RS(aff))
                    P.op("pe", lambda e: e.transpose(paT.t[:], aff.t[:], C.ident_f.t[:]), reads=RS(aff, C.ident_f), writes=RS(paT))
                    P.op("act", lambda e: e.copy(affT.t[:], paT.t[:]), reads=RS(paT), writes=RS(affT))
                    P.op("sp", lambda e, b=b, tok0=tok0: e.dma_start(out=C.AFF[b, :, tok0:tok0 + 128], in_=affT.t[:]), reads=RS(affT), dma=True)


def alloc_route_tiles(C):
    g = C.es
    nc = C.nc
    pers = lambda shape, dt, nm: T(g.enter_context(nc.sbuf_tensor("c_" + nm, list(shape), dt)), nm)
    C.idxT = pers([128, 4, 32], U32, "idxT")
    C.valsT = pers([128, 4, 32], F32, "valsT")
    C.idxcT = pers([32, 32], U32, "idxcT")
    C.valscT = pers([32, 32], F32, "valscT")


def stage_topk(C, l):
    nc, P = C.nc, C.P
    last = (l == DEPTH - 1)
    with Stage(C, f"s5_{l}") as S:
        work = S.sb([32, NL], F32, "work")
        vals = S.sb([32, CAP_L], F32, "vals")
        idx = S.sb([32, CAP_L], U32, "idx")
        idxf = S.sb([32, CAP_L], F32, "idxf")
        pt = [S.ps([128, 32], F32, f"pt{i}") for i in range(2)]
        for b in range(NB):
            P.op("sp", lambda e, b=b: e.dma_start(out=work.t[b * 16:(b + 1) * 16, :], in_=C.AFF[b, :, 0:NL]), writes=RS(work), dma=True)
        for r in range(CAP_L // 8):
            rs_ = slice(r * 8, (r + 1) * 8)
            P.op("dve", lambda e, rs_=rs_: e.max(out=vals.t[:, rs_], in_=work.t[:]), reads=RS(work), writes=RS(vals))
            P.op("dve", lambda e, rs_=rs_: e.max_index(out=idx.t[:, rs_], in_max=vals.t[:, rs_], in_values=work.t[:]), reads=RS(work, vals), writes=RS(idx))
            P.op("dve", lambda e, rs_=rs_: e.match_replace(out=work.t[:], in_to_replace=vals.t[:, rs_], in_values=work.t[:], imm_value=-1.0), reads=RS(work, vals), writes=RS(work))
        P.op("dve", lambda e: e.tensor_copy(idxf.t[:], idx.t[:]), reads=RS(idx), writes=RS(idxf))
        k = 0
        for s in range(4):
            for src, dst in ((idxf, C.idxT), (vals, C.valsT)):
                p_ = pt[k % 2]
                k += 1
                P.op("pe", lambda e, p_=p_, src=src, s=s: e.transpose(p_.t[:], src.t[:, s * 128:(s + 1) * 128], C.ident_f.t[0:32, 0:32]), reads=RS(src, C.ident_f), writes=RS(p_))
                P.op("dve", lambda e, p_=p_, dst=dst, s=s: e.tensor_copy(dst.t[:, s, :], p_.t[:]), reads=RS(p_), writes=RS(dst))
        if not last:
            workc = S.sb([32, NCX], F32, "workc")
            valsc = S.sb([32, CAP_C], F32, "valsc")
            idxc = S.sb([32, CAP_C], U32, "idxc")
            idxcf = S.sb([32, CAP_C], F32, "idxcf")
            for b in range(NB):
                P.op("sp", lambda e, b=b: e.dma_start(out=workc.t[b * 16:(b + 1) * 16, :], in_=C.AFF[b, :, NL:NT]), writes=RS(workc), dma=True)
            for r in range(CAP_C // 8):
                rs_ = slice(r * 8, (r + 1) * 8)
                P.op("dve", lambda e, rs_=rs_: e.max(out=valsc.t[:, rs_], in_=workc.t[:]), reads=RS(workc), writes=RS(valsc))
                P.op("dve", lambda e, rs_=rs_: e.max_index(out=idxc.t[:, rs_], in_max=valsc.t[:, rs_], in_values=workc.t[:]), reads=RS(workc, valsc), writes=RS(idxc))
                P.op("dve", lambda e, rs_=rs_: e.match_replace(out=workc.t[:], in_to_replace=valsc.t[:, rs_], in_values=workc.t[:], imm_value=-1.0), reads=RS(workc, valsc), writes=RS(workc))
            P.op("dve", lambda e: e.tensor_copy(idxcf.t[:], idxc.t[:]), reads=RS(idxc), writes=RS(idxcf))
            for src, dst in ((idxcf, C.idxcT), (valsc, C.valscT)):
                p_ = pt[k % 2]
                k += 1
                P.op("pe", lambda e, p_=p_, src=src: e.transpose(p_.t[0:32, :], src.t[:], C.ident_f.t[0:32, 0:32]), reads=RS(src, C.ident_f), writes=RS(p_))
                P.op("dve", lambda e, p_=p_, dst=dst: e.tensor_copy(dst.t[:], p_.t[0:32, :]), reads=RS(p_), writes=RS(dst))
        if "dump_route" in DBG:
            P.op("sp", lambda e: e.dma_start(out=C.DIDX[l], in_=C.idxT.t[:].rearrange("p s c -> p (s c)")), reads=RS(C.idxT), dma=True)
            P.op("sp", lambda e: e.dma_start(out=C.DVAL[l], in_=C.valsT.t[:].rearrange("p s c -> p (s c)")), reads=RS(C.valsT), dma=True)


def stage_experts(C, l):
    nc, P, W = C.nc, C.P, C.W
    last = (l == DEPTH - 1)
    ncx = 0 if last else 2 * CAP_C
    NTOK = NB * CAP_L + ncx
    with Stage(C, f"s6_{l}") as S:
        xgT = S.sb([128, 8, NB * CAP_L + 2 * CAP_C], BF16, "xgT")
        hT = S.sb([128, FFC, NB * CAP_L + 2 * CAP_C], BF16, "hT")
        w2 = S.sb([128, FFC, D], BF16, "w2")
        wblk = [S.sb([128, 2, 8, 256], BF16, f"wblk{i}") for i in range(4)]
        xgs = [S.sb([128, D], BF16, f"xg{i}") for i in range(NB * 4 + NB)]
        ye = [S.sb([128, D], F32, f"ye{i}") for i in range(2)]
        stmp = [S.sb([128, 512], F32, f"stmp{i}") for i in range(2)]
        ytmp = [S.sb([128, 512], F32, f"ytmp{i}") for i in range(2)]
        G2g = [S.sb([128, D], F32, f"G2g{i}") for i in range(3)]
        pT = [S.ps([128, 8, 128], BF16, f"pT{i}") for i in range(2)]
        ps1 = [S.ps([128, 512], F32, f"ps1{i}") for i in range(2)]
        ps3 = [S.ps([128, 512], F32, f"ps3{i}") for i in range(2)]
        py = [S.ps([128, 512], F32, f"py{i}") for i in range(2)]
        for r in range(3):
            P.op("sp", lambda e, r=r: e.dma_start(out=G2g[r].t[:], in_=modrow(C, l, r, 5).to_broadcast([128, D])), writes=RS(G2g[r]), dma=True)
        xres_r = [Res(f"xres{b}") for b in range(NB)]
        xcres_r = [Res(f"xcres{b}") for b in range(NB)]
        ipT = iw = ips = iy = iye = 0
        experts = list(range(NE) if "s6_experts" not in DBG else range(DBG["s6_experts"]))
        n_g = NB * 4 + (0 if last else NB)

        def gather(ex):
            k = 0
            for b in range(NB):
                be = b * 16 + ex
                for s in range(4):
                    g_ = xgs[k]
                    k += 1
                    P.op("pool", lambda e, g_=g_, b=b, s=s, be=be: e.indirect_dma_start(
                        out=g_.t[:], out_offset=None, in_=C.H2[b], in_offset=bass.IndirectOffsetOnAxis(ap=C.idxT.t[:, s, be:be + 1], axis=0)),
                        reads=RS(C.idxT), writes=RS(g_), dma=True)
            if not last:
                for b in range(NB):
                    be = b * 16 + ex
                    g_ = xgs[k]
                    k += 1
                    P.op("pool", lambda e, g_=g_, b=b, be=be: e.indirect_dma_start(
                        out=g_.t[0:CAP_C, :], out_offset=None, in_=C.H2C[b], in_offset=bass.IndirectOffsetOnAxis(ap=C.idxcT.t[0:CAP_C, be:be + 1], axis=0)),
                        reads=RS(C.idxcT), writes=RS(g_), dma=True)

        def to_feature_major(ex):
            nonlocal ipT
            k = 0
            for b in range(NB):
                for s in range(4):
                    g_ = xgs[k]
                    k += 1
                    p_ = pT[ipT % 2]
                    ipT += 1
                    for j in range(8):
                        P.op("pe", lambda e, p_=p_, g_=g_, j=j: e.transpose(p_.t[:, j, :], g_.t[:, j * 128:(j + 1) * 128], C.ident_b.t[:]), reads=RS(g_, C.ident_b), writes=RS(p_))
                    c0 = b * CAP_L + s * 128
                    P.op("act", lambda e, p_=p_, c0=c0: e.copy(xgT.t[:, :, c0:c0 + 128], p_.t[:]), reads=RS(p_), writes=RS(xgT))
            if not last:
                for b in range(NB):
                    g_ = xgs[k]
                    k += 1
                    p_ = pT[ipT % 2]
                    ipT += 1
                    for j in range(8):
                        P.op("pe", lambda e, p_=p_, g_=g_, j=j: e.transpose(p_.t[:, j, 0:CAP_C], g_.t[0:CAP_C, j * 128:(j + 1) * 128], C.ident_b.t[0:CAP_C, 0:CAP_C]), reads=RS(g_, C.ident_b), writes=RS(p_))
                    c0 = NB * CAP_L + b * CAP_C
                    P.op("act", lambda e, p_=p_, c0=c0: e.copy(xgT.t[:, :, c0:c0 + CAP_C], p_.t[:, :, 0:CAP_C]), reads=RS(p_), writes=RS(xgT))

        pre_w = {}

        def load_w13(ex, fg):
            nonlocal iw
            wk = wblk[iw % 4]
            iw += 1
            for k, nm in enumerate(("w_e1", "w_e3")):
                P.op("pool", lambda e, wk=wk, k=k, nm=nm, fg=fg, ex=ex: e.dma_start(out=wk.t[:, k, :, :], in_=W[nm][l, ex, :, fg * 256:(fg + 1) * 256].rearrange("(j p) f -> p j f", p=128)),
                     writes=RS(wk), dma=True)
            return wk

        gather(experts[0])
        to_feature_major(experts[0])
        for iex, ex in enumerate(experts):
            nxt = experts[iex + 1] if iex + 1 < len(experts) else None
            for f0, f1 in ((0, 6), (6, 12), (12, 17), (17, 22)):
                P.op("pool", lambda e, f0=f0, f1=f1, ex=ex: e.dma_start(out=w2.t[:, f0:f1, :], in_=W["w_e2"][l, ex, f0 * 128:f1 * 128, :].rearrange("(f p) d -> p f d", p=128)),
                     writes=RS(w2), dma=True)
            groups = [(0, 384), (384, 384), (768, NTOK - 768)] if ncx else [(0, 512), (512, 512)]
            for fg in range(FFC // 2):
                wk = pre_w.pop((ex, fg)) if (ex, fg) in pre_w else load_w13(ex, fg)
                if fg == 4 and nxt is not None:
                    gather(nxt)
                for fc in range(2):
                    f = fg * 2 + fc
                    for (c0, n) in groups:
                        p1, p3 = ps1[ips % 2], ps3[ips % 2]
                        st_ = stmp[ips % 2]
                        ips += 1
                        for j in range(8):
                            P.op("pe", lambda e, p1=p1, wk=wk, j=j, fc=fc, c0=c0, n=n: e.matmul(p1.t[:, 0:n], wk.t[:, 0, j, fc * 128:(fc + 1) * 128], xgT.t[:, j, c0:c0 + n], start=(j == 0), stop=(j == 7)),
                                 reads=RS(wk, xgT), writes=RS(p1))
                        for j in range(8):
                            P.op("pe", lambda e, p3=p3, wk=wk, j=j, fc=fc, c0=c0, n=n: e.matmul(p3.t[:, 0:n], wk.t[:, 1, j, fc * 128:(fc + 1) * 128], xgT.t[:, j, c0:c0 + n], start=(j == 0), stop=(j == 7)),
                                 reads=RS(wk, xgT), writes=RS(p3))
                        P.op("act", lambda e, p1=p1, st_=st_, n=n: e.activation(out=st_.t[:, 0:n], in_=p1.t[:, 0:n], func=ACT.Silu), reads=RS(p1), writes=RS(st_))
                        P.op("dve", lambda e, p3=p3, st_=st_, f=f, c0=c0, n=n: e.tensor_tensor(out=hT.t[:, f, c0:c0 + n], in0=p3.t[:, 0:n], in1=st_.t[:, 0:n], op=ALU.mult), reads=RS(p3, st_), writes=RS(hT))
            if nxt is not None:
                for fg in range(4):
                    pre_w[(nxt, fg)] = load_w13(nxt, fg)
                to_feature_major(nxt)
            subt = [(b, s, b * CAP_L + s * 128, 128) for b in range(NB) for s in range(4)]
            if not last:
                subt += [(b, None, NB * CAP_L + b * CAP_C, CAP_C) for b in range(NB)]
            for (b, s, c0, m) in subt:
                be = b * 16 + ex
                ye_ = ye[iye % 2]
                iye += 1
                if s is not None:
                    gate_ap = C.valsT.t[:, s, be:be + 1]
                    gres = C.valsT
                    g2_ = G2g[b]
                else:
                    gate_ap = C.valscT.t[0:CAP_C, be:be + 1]
                    gres = C.valscT
                    g2_ = G2g[2]
                for n in range(2):
                    p_ = py[iy % 2]
                    yt_ = ytmp[iy % 2]
                    iy += 1
                    for f in range(FFC):
                        P.op("pe", lambda e, p_=p_, f=f, c0=c0, m=m, n=n: e.matmul(p_.t[0:m, :], hT.t[:, f, c0:c0 + m], w2.t[:, f, n * 512:(n + 1) * 512], start=(f == 0), stop=(f == FFC - 1)),
                             reads=RS(hT, w2), writes=RS(p_))
                    P.op("act", lambda e, p_=p_, yt_=yt_, m=m, gate_ap=gate_ap: e.activation(out=yt_.t[0:m, :], in_=p_.t[0:m, :], func=ACT.Copy, scale=gate_ap), reads=RS(p_, gres), writes=RS(yt_))
                    P.op("dve", lambda e, yt_=yt_, ye_=ye_, g2_=g2_, m=m, n=n: e.tensor_tensor(out=ye_.t[0:m, n * 512:(n + 1) * 512], in0=yt_.t[0:m, :], in1=g2_.t[0:m, n * 512:(n + 1) * 512], op=ALU.mult),
                         reads=RS(yt_, g2_), writes=RS(ye_))
                if s is not None:
                    P.op("pool", lambda e, ye_=ye_, b=b, s=s, be=be: e.indirect_dma_start(
                        out=C.out[b], out_offset=bass.IndirectOffsetOnAxis(ap=C.idxT.t[:, s, be:be + 1], axis=0), in_=ye_.t[:], in_offset=None, compute_op=ALU.add),
                        reads=RS(ye_, C.idxT), writes=[xres_r[b]], dma=True)
                else:
                    P.op("pool", lambda e, ye_=ye_, b=b, be=be: e.indirect_dma_start(
                        out=C.XC[b], out_offset=bass.IndirectOffsetOnAxis(ap=C.idxcT.t[0:CAP_C, be:be + 1], axis=0), in_=ye_.t[0:CAP_C, :], in_offset=None, compute_op=ALU.add),
                        reads=RS(ye_, C.idxcT), writes=[xcres_r[b]], dma=True)


def build_program(nc, n_layers=DEPTH, dbg_outs=(), no_experts=False, stages=None):
    C = setup(nc, dbg_outs=dbg_outs, n_layers=n_layers, no_experts=no_experts)
    if "dump_route" in DBG:
        C.DIDX = nc.dram_tensor("DIDX", [DEPTH, 128, 128], U32, kind="ExternalOutput").ap()
        C.DVAL = nc.dram_tensor("DVAL", [DEPTH, 128, 128], F32, kind="ExternalOutput").ap()
    stage_consts(C)
    alloc_route_tiles(C)
    fns = {"inproj": stage_inproj, "diff": stage_diff, "swa": stage_swa, "merge": stage_merge, "topk": stage_topk, "experts": stage_experts}
    order = ["inproj", "diff", "swa", "merge", "topk", "experts"]
    for l in range(n_layers):
        for nm in order:
            if stages is None or nm in stages:
                fns[nm](C, l)
    C.es.close()
    return C


def _rope_table():
    rows = NL // 64
    row = np.repeat(np.arange(rows), 64).astype(np.float32)
    col = np.tile(np.arange(64), rows).astype(np.float32)
    inv = (np.float32(10000.0) ** (-np.arange(0, 32, 2, dtype=np.float32) / np.float32(32))).astype(np.float32)
    ang = np.stack([row[:, None] * inv, col[:, None] * inv], axis=1).astype(np.float32)
    cs, sn = np.cos(ang).astype(np.float32), np.sin(ang).astype(np.float32)
    csx = np.repeat(cs[:, :, None, :], 2, axis=2).reshape(NL, 64)
    return np.ascontiguousarray(np.concatenate([csx, sn.reshape(NL, 32)], axis=1).astype(np.float32))


_NC_CACHE = {}


def kernel(**inputs):
    n_cores = 8
    if "nc" not in _NC_CACHE:
        nc = bass.Bass("TRN2", target_bir_lowering=False)
        build_program(nc)
        _NC_CACHE["nc"] = nc
    nc = _NC_CACHE["nc"]
    f32 = lambda a: np.ascontiguousarray(np.asarray(a, dtype=np.float32))
    x, c, ctx, c_ctx = f32(inputs["x"]), f32(inputs["c"]), f32(inputs["ctx"]), f32(inputs["c_ctx"])
    rope = _rope_table()
    shared = {}
    for n, s in WEIGHT_SPECS:
        shared[n] = f32(inputs[n]).reshape(s)
    in_maps = []
    for core in range(n_cores):
        b0 = core * NB
        cc = np.concatenate([c[b0:b0 + NB], c_ctx[None, :]], axis=0)
        m = {"x": x[b0:b0 + NB], "ctxin": ctx[b0:b0 + NB],
             "cT": np.ascontiguousarray(cc.reshape(3, 8, 128).transpose(2, 1, 0)), "rope": rope}
        m.update(shared)
        in_maps.append(m)
    res = run_bass_kernel_spmd(nc, in_maps, core_ids=list(range(n_cores)))
    out = np.empty((n_cores * NB, NL, D), np.float32)
    for core in range(n_cores):
        for b in range(NB):
            out[core * NB + b] = res.results[core][f"out{b}"]
    return out
```
